# Optimizing a Trainium2 kernel written in Bass

```python
import jax
import jax.numpy as jnp
from jax import lax
import numpy as np

D_MODEL = 2048
BATCH = 2
SEQ = 4096
DEPTH = 4
DEC_BATCH = 8
DEC_SEQ = 4
PAST_LEN = 16384
PAGE_SIZE = 128

N_EVEN = (DEPTH + 1) // 2
N_ODD = DEPTH // 2
N_MOD = 6
A_HEAD = 64
A_HEADS = 16
A_WIDTH = A_HEADS * A_HEAD
DECAY_LORA = 96
AAA_LORA = 96
GATE_LORA = 256
A_GN_EPS = 64e-5
B_WIDTH = 1024
B_CONV = 31
AB_IN = 3 * A_WIDTH + 2 * B_WIDTH
C_WINDOWS = (128, 512, 2048)
C_DILATIONS = (1, 4, 16)
C_GROUPS = len(C_WINDOWS)
C_HEADS = 8
C_HEAD = 128
C_WIDTH = C_HEADS * C_HEAD
C_QBLOCK = 128
C_SCALE = C_HEAD ** -0.5
D_FF = 5632
FFN_CONV = 3
RMS_EPS = 1e-6
LN_EPS = 1e-5

kernel_name = 'hybrid_rwkv7_conformer_dilated_attn_decoder_step'


def rmsnorm(x, g):
    xf = x.astype(jnp.float32)
    y = xf * lax.rsqrt(jnp.mean(xf * xf, axis=-1, keepdims=True) + RMS_EPS)
    return (y * g.astype(jnp.float32)).astype(x.dtype)


def layernorm(x, w, b, eps):
    xf = x.astype(jnp.float32)
    xc = xf - jnp.mean(xf, axis=-1, keepdims=True)
    var = jnp.mean(xc * xc, axis=-1, keepdims=True)
    return (xc * lax.rsqrt(var + eps) * w.astype(jnp.float32) + b.astype(jnp.float32)).astype(x.dtype)


def modulate(x, g, shift, scale):
    return rmsnorm(x, g) * (1 + scale) + shift


def causal_dwconv(buf, u, w, b):
    xx = jnp.concatenate([buf.astype(u.dtype), u], axis=1)
    y = lax.conv_general_dilated(xx, w[:, None, :].astype(u.dtype), window_strides=(1,), padding='VALID',
                                 dimension_numbers=('NWC', 'WIO', 'NWC'), feature_group_count=u.shape[-1])
    return y + b.astype(u.dtype), xx[:, xx.shape[1] - (w.shape[0] - 1):]


def wkv7_scan(s0, r, w, k, v, kk, a):
    def step(S, xs):
        r_t, w_t, k_t, v_t, kk_t, a_t = xs
        sa = jnp.einsum('bhvk,bhk->bhv', S, -kk_t)
        S = S * w_t[:, :, None, :] + sa[..., None] * (kk_t * a_t)[:, :, None, :] + v_t[..., None] * k_t[:, :, None, :]
        return S, jnp.einsum('bhvk,bhk->bhv', S, r_t)
    xs = tuple(jnp.moveaxis(t, 1, 0) for t in (r, w, k, v, kk, a))
    S, o = lax.scan(step, s0, xs)
    return jnp.moveaxis(o, 0, 1), S


def rwkv_conv_mixer(h, shift_prev, wkv_prev, convb_prev, W, e):
    f32 = jnp.float32
    Bn, T, _ = h.shape
    dt = h.dtype
    w_in = W['ab_w_in'][e]
    shift_prev = shift_prev.astype(dt)
    h_prev = jnp.concatenate([shift_prev[:, None], h[:, :-1]], axis=1)
    delta = h_prev - h
    p = h @ w_in
    pa = p[..., :3 * A_WIDTH]
    pa_first = (shift_prev @ w_in[:, :3 * A_WIDTH])[:, None]
    pa_prev = jnp.concatenate([pa_first, pa[:, :-1]], axis=1)
    rkv = (pa + (pa_prev - pa) * W['a_mu_rkv'][e]).astype(f32).reshape(Bn, T, 3, A_HEADS, A_HEAD)
    r, k, v = rkv[:, :, 0], rkv[:, :, 1], rkv[:, :, 2]
    mu = W['a_mu_wag'][e]
    xw = h + delta * mu[0]
    xa = h + delta * mu[1]
    xg = h + delta * mu[2]
    def heads(t):
        return t.astype(f32).reshape(Bn, T, A_HEADS, A_HEAD)
    def per_ch(t):
        return t.astype(f32).reshape(A_HEADS, A_HEAD)
    w_log = -jax.nn.softplus(-heads(W['a_w0'][e] + jnp.tanh(xw @ W['a_w1'][e]) @ W['a_w2'][e])) - 0.5
    decay = jnp.exp(-jnp.exp(w_log))
    a = jax.nn.sigmoid(heads(W['a_a0'][e] + (xa @ W['a_a1'][e]) @ W['a_a2'][e]))
    g = jax.nn.sigmoid(xg @ W['a_g1'][e]) @ W['a_g2'][e]
    kk = k * per_ch(W['a_k_k'][e])
    kk = kk * lax.rsqrt(jnp.maximum(jnp.sum(kk * kk, axis=-1, keepdims=True), 1e-24))
    k = k * (1 + (a - 1) * per_ch(W['a_k_a'][e]))
    o, S = wkv7_scan(wkv_prev.astype(f32), r, decay, k, v, kk, a)
    o = layernorm(o, per_ch(W['a_ln_w'][e]), per_ch(W['a_ln_b'][e]), A_GN_EPS)
    o = o + jnp.sum(r * k * W['a_r_k'][e].astype(f32), axis=-1, keepdims=True) * v
    o_a = o.reshape(Bn, T, A_WIDTH).astype(dt) * g
    pb = p[..., 3 * A_WIDTH:]
    u = pb[..., :B_WIDTH] * jax.nn.sigmoid(pb[..., B_WIDTH:])
    ub, convb_new = causal_dwconv(convb_prev, u, W['b_conv_w'][e], W['b_conv_b'][e])
    o_b = jax.nn.silu(layernorm(ub, W['b_ln_w'][e], W['b_ln_b'][e], LN_EPS))
    y = jnp.concatenate([o_a, o_b.astype(dt)], axis=-1) @ W['ab_w_out'][e]
    return y, h[:, -1], S, convb_new


def dilated_attention(q, kc, vc, q_off, dil, n_keys):
    Bn, T, H, hd = q.shape
    L = kc.shape[1]
    qb = min(T, C_QBLOCK)
    nb = -(-T // qb)
    qp = jnp.pad(q, ((0, 0), (0, nb * qb - T), (0, 0), (0, 0)))
    qblocks = jnp.moveaxis(qp.reshape(Bn, nb, qb, H, hd), 1, 0)
    offs = dil * jnp.arange(n_keys)
    def block(args):
        qblk, bi = args
        t = q_off + bi * qb + jnp.arange(qb)
        pos = t[:, None] - offs[None, :]
        valid = pos >= 0
        idx = jnp.clip(pos, 0, L - 1)
        kg = jnp.take(kc, idx, axis=1)
        vg = jnp.take(vc, idx, axis=1)
        s = jnp.einsum('bqhd,bqjhd->bhqj', qblk, kg).astype(jnp.float32) * C_SCALE
        s = jnp.where(valid[None, None], s, -jnp.inf)
        lse = jax.nn.logsumexp(s, axis=-1)
        p = jnp.exp(s - lse[..., None]).astype(vg.dtype)
        return jnp.einsum('bhqj,bqjhd->bqhd', p, vg), lse
    o, lse = lax.map(block, (qblocks, jnp.arange(nb)))
    o = jnp.moveaxis(o, 0, 1).reshape(Bn, nb * qb, H, hd)[:, :T]
    lse = jnp.transpose(lse, (1, 0, 3, 2)).reshape(Bn, nb * qb, H)[:, :T]
    return o, lse


def dilated_mixer(h, kv_bufs, w_qkv, w_out):
    Bn, T, _ = h.shape
    p = (h @ w_qkv).reshape(Bn, T, 3, C_GROUPS, C_HEADS, C_HEAD)
    outs, lses, rows = [], [], []
    for gi in range(C_GROUPS):
        win, dil = C_WINDOWS[gi], C_DILATIONS[gi]
        q, k, v = p[:, :, 0, gi], p[:, :, 1, gi], p[:, :, 2, gi]
        buf = kv_bufs[gi].astype(h.dtype)
        kc = jnp.concatenate([buf[0], k], axis=1)
        vc = jnp.concatenate([buf[1], v], axis=1)
        o, lse = dilated_attention(q, kc, vc, buf.shape[2], dil, win // dil + 1)
        outs.append(o.astype(jnp.float32))
        lses.append(lse)
        keep = min(win, T)
        rows.append(jnp.stack([k[:, T - keep:], v[:, T - keep:]]))
    alpha = jax.nn.softmax(jnp.stack(lses), axis=0)
    o = jnp.sum(alpha[..., None] * jnp.stack(outs), axis=0)
    y = o.reshape(Bn, T, C_WIDTH).astype(h.dtype) @ w_out
    return y, rows


def conv_ffn(h, buf, W, l):
    gate = h @ W['ffn_w_gate'][l]
    up = h @ W['ffn_w_up'][l]
    gc, buf_new = causal_dwconv(buf, gate, W['ffn_conv_w'][l], W['ffn_conv_b'][l])
    return (jax.nn.silu(gc) * up) @ W['ffn_w_down'][l], buf_new


def trunk(x, c, shift_s, wkv_s, convb_s, ffn_s, kv_s, W):
    Bn = x.shape[0]
    new_shift, new_wkv, new_convb, new_ffn = [], [], [], []
    new_kv = [[] for _ in C_WINDOWS]
    for l in range(DEPTH):
        mod = (c @ W['w_mod'][l] + W['b_mod'][l]).reshape(Bn, N_MOD, 1, D_MODEL)
        h = modulate(x, W['g_pre_mix'][l], mod[:, 0], mod[:, 1])
        if l % 2 == 0:
            e = l // 2
            y, s_shift, s_wkv, s_convb = rwkv_conv_mixer(h, shift_s[e], wkv_s[e], convb_s[e], W, e)
            new_shift.append(s_shift)
            new_wkv.append(s_wkv)
            new_convb.append(s_convb)
        else:
            o = l // 2
            y, rows = dilated_mixer(h, tuple(kv[o] for kv in kv_s), W['attn_w_qkv'][o], W['attn_w_out'][o])
            for gi in range(C_GROUPS):
                new_kv[gi].append(rows[gi])
        x = x + mod[:, 2] * rmsnorm(y, W['g_post_mix'][l])
        h = modulate(x, W['g_pre_ffn'][l], mod[:, 3], mod[:, 4])
        y, s_ffn = conv_ffn(h, ffn_s[l], W, l)
        new_ffn.append(s_ffn)
        x = x + mod[:, 5] * rmsnorm(y, W['g_post_ffn'][l])
    return (x, jnp.stack(new_shift), jnp.stack(new_wkv), jnp.stack(new_convb), jnp.stack(new_ffn),
            jnp.stack(new_kv[0]), jnp.stack(new_kv[1]), jnp.stack(new_kv[2]))


def setup_inputs(seed: int = 0) -> dict:
    key = jax.random.key(seed)
    ks = iter(jax.random.split(key, 64))
    f32 = jnp.float32
    D = D_MODEL
    def nrm(shape, scale=1.0):
        return jax.random.normal(next(ks), shape, f32) * scale
    def unif(shape, lo, hi):
        return jax.random.uniform(next(ks), shape, f32, lo, hi)
    def gain(shape):
        return 1.0 + nrm(shape, 0.02)
    kv_len = [min(w, PAST_LEN) for w in C_WINDOWS]
    return {
        'x_prompt': nrm((BATCH, SEQ, D)),
        'x_sample': nrm((DEC_BATCH, DEC_SEQ, D)),
        'state_shift': nrm((N_EVEN, DEC_BATCH, D)),
        'state_wkv': nrm((N_EVEN, DEC_BATCH, A_HEADS, A_HEAD, A_HEAD), 0.5),
        'state_conv_b': nrm((N_EVEN, DEC_BATCH, B_CONV - 1, B_WIDTH), 0.5),
        'state_ffn': nrm((DEPTH, DEC_BATCH, FFN_CONV - 1, D_FF)),
        'cache_kv_w128': nrm((N_ODD, 2, DEC_BATCH, kv_len[0], C_HEADS, C_HEAD)),
        'cache_kv_w512': nrm((N_ODD, 2, DEC_BATCH, kv_len[1], C_HEADS, C_HEAD)),
        'cache_kv_w2048': nrm((N_ODD, 2, DEC_BATCH, kv_len[2], C_HEADS, C_HEAD)),
        'c_prompt': nrm((BATCH, D)),
        'c_sample': nrm((DEC_BATCH, D)),
        'w_mod': nrm((DEPTH, D, N_MOD * D), 0.5 * D ** -0.5),
        'b_mod': nrm((DEPTH, N_MOD * D), 0.02),
        'g_pre_mix': gain((DEPTH, D)),
        'g_post_mix': gain((DEPTH, D)),
        'g_pre_ffn': gain((DEPTH, D)),
        'g_post_ffn': gain((DEPTH, D)),
        'ab_w_in': nrm((N_EVEN, D, AB_IN), D ** -0.5),
        'a_mu_rkv': unif((N_EVEN, 3 * A_WIDTH), 0.0, 1.0),
        'a_mu_wag': unif((N_EVEN, 3, D), 0.0, 1.0),
        'a_w0': unif((N_EVEN, A_WIDTH), -6.0, 1.0),
        'a_w1': nrm((N_EVEN, D, DECAY_LORA), D ** -0.5),
        'a_w2': nrm((N_EVEN, DECAY_LORA, A_WIDTH), 0.5 * DECAY_LORA ** -0.5),
        'a_a0': nrm((N_EVEN, A_WIDTH), 0.1),
        'a_a1': nrm((N_EVEN, D, AAA_LORA), D ** -0.5),
        'a_a2': nrm((N_EVEN, AAA_LORA, A_WIDTH), 0.5 * AAA_LORA ** -0.5),
        'a_g1': nrm((N_EVEN, D, GATE_LORA), D ** -0.5),
        'a_g2': nrm((N_EVEN, GATE_LORA, A_WIDTH), GATE_LORA ** -0.5),
        'a_k_k': 0.85 + nrm((N_EVEN, A_WIDTH), 0.05),
        'a_k_a': 1.0 + nrm((N_EVEN, A_WIDTH), 0.05),
        'a_r_k': nrm((N_EVEN, A_HEADS, A_HEAD), 0.1),
        'a_ln_w': gain((N_EVEN, A_WIDTH)),
        'a_ln_b': nrm((N_EVEN, A_WIDTH), 0.02),
        'b_conv_w': nrm((N_EVEN, B_CONV, B_WIDTH), B_CONV ** -0.5),
        'b_conv_b': nrm((N_EVEN, B_WIDTH), 0.02),
        'b_ln_w': gain((N_EVEN, B_WIDTH)),
        'b_ln_b': nrm((N_EVEN, B_WIDTH), 0.02),
        'ab_w_out': nrm((N_EVEN, A_WIDTH + B_WIDTH, D), (A_WIDTH + B_WIDTH) ** -0.5),
        'attn_w_qkv': nrm((N_ODD, D, 3 * C_GROUPS * C_WIDTH), D ** -0.5),
        'attn_w_out': nrm((N_ODD, C_WIDTH, D), C_WIDTH ** -0.5),
        'ffn_w_gate': nrm((DEPTH, D, D_FF), D ** -0.5),
        'ffn_w_up': nrm((DEPTH, D, D_FF), D ** -0.5),
        'ffn_conv_w': nrm((DEPTH, FFN_CONV, D_FF), FFN_CONV ** -0.5),
        'ffn_conv_b': nrm((DEPTH, D_FF), 0.02),
        'ffn_w_down': nrm((DEPTH, D_FF, D), D_FF ** -0.5),
    }


def reference(x_prompt, x_sample, state_shift, state_wkv, state_conv_b, state_ffn,
              cache_kv_w128, cache_kv_w512, cache_kv_w2048, c_prompt, c_sample,
              w_mod, b_mod, g_pre_mix, g_post_mix, g_pre_ffn, g_post_ffn,
              ab_w_in, a_mu_rkv, a_mu_wag, a_w0, a_w1, a_w2, a_a0, a_a1, a_a2, a_g1, a_g2,
              a_k_k, a_k_a, a_r_k, a_ln_w, a_ln_b, b_conv_w, b_conv_b, b_ln_w, b_ln_b, ab_w_out,
              attn_w_qkv, attn_w_out, ffn_w_gate, ffn_w_up, ffn_conv_w, ffn_conv_b, ffn_w_down):
    W = dict(w_mod=w_mod, b_mod=b_mod, g_pre_mix=g_pre_mix, g_post_mix=g_post_mix,
             g_pre_ffn=g_pre_ffn, g_post_ffn=g_post_ffn, ab_w_in=ab_w_in, a_mu_rkv=a_mu_rkv,
             a_mu_wag=a_mu_wag, a_w0=a_w0, a_w1=a_w1, a_w2=a_w2, a_a0=a_a0, a_a1=a_a1, a_a2=a_a2,
             a_g1=a_g1, a_g2=a_g2, a_k_k=a_k_k, a_k_a=a_k_a, a_r_k=a_r_k, a_ln_w=a_ln_w, a_ln_b=a_ln_b,
             b_conv_w=b_conv_w, b_conv_b=b_conv_b, b_ln_w=b_ln_w, b_ln_b=b_ln_b, ab_w_out=ab_w_out,
             attn_w_qkv=attn_w_qkv, attn_w_out=attn_w_out, ffn_w_gate=ffn_w_gate, ffn_w_up=ffn_w_up,
             ffn_conv_w=ffn_conv_w, ffn_conv_b=ffn_conv_b, ffn_w_down=ffn_w_down)
    nbp = x_prompt.shape[0]
    dt = x_prompt.dtype
    zero_kv = tuple(jnp.zeros((N_ODD, 2, nbp, 0, C_HEADS, C_HEAD), dt) for _ in C_WINDOWS)
    (y_prompt, p_shift, p_wkv, p_conv_b, p_ffn, p_kv_w128, p_kv_w512, p_kv_w2048) = trunk(
        x_prompt, c_prompt,
        jnp.zeros((N_EVEN, nbp, D_MODEL), dt),
        jnp.zeros((N_EVEN, nbp, A_HEADS, A_HEAD, A_HEAD), jnp.float32),
        jnp.zeros((N_EVEN, nbp, B_CONV - 1, B_WIDTH), dt),
        jnp.zeros((DEPTH, nbp, FFN_CONV - 1, D_FF), dt),
        zero_kv, W)
    (y_sample, s_shift, s_wkv, s_conv_b, s_ffn, s_kv_w128, s_kv_w512, s_kv_w2048) = trunk(
        x_sample, c_sample, state_shift, state_wkv, state_conv_b, state_ffn,
        (cache_kv_w128, cache_kv_w512, cache_kv_w2048), W)
    return (y_prompt, y_sample, p_shift, p_wkv, p_conv_b, p_ffn, p_kv_w128, p_kv_w512, p_kv_w2048,
            s_shift, s_wkv, s_conv_b, s_ffn, s_kv_w128, s_kv_w512, s_kv_w2048)
```

```python
import contextlib
import os
import numpy as np
import concourse.bass as bass
import concourse.mybir as mybir
from concourse.bass_utils import run_bass_kernel_spmd

F32 = mybir.dt.float32
BF16 = mybir.dt.bfloat16
ALU = mybir.AluOpType
AF = mybir.ActivationFunctionType
AX = mybir.AxisListType


class Res:
    __slots__ = ("name", "w", "rd", "excl")

    def __init__(self, name):
        self.name = name
        self.excl = False
        self.w = None
        self.rd = {}


class T:
    def __init__(self, t, name, nres=1):
        self.t = t
        self.name = name
        self.rs = [Res(f"{name}.{i}") for i in range(nres)]

    @property
    def r(self):
        return self.rs[0]

    def __getitem__(self, idx):
        return self.t[idx]


class Eng:
    def __init__(self, b, key, h):
        self.b, self.key, self.h = b, key, h
        self.sem = None
        self.semid = None
        self.cnt = 0
        self.seen = {}
        self.nins = 0


class B:
    EPOCH = 8000
    NSLOT = 12

    def __init__(self):
        self.nc = bass.Bass("TRN2", target_bir_lowering=False)
        self.es = contextlib.ExitStack()
        self.semes = self.es
        self._stk = []
        self.old_latest = {}
        nc = self.nc
        self.E = {k: Eng(self, k, h) for k, h in
                  (("pe", nc.tensor), ("dve", nc.vector), ("act", nc.scalar), ("pool", nc.gpsimd), ("sp", nc.sync))}
        self.sems = {}
        self.nsem = 0
        self.slots = {}
        for q in ("sp", "pool"):
            self.slots[q] = [[self._newsem(f"d{q}{i}"), 0] for i in range(self.NSLOT)]
        self.slot_rr = {"sp": 0, "pool": 0}
        self.uid = 0

    def _newsem(self, name):
        h = self.semes.enter_context(self.nc.semaphore(f"{name}_{self.nsem}"))
        key = self.nsem
        self.sems[key] = h
        self.nsem += 1
        return key

    def sb(self, name, shape, dt=F32, nres=1):
        self.uid += 1
        name = f"{name}_u{self.uid}"
        t = self.es.enter_context(self.nc.sbuf_tensor(name, list(shape), dt))
        return T(t, name, nres)

    def ps(self, name, shape, dt=F32):
        t = self.es.enter_context(self.nc.psum_tensor(name, list(shape), dt))
        tt_ = T(t, name)
        tt_.rs[0].excl = True
        return tt_

    def dram(self, name, shape, dt, kind="Internal", nres=1):
        t = self.nc.dram_tensor(name, list(shape), dt, kind=kind).ap()
        return T(t, name, nres)

    def _needs(self, reads, writes):
        need = {}

        def add(kv):
            if kv is None:
                return
            k, v = kv
            if need.get(k, 0) < v:
                need[k] = v
        for r in reads:
            add(r.w)
            if r.excl:
                for kv in r.rd.items():
                    add(kv)
        for r in writes:
            add(r.w)
            for kv in r.rd.items():
                add(kv)
        return need

    def _emit_waits(self, e, need, skip_self=False):
        for k, v in need.items():
            if skip_self and k == e.semid:
                continue
            if e.seen.get(k, 0) >= v:
                continue
            e.h.wait_ge(self.sems[k], v)
            e.seen[k] = v

    @staticmethod
    def _resl(x):
        out = []
        for a in x:
            if isinstance(a, T):
                out.extend(a.rs)
            elif isinstance(a, Res):
                out.append(a)
            elif a is None:
                pass
            else:
                out.extend(B._resl(a))
        return out

    def op(self, ek, fn, reads=(), writes=()):
        e = self.E[ek]
        reads = self._resl(reads)
        writes = self._resl(writes)
        if e.sem is None or e.cnt >= self.EPOCH:
            if e.semid is not None:
                self.old_latest[e.semid] = e.cnt
            e.semid = self._newsem(f"e{ek}")
            e.sem = self.sems[e.semid]
            e.cnt = 0
        need = self._needs(reads, writes)
        self._emit_waits(e, need, skip_self=(ek == "pe"))
        ins = fn(e.h)
        e.cnt += 1
        e.nins += 1
        ins.then_inc(e.sem, 1)
        kv = (e.semid, e.cnt)
        for r in reads:
            r.rd[e.semid] = e.cnt
        for r in writes:
            r.w = kv
            r.rd = {}
        return ins

    def dma(self, q, out, in_, reads=(), writes=(), **kw):
        e = self.E[q]
        reads = self._resl(reads)
        writes = self._resl(writes)
        need = self._needs(reads, writes)
        i = self.slot_rr[q]
        self.slot_rr[q] = (i + 1) % self.NSLOT
        slot = self.slots[q][i]
        if slot[1] > 0:
            need[slot[0]] = max(need.get(slot[0], 0), 16 * slot[1])
        if slot[1] >= 500:
            self.old_latest[slot[0]] = 16 * slot[1]
            slot[0] = self._newsem(f"d{q}{i}")
            slot[1] = 0
        self._emit_waits(e, need)
        ins = e.h.dma_start(out=out, in_=in_, **kw)
        slot[1] += 1
        e.nins += 1
        ins.then_inc(self.sems[slot[0]], 16)
        kv = (slot[0], 16 * slot[1])
        for r in reads:
            r.rd[slot[0]] = 16 * slot[1]
        for r in writes:
            r.w = kv
            r.rd = {}
        return ins

    def push(self):
        self._stk.append(self.es)
        self.es = contextlib.ExitStack()

    def pop(self):
        self.barrier()
        self.es.close()
        self.es = self._stk.pop()

    def barrier(self):
        latest = {}
        for e in self.E.values():
            if e.semid is not None:
                latest[e.semid] = e.cnt
        for q in self.slots:
            for sl in self.slots[q]:
                if sl[1] > 0:
                    latest[sl[0]] = 16 * sl[1]
        for k, v in self.old_latest.items():
            latest.setdefault(k, v)
        for e in self.E.values():
            self._emit_waits(e, latest, skip_self=False)

    def finish(self, outs):
        e = self.E["sp"]
        need = self._needs(self._resl(outs), [])
        self._emit_waits(e, need)

    def close(self):
        self.es.close()


D = 2048
TP = 4096
TS = 4
NT = TP + TS
NKC = 16
DFF = 5632
NFC = 44
TILES = [(i * 512, 512, 0) for i in range(8)] + [(TP, TS, 1)]
EXPC = 0.6065306597126334
C_SCALE = 128 ** -0.5
WINS = (128, 512, 2048)
DO_PROMPT = [True]
DILS = (1, 4, 16)


def build_program():
    b = B()
    nc = b.nc
    nc_cm = nc.allow_non_contiguous_dma(reason="small param / state layout transforms")
    nc_cm.__enter__()
    I = {}

    def inp(name, shape):
        I[name] = b.dram(name, shape, F32, kind="ExternalInput")
        return I[name]

    def outp(name, shape, nres=1):
        I[name] = b.dram(name, shape, F32, kind="ExternalOutput", nres=nres)
        return I[name]

    xp = inp("xp", [TP, D]); xs = inp("xs", [TS, D]); cc = inp("cc", [2, D])
    st_shift = inp("st_shift", [2, D]); st_wkv = inp("st_wkv", [2, 16, 64, 64])
    st_convb = inp("st_convb", [2, 30, 1024]); st_ffn = inp("st_ffn", [4, 2, DFF])
    cks = [inp(f"ck{w}", [2, 2, w, 1024]) for w in WINS]
    w_mod = inp("w_mod", [4, D, 6 * D]); b_mod = inp("b_mod", [4, 6 * D])
    gpar = {k: inp(k, [4, D]) for k in ("g_pre_mix", "g_post_mix", "g_pre_ffn", "g_post_ffn")}
    ab_w_in = inp("ab_w_in", [2, D, 5120]); a_mu_rkv = inp("a_mu_rkv", [2, 3072]); a_mu_wag = inp("a_mu_wag", [2, 3, D])
    a_w0 = inp("a_w0", [2, 1024]); a_w1 = inp("a_w1", [2, D, 96]); a_w2 = inp("a_w2", [2, 96, 1024])
    a_a0 = inp("a_a0", [2, 1024]); a_a1 = inp("a_a1", [2, D, 96]); a_a2 = inp("a_a2", [2, 96, 1024])
    a_g1 = inp("a_g1", [2, D, 256]); a_g2 = inp("a_g2", [2, 256, 1024])
    a_k_k = inp("a_k_k", [2, 1024]); a_k_a = inp("a_k_a", [2, 1024]); a_r_k = inp("a_r_k", [2, 1024])
    a_ln_w = inp("a_ln_w", [2, 1024]); a_ln_b = inp("a_ln_b", [2, 1024])
    b_conv_w = inp("b_conv_w", [2, 31, 1024]); b_conv_b = inp("b_conv_b", [2, 1024])
    b_ln_w = inp("b_ln_w", [2, 1024]); b_ln_b = inp("b_ln_b", [2, 1024])
    ab_w_out = inp("ab_w_out", [2, D, D])
    attn_w_qkv = inp("attn_w_qkv", [2, D, 9216]); attn_w_out = inp("attn_w_out", [2, 1024, D])
    ffn_w_gate = inp("ffn_w_gate", [4, D, DFF]); ffn_w_up = inp("ffn_w_up", [4, D, DFF])
    ffn_conv_w = inp("ffn_conv_w", [4, 3, DFF]); ffn_conv_b = inp("ffn_conv_b", [4, DFF]); ffn_w_down = inp("ffn_w_down", [4, DFF, D])

    yp = outp("yp", [TP, D], nres=8); ys = outp("ys", [TS, D])
    o_shift = [outp("p_shift", [2, D]), outp("s_shift", [2, D])]
    o_wkv = [outp("p_wkv", [2, 16, 64, 64]), outp("s_wkv", [2, 16, 64, 64])]
    o_convb = [outp("p_convb", [2, 30, 1024]), outp("s_convb", [2, 30, 1024])]
    o_ffn = [outp("p_ffn", [4, 2, DFF]), outp("s_ffn", [4, 2, DFF])]
    o_kv = [[outp(f"pkv{w}", [2, 2, w, 1024]) for w in WINS], [outp(f"skv{w}", [2, 2, TS, 1024]) for w in WINS]]
    all_outs = [yp, ys] + o_shift + o_wkv + o_convb + o_ffn + o_kv[0] + o_kv[1]

    X = b.dram("X", [D, NT], F32, nres=9)
    sR, sK2, sV, sW, sA, sBb, sG, sU, sO = [b.dram(n, [1024, NT], F32, nres=9) for n in
                                          ("sR", "sK2", "sV", "sW", "sA", "sBb", "sG", "sU", "sO")]
    sVtm = b.dram("sVtm", [NT, 1024], F32, nres=9)
    sQT = b.dram("sQT", [3072, NT], BF16, nres=9); sKT = b.dram("sKT", [3072, NT], BF16, nres=9)
    sVa = b.dram("sVa", [NT, 3072], BF16, nres=9); sOT = b.dram("sOT", [1024, NT], BF16, nres=9)

    idf = b.sb("idf", [128, 128]); idb = b.sb("idb", [128, 128], BF16)
    onesf = b.sb("onesf", [128, 128]); onesb = b.sb("onesb", [128, 128], BF16)
    blk1 = b.sb("blk1", [128, 128])
    sel = b.sb("sel", [2, 128]); maskj = b.sb("maskj", [128, 2])
    mask2 = b.sb("mask2", [128, 256], BF16)
    b.op("pool", lambda e: e.memset(idf[:], 1.0), writes=[idf])
    b.op("pool", lambda e: e.affine_select(out=idf[:], in_=idf[:], pattern=[[-1, 128]], compare_op=ALU.is_equal, fill=0.0, base=0, channel_multiplier=1), reads=[idf], writes=[idf])
    b.op("dve", lambda e: e.tensor_copy(out=idb[:], in_=idf[:]), reads=[idf], writes=[idb])
    b.op("pool", lambda e: e.memset(onesf[:], 1.0), writes=[onesf])
    b.op("pool", lambda e: e.memset(onesb[:], 1.0), writes=[onesb])
    b.op("pool", lambda e: e.memset(sel[:], 1.0), writes=[sel])
    b.op("pool", lambda e: e.affine_select(out=sel[:], in_=sel[:], pattern=[[1, 128]], compare_op=ALU.is_ge, fill=0.0, base=0, channel_multiplier=-64), reads=[sel], writes=[sel])
    b.op("pool", lambda e: e.affine_select(out=sel[:], in_=sel[:], pattern=[[-1, 128]], compare_op=ALU.is_ge, fill=0.0, base=63, channel_multiplier=64), reads=[sel], writes=[sel])
    pst = [b.ps(f"pst{i}", [128, 512]) for i in range(8)]
    b.op("pe", lambda e: e.matmul(pst[0][:, 0:128], lhsT=sel[:], rhs=sel[:], start=True, stop=True), reads=[sel], writes=[pst[0]])
    b.op("dve", lambda e: e.tensor_copy(out=blk1[:], in_=pst[0][:, 0:128]), reads=[pst[0]], writes=[blk1])
    b.op("pe", lambda e: e.matmul(pst[1][:, 0:2], lhsT=sel[:], rhs=idf[0:2, 0:2], start=True, stop=True), reads=[sel, idf], writes=[pst[1]])
    b.op("dve", lambda e: e.tensor_copy(out=maskj[:], in_=pst[1][:, 0:2]), reads=[pst[1]], writes=[maskj])
    b.op("pool", lambda e: e.memset(mask2[:], 1.0), writes=[mask2])
    b.op("pool", lambda e: e.affine_select(out=mask2[:, 0:128], in_=mask2[:, 0:128], pattern=[[1, 128]], compare_op=ALU.is_ge, fill=0.0, base=0, channel_multiplier=-1), reads=[mask2], writes=[mask2])
    b.op("pool", lambda e: e.affine_select(out=mask2[:, 128:256], in_=mask2[:, 128:256], pattern=[[-1, 128]], compare_op=ALU.is_ge, fill=0.0, base=0, channel_multiplier=1), reads=[mask2], writes=[mask2])

    xt = b.sb("xt", [128, NKC, 512])
    S = {}
    hx = b.sb("hx", [128, NKC, 514], BF16)
    hf = b.sb("hf", [128, 513])
    wb = [b.sb(f"wb{i}", [128, 8192], BF16) for i in range(2)]
    wsel = [0]
    t1 = b.sb("t1", [128, 512]); t2 = b.sb("t2", [128, 512]); t3 = b.sb("t3", [128, 512]); t4 = b.sb("t4", [128, 512])
    rstd = b.sb("rstd", [128, 512])
    carry_h = b.sb("carry_h", [128, NKC, 1], BF16)
    colf = b.sb("colf", [128, NKC, 1])

    def fmvec(dst_ap, dram_ap_1d, q="sp", reads=(), writes=()):
        b.dma(q, dst_ap, dram_ap_1d.rearrange("(k p) -> p k", p=128), reads=reads, writes=writes)

    def load_w(wap, nk, ncols, krows=128):
        t = wb[wsel[0]]
        wsel[0] ^= 1
        v = t[0:krows, 0:nk * ncols].rearrange("p (k c) -> p k c", c=ncols)
        b.dma("pool", v, wap.rearrange("(k p) c -> p k c", p=krows), writes=[t])
        return t, v

    modv = b.sb("modv", [128, 4, 96, 2]); bmod = b.sb("bmod", [128, 4, 96])
    ccT = b.sb("ccT", [128, NKC, 2], BF16)
    b.push()
    ccf = b.sb("ccf", [2, D])
    b.dma("sp", ccf[:], cc[:, :], writes=[ccf])
    for kc in range(NKC):
        b.op("pe", lambda e: e.transpose(pst[kc % 2][:, 0:2], ccf[0:2, kc * 128:(kc + 1) * 128], idf[0:2, 0:2]), reads=[ccf, idf], writes=[pst[kc % 2]])
        b.op("dve", lambda e: e.tensor_copy(out=ccT[:, kc, :], in_=pst[kc % 2][:, 0:2]), reads=[pst[kc % 2]], writes=[ccT])
    b.pop()
    for l in range(4):
        fmvec(bmod[:, l, :], b_mod[l, :], writes=[bmod])
        for gq in range(24):
            wt_, wv = load_w(w_mod[l, :, gq * 512:(gq + 1) * 512], NKC, 512)
            for mc in range(4):
                ps = pst[(gq * 4 + mc) % 4]
                for kc in range(NKC):
                    b.op("pe", lambda e: e.matmul(ps[:, 0:2], lhsT=wv[:, kc, mc * 128:(mc + 1) * 128], rhs=ccT[:, kc, :], start=(kc == 0), stop=(kc == NKC - 1)), reads=[wt_, ccT], writes=[ps])
                ch = gq * 4 + mc
                b.op("dve", lambda e: e.tensor_scalar(out=modv[:, l, ch, :], in0=ps[:, 0:2], scalar1=bmod[:, l, ch:ch + 1], scalar2=None, op0=ALU.add), reads=[ps, bmod], writes=[modv])

    gv = {k: b.sb("gv_" + k, [128, 4, NKC]) for k in gpar}
    for k in gpar:
        for l in range(4):
            fmvec(gv[k][:, l, :], gpar[k][l, :], writes=[gv[k]])
    gs = b.sb("gs", [128, 4, 2, NKC, 2])
    for l in range(4):
        for wi, (gk, si) in enumerate((("g_pre_mix", 1), ("g_pre_ffn", 4))):
            for ctx in range(2):
                b.op("dve", lambda e: e.tensor_scalar(out=gs[:, l, wi, :, ctx], in0=modv[:, l, si * 16:(si + 1) * 16, ctx], scalar1=1.0, scalar2=None, op0=ALU.add), reads=[modv], writes=[gs])
                b.op("dve", lambda e: e.tensor_tensor(out=gs[:, l, wi, :, ctx], in0=gs[:, l, wi, :, ctx], in1=gv[gk][:, l, :], op=ALU.mult), reads=[gs, gv[gk]], writes=[gs])
    gp = b.sb("gp", [128, 4, 2, NKC, 2])
    for l in range(4):
        for wi, (gk, gi) in enumerate((("g_post_mix", 2), ("g_post_ffn", 5))):
            for ctx in range(2):
                b.op("dve", lambda e: e.tensor_tensor(out=gp[:, l, wi, :, ctx], in0=modv[:, l, gi * 16:(gi + 1) * 16, ctx], in1=gv[gk][:, l, :], op=ALU.mult), reads=[modv, gv[gk]], writes=[gp])

    b.push()
    xin = b.sb("xin", [128, D])
    for (t0, n, ctx) in TILES:
        ti = t0 // 512
        for s in range((n + 127) // 128):
            m = min(128, n - s * 128)
            src = xp[t0 + s * 128:t0 + s * 128 + m, :] if ctx == 0 else xs[0:m, :]
            b.dma("sp", xin[0:m, :], src, writes=[xin])
            for kc in range(NKC):
                ps = pst[kc % 4]
                b.op("pe", lambda e: e.transpose(ps[:, 0:m], xin[0:m, kc * 128:(kc + 1) * 128], idf[0:m, 0:m]), reads=[xin, idf], writes=[ps])
                b.op("act" if kc % 2 else "dve", lambda e: (e.copy if kc % 2 else e.tensor_copy)(out=xt[:, kc, s * 128:s * 128 + m], in_=ps[:, 0:m]), reads=[ps], writes=[xt])
        b.dma("sp", X[:, t0:t0 + n].rearrange("(k p) t -> p k t", p=128), xt[:, :, 0:n], reads=[xt], writes=[X.rs[ti]])

    b.pop()
    def sumsq_rstd(src, n, nch, dim, eps, src_res):
        ps = pst[7]
        for kc in range(nch):
            b.op("act", lambda e: e.activation(out=t1[:, 0:n], in_=src[:, kc, 0:n], func=AF.Square), reads=[src_res], writes=[t1])
            b.op("pe", lambda e: e.matmul(ps[:, 0:n], lhsT=onesf[:], rhs=t1[:, 0:n], start=(kc == 0), stop=(kc == nch - 1)), reads=[onesf, t1], writes=[ps])
        b.op("act", lambda e: e.activation(out=rstd[:, 0:n], in_=ps[:, 0:n], func=AF.Ln, bias=float(eps), scale=1.0 / dim), reads=[ps], writes=[rstd])
        b.op("act", lambda e: e.activation(out=rstd[:, 0:n], in_=rstd[:, 0:n], func=AF.Exp, scale=-0.5), reads=[rstd], writes=[rstd])

    def pre_norm(l, wi, ti, t0, n, ctx, shift_out=None):
        si = 0 if wi == 0 else 3
        b.dma("sp", xt[:, :, 0:n], X[:, t0:t0 + n].rearrange("(k p) t -> p k t", p=128), reads=[X.rs[ti]], writes=[xt])
        sumsq_rstd(xt, n, NKC, D, 1e-6, xt)
        for kc in range(NKC):
            b.op("dve", lambda e: e.tensor_tensor(out=hf[:, 1:n + 1], in0=xt[:, kc, 0:n], in1=rstd[:, 0:n], op=ALU.mult), reads=[xt, rstd], writes=[hf])
            if shift_out is not None:
                b.op("dve", lambda e: e.tensor_scalar(out=colf[:, kc, :], in0=hf[:, n:n + 1], scalar1=gs[:, l, wi, kc, ctx:ctx + 1], scalar2=modv[:, l, si * 16 + kc, ctx:ctx + 1], op0=ALU.mult, op1=ALU.add), reads=[hf, gs, modv], writes=[colf])
            b.op("dve", lambda e: e.tensor_scalar(out=hx[:, kc, 2:n + 2], in0=hf[:, 1:n + 1], scalar1=gs[:, l, wi, kc, ctx:ctx + 1], scalar2=modv[:, l, si * 16 + kc, ctx:ctx + 1], op0=ALU.mult, op1=ALU.add), reads=[hf, gs, modv], writes=[hx])
        if shift_out is not None:
            b.dma("sp", shift_out.rearrange("(k p) -> p k", p=128), colf[:, :, 0], reads=[colf], writes=[shift_out_res[0]])

    shift_out_res = [None]

    def post_norm_residual(l, wi, ti, t0, n, ctx):
        sumsq_rstd(S['yt'], n, NKC, D, 1e-6, S['yt'])
        b.dma("sp", xt[:, :, 0:n], X[:, t0:t0 + n].rearrange("(k p) t -> p k t", p=128), reads=[X.rs[ti]], writes=[xt])
        for kc in range(NKC):
            b.op("dve", lambda e: e.tensor_tensor(out=t2[:, 0:n], in0=S['yt'][:, kc, 0:n], in1=rstd[:, 0:n], op=ALU.mult), reads=[S['yt'], rstd], writes=[t2])
            b.op("dve", lambda e: e.scalar_tensor_tensor(out=xt[:, kc, 0:n], in0=t2[:, 0:n], scalar=gp[:, l, wi, kc, ctx:ctx + 1], in1=xt[:, kc, 0:n], op0=ALU.mult, op1=ALU.add), reads=[t2, gp, xt], writes=[xt])
        b.dma("sp", X[:, t0:t0 + n].rearrange("(k p) t -> p k t", p=128), xt[:, :, 0:n], reads=[xt], writes=[X.rs[ti]])

    def mm_group(wap, nk, ncols, rhs_fn, n, evac, rhs_res, krows=128, ps_base=0):
        wt_, wv = load_w(wap, nk, ncols, krows)
        for mc in range(ncols // 128):
            ps = pst[ps_base + (mc % 4)]
            for kc in range(nk):
                b.op("pe", lambda e: e.matmul(ps[:, 0:n], lhsT=wv[:, kc, mc * 128:(mc + 1) * 128], rhs=rhs_fn(kc), start=(kc == 0), stop=(kc == nk - 1)), reads=[wt_] + list(rhs_res), writes=[ps])
            evac(mc, ps)

    def out_proj(wap, nk, rhs_fn, n, rhs_res):
        for gq in range(4):
            def ev(mc, ps, gq=gq):
                ch = gq * 4 + mc
                b.op("act" if ch % 2 else "dve", lambda e: (e.copy if ch % 2 else e.tensor_copy)(out=S['yt'][:, ch, 0:n], in_=ps[:, 0:n]), reads=[ps], writes=[S['yt']])
            mm_group(wap[:, gq * 512:(gq + 1) * 512], nk, 512, rhs_fn, n, ev, rhs_res)

    def ffn_layer(l):
        b.push()
        S['yt'] = b.sb("yt", [128, NKC, 512]); S['big'] = b.sb("big", [128, NFC, 512], BF16)
        gcar = b.sb("gcar", [128, NFC, 2]); gpad = b.sb("gpad", [128, 514]); gst = b.sb("gst", [128, NFC, 2])
        fcw = b.sb("fcw", [128, 3, NFC]); fcb = b.sb("fcb", [128, NFC])
        sg = b.sb("sg", [128, 4, 512])
        for j in range(3):
            fmvec(fcw[:, j, :], ffn_conv_w[l, j, :], writes=[fcw])
        fmvec(fcb[:, :], ffn_conv_b[l, :], writes=[fcb])
        for (t0, n, ctx) in TILES:
            ti = t0 // 512
            if t0 == 0:
                b.op("dve", lambda e: e.memset(gcar[:], 0.0), writes=[gcar])
            if ctx == 1:
                for j in range(2):
                    fmvec(gcar[:, :, j], st_ffn[l, j, :], writes=[gcar])
            pre_norm(l, 1, ti, t0, n, ctx)
            for gq in range(11):
                def ev_gate(mc, ps, gq=gq):
                    ch = gq * 4 + mc
                    b.op("act", lambda e: e.copy(out=gpad[:, 2:n + 2], in_=ps[:, 0:n]), reads=[ps], writes=[gpad])
                    b.op("dve", lambda e: e.tensor_copy(out=gpad[:, 0:2], in_=gcar[:, ch, :]), reads=[gcar], writes=[gpad])
                    b.op("dve", lambda e: e.tensor_copy(out=gcar[:, ch, :], in_=gpad[:, n:n + 2]), reads=[gpad], writes=[gcar])
                    if t0 + n == TP or ctx == 1:
                        b.op("dve", lambda e: e.tensor_copy(out=gst[:, ch, :], in_=gpad[:, n:n + 2]), reads=[gpad], writes=[gst])
                    b.op("dve", lambda e: e.tensor_scalar(out=t3[:, 0:n], in0=gpad[:, 0:n], scalar1=fcw[:, 0, ch:ch + 1], scalar2=fcb[:, ch:ch + 1], op0=ALU.mult, op1=ALU.add), reads=[gpad, fcw, fcb], writes=[t3])
                    b.op("dve", lambda e: e.scalar_tensor_tensor(out=t3[:, 0:n], in0=gpad[:, 1:n + 1], scalar=fcw[:, 1, ch:ch + 1], in1=t3[:, 0:n], op0=ALU.mult, op1=ALU.add), reads=[gpad, fcw, t3], writes=[t3])
                    b.op("dve", lambda e: e.scalar_tensor_tensor(out=t3[:, 0:n], in0=gpad[:, 2:n + 2], scalar=fcw[:, 2, ch:ch + 1], in1=t3[:, 0:n], op0=ALU.mult, op1=ALU.add), reads=[gpad, fcw, t3], writes=[t3])
                    b.op("act", lambda e: e.activation(out=sg[:, mc, 0:n], in_=t3[:, 0:n], func=AF.Silu), reads=[t3], writes=[sg])

                def ev_up(mc, ps, gq=gq):
                    ch = gq * 4 + mc
                    b.op("dve", lambda e: e.tensor_tensor(out=S['big'][:, ch, 0:n], in0=ps[:, 0:n], in1=sg[:, mc, 0:n], op=ALU.mult), reads=[ps, sg], writes=[S['big']])
                mm_group(ffn_w_gate[l, :, gq * 512:(gq + 1) * 512], NKC, 512, lambda kc: hx[:, kc, 2:n + 2], n, ev_gate, [hx])
                mm_group(ffn_w_up[l, :, gq * 512:(gq + 1) * 512], NKC, 512, lambda kc: hx[:, kc, 2:n + 2], n, ev_up, [hx], ps_base=4)
            if t0 + n == TP or ctx == 1:
                for j in range(2):
                    b.dma("sp", o_ffn[ctx][l, j, :].rearrange("(k p) -> p k", p=128), gst[:, :, j], reads=[gst], writes=[o_ffn[ctx]])
            for ch in range(NKC):
                def ev(mc, ps, ch=ch):
                    b.op("act" if ch % 2 else "dve", lambda e: (e.copy if ch % 2 else e.tensor_copy)(out=S['yt'][:, ch, 0:n], in_=ps[:, 0:n]), reads=[ps], writes=[S['yt']])
                mm_group(ffn_w_down[l, :, ch * 128:(ch + 1) * 128], NFC, 128, lambda kc: S['big'][:, kc, 0:n], n, ev, [S['big']], ps_base=ch % 4)
            post_norm_residual(l, 1, ti, t0, n, ctx)
        b.pop()


    evp = b.sb("evp", [128, 16, 8])
    murkv = b.sb("murkv", [128, 24]); muwag = b.sb("muwag", [128, 3, NKC])
    cw = b.sb("cw", [128, 31, 8])
    def even_A(l, e_):
        b.push()
        S['big'] = b.sb("bigA", [128, NKC, 512], BF16)
        lw1 = b.sb("lw1", [128, NKC, 448], BF16)
        lw2 = b.sb("lw2", [128, 4, 1024], BF16)
        pa = b.sb("pa", [128, 513]); pacar = b.sb("pacar", [128, 24, 1])
        kmix = b.sb("kmix", [128, 8, 512]); sigb = b.sb("sigb", [128, 8, 512])
        lmid = b.sb("lmid", [128, 4, 512], BF16)
        vtm = b.sb("vtm", [128, 4, 1024])
        for i, src in enumerate((a_w0, a_a0, a_k_k, a_k_a, a_r_k, a_ln_w, a_ln_b, b_conv_b, b_ln_w, b_ln_b)):
            fmvec(evp[:, i, :], src[e_, :], writes=[evp])
        fmvec(murkv[:, :], a_mu_rkv[e_, :], writes=[murkv])
        for i in range(3):
            fmvec(muwag[:, i, :], a_mu_wag[e_, i, :], writes=[muwag])
        for j in range(31):
            fmvec(cw[:, j, :], b_conv_w[e_, j, :], writes=[cw])
        b.dma("pool", lw1[:, :, 0:96], a_w1[e_].rearrange("(k p) c -> p k c", p=128), writes=[lw1])
        b.dma("pool", lw1[:, :, 96:192], a_a1[e_].rearrange("(k p) c -> p k c", p=128), writes=[lw1])
        b.dma("pool", lw1[:, :, 192:448], a_g1[e_].rearrange("(k p) c -> p k c", p=128), writes=[lw1])
        b.dma("pool", lw2[0:96, 0, :], a_w2[e_], writes=[lw2])
        b.dma("pool", lw2[0:96, 1, :], a_a2[e_], writes=[lw2])
        b.dma("pool", lw2[:, 2:4, :], a_g2[e_].rearrange("(k p) c -> p k c", p=128), writes=[lw2])
        for (t0, n, ctx) in TILES:
            ti = t0 // 512
            last = (t0 + n == TP) or ctx == 1
            shift_out_res[0] = o_shift[ctx]
            pre_norm(l, 0, ti, t0, n, ctx, shift_out=(o_shift[ctx][e_, :] if last else None))
            if t0 == 0:
                b.op("dve", lambda e: e.memset(carry_h[:], 0.0), writes=[carry_h])
                b.op("dve", lambda e: e.memset(pacar[:], 0.0), writes=[pacar])
            if ctx == 1:
                fmvec(colf[:, :, 0], st_shift[e_, :], writes=[colf])
                b.op("dve", lambda e: e.tensor_copy(out=carry_h[:], in_=colf[:]), reads=[colf], writes=[carry_h])
            b.op("dve", lambda e: e.tensor_copy(out=hx[:, :, 1:2], in_=carry_h[:]), reads=[carry_h], writes=[hx])
            b.op("dve", lambda e: e.tensor_copy(out=carry_h[:], in_=hx[:, :, n + 1:n + 2]), reads=[hx], writes=[carry_h])
            c0 = 0 if ctx == 1 else 1
            for gq in (0, 1, 2, 3, 4, 5, 8, 9, 6, 7):
                def ev(mc, ps, gq=gq):
                    ch = gq * 4 + mc
                    if ch < 24:
                        b.op("act", lambda e: e.copy(out=pa[:, c0:n + 1], in_=ps[:, 0:n + 1 - c0]), reads=[ps], writes=[pa])
                        if c0 == 1:
                            b.op("dve", lambda e: e.tensor_copy(out=pa[:, 0:1], in_=pacar[:, ch, :]), reads=[pacar], writes=[pa])
                        b.op("dve", lambda e: e.tensor_copy(out=pacar[:, ch, :], in_=pa[:, n:n + 1]), reads=[pa], writes=[pacar])
                        b.op("dve", lambda e: e.tensor_tensor(out=t2[:, 0:n], in0=pa[:, 0:n], in1=pa[:, 1:n + 1], op=ALU.subtract), reads=[pa], writes=[t2])
                        sec, cc_ = ch // 8, ch % 8
                        dst = kmix[:, cc_, 0:n] if sec == 1 else t3[:, 0:n]
                        b.op("dve", lambda e: e.scalar_tensor_tensor(out=dst, in0=t2[:, 0:n], scalar=murkv[:, ch:ch + 1], in1=pa[:, 1:n + 1], op0=ALU.mult, op1=ALU.add), reads=[t2, murkv, pa], writes=[kmix if sec == 1 else t3])
                        if sec == 0:
                            b.dma("sp", sR[cc_ * 128:(cc_ + 1) * 128, t0:t0 + n], t3[:, 0:n], reads=[t3], writes=[sR.rs[ti]])
                        if sec == 2:
                            b.dma("sp", sV[cc_ * 128:(cc_ + 1) * 128, t0:t0 + n], t3[:, 0:n], reads=[t3], writes=[sV.rs[ti]])
                            for s_ in range((n + 127) // 128):
                                m = min(128, n - s_ * 128)
                                pq = pst[4 + s_ % 2]
                                b.op("pe", lambda e: e.transpose(pq[0:m, 0:128], t3[:, s_ * 128:s_ * 128 + m], idf[:]), reads=[t3, idf], writes=[pq])
                                b.op("act", lambda e: e.copy(out=vtm[0:m, s_, cc_ * 128:(cc_ + 1) * 128], in_=pq[0:m, 0:128]), reads=[pq], writes=[vtm])
                                if cc_ == 7:
                                    b.dma("sp", sVtm[t0 + s_ * 128:t0 + s_ * 128 + m, :], vtm[0:m, s_, :], reads=[vtm], writes=[sVtm.rs[ti]])
                    elif ch >= 32:
                        b.op("act", lambda e: e.activation(out=sigb[:, ch - 32, 0:n], in_=ps[:, c0 ^ 1:n + (c0 ^ 1)], func=AF.Sigmoid), reads=[ps], writes=[sigb])
                    else:
                        cc_ = ch - 24
                        b.op("dve", lambda e: e.tensor_tensor(out=t3[:, 0:n], in0=ps[:, c0 ^ 1:n + (c0 ^ 1)], in1=sigb[:, cc_, 0:n], op=ALU.mult), reads=[ps, sigb], writes=[t3])
                        b.dma("sp", sU[cc_ * 128:(cc_ + 1) * 128, t0:t0 + n], t3[:, 0:n], reads=[t3], writes=[sU.rs[ti]])
                mm_group(ab_w_in[e_, :, gq * 512:(gq + 1) * 512], NKC, 512, lambda kc: hx[:, kc, c0 + 1:n + 2], n + 1 - c0, ev, [hx])
            for i in range(3):
                for kc in range(NKC):
                    b.op("dve", lambda e: e.tensor_tensor(out=t2[:, 0:n], in0=hx[:, kc, 1:n + 1], in1=hx[:, kc, 2:n + 2], op=ALU.subtract), reads=[hx], writes=[t2])
                    b.op("dve", lambda e: e.scalar_tensor_tensor(out=S['big'][:, kc, 0:n], in0=t2[:, 0:n], scalar=muwag[:, i, kc:kc + 1], in1=hx[:, kc, 2:n + 2], op0=ALU.mult, op1=ALU.add), reads=[t2, muwag, hx], writes=[S['big']])
                if i < 2:
                    ps = pst[4 + i]
                    for kc in range(NKC):
                        b.op("pe", lambda e: e.matmul(ps[0:96, 0:n], lhsT=lw1[:, kc, i * 96:(i + 1) * 96], rhs=S['big'][:, kc, 0:n], start=(kc == 0), stop=(kc == NKC - 1)), reads=[lw1, S['big']], writes=[ps])
                    if i == 0:
                        b.op("act", lambda e: e.activation(out=lmid[0:96, 0, 0:n], in_=ps[0:96, 0:n], func=AF.Tanh), reads=[ps], writes=[lmid])
                    else:
                        b.op("act", lambda e: e.copy(out=lmid[0:96, 1, 0:n], in_=ps[0:96, 0:n]), reads=[ps], writes=[lmid])
                else:
                    for hh in range(2):
                        ps = pst[6 + hh]
                        for kc in range(NKC):
                            b.op("pe", lambda e: e.matmul(ps[:, 0:n], lhsT=lw1[:, kc, 192 + hh * 128:192 + (hh + 1) * 128], rhs=S['big'][:, kc, 0:n], start=(kc == 0), stop=(kc == NKC - 1)), reads=[lw1, S['big']], writes=[ps])
                        b.op("act", lambda e: e.activation(out=lmid[:, 2 + hh, 0:n], in_=ps[:, 0:n], func=AF.Sigmoid), reads=[ps], writes=[lmid])
            for cc_ in range(8):
                cs = slice(cc_ * 128, (cc_ + 1) * 128)
                ps = pst[cc_ % 4]
                b.op("pe", lambda e: e.matmul(ps[:, 0:n], lhsT=lw2[0:96, 0, cs], rhs=lmid[0:96, 0, 0:n], start=True, stop=True), reads=[lw2, lmid], writes=[ps])
                b.op("act", lambda e: e.activation(out=t2[:, 0:n], in_=ps[:, 0:n], func=AF.Sigmoid, bias=evp[:, 0, cc_:cc_ + 1], scale=1.0), reads=[ps, evp], writes=[t2])
                b.op("act", lambda e: e.activation(out=t2[:, 0:n], in_=t2[:, 0:n], func=AF.Exp, scale=-EXPC), reads=[t2], writes=[t2])
                b.dma("sp", sW[cs, t0:t0 + n], t2[:, 0:n], reads=[t2], writes=[sW.rs[ti]])
                ps = pst[4 + cc_ % 2]
                b.op("pe", lambda e: e.matmul(ps[:, 0:n], lhsT=lw2[0:96, 1, cs], rhs=lmid[0:96, 1, 0:n], start=True, stop=True), reads=[lw2, lmid], writes=[ps])
                b.op("act", lambda e: e.activation(out=t4[:, 0:n], in_=ps[:, 0:n], func=AF.Sigmoid, bias=evp[:, 1, cc_:cc_ + 1], scale=1.0), reads=[ps, evp], writes=[t4])
                ps = pst[6 + cc_ % 2]
                for hh in range(2):
                    b.op("pe", lambda e: e.matmul(ps[:, 0:n], lhsT=lw2[:, 2 + hh, cs], rhs=lmid[:, 2 + hh, 0:n], start=(hh == 0), stop=(hh == 1)), reads=[lw2, lmid], writes=[ps])
                b.op("act", lambda e: e.copy(out=t3[:, 0:n], in_=ps[:, 0:n]), reads=[ps], writes=[t3])
                b.dma("sp", sG[cs, t0:t0 + n], t3[:, 0:n], reads=[t3], writes=[sG.rs[ti]])
                b.op("dve", lambda e: e.tensor_scalar(out=t1[:, 0:n], in0=kmix[:, cc_, 0:n], scalar1=evp[:, 2, cc_:cc_ + 1], scalar2=None, op0=ALU.mult), reads=[kmix, evp], writes=[t1])
                b.op("dve", lambda e: e.tensor_tensor(out=t2[:, 0:n], in0=t1[:, 0:n], in1=t1[:, 0:n], op=ALU.mult), reads=[t1], writes=[t2])
                ps = pst[cc_ % 4]
                b.op("pe", lambda e: e.matmul(ps[:, 0:n], lhsT=blk1[:], rhs=t2[:, 0:n], start=True, stop=True), reads=[blk1, t2], writes=[ps])
                b.op("dve", lambda e: e.tensor_scalar(out=t2[:, 0:n], in0=ps[:, 0:n], scalar1=1e-24, scalar2=None, op0=ALU.max), reads=[ps], writes=[t2])
                b.op("act", lambda e: e.activation(out=t2[:, 0:n], in_=t2[:, 0:n], func=AF.Ln), reads=[t2], writes=[t2])
                b.op("act", lambda e: e.activation(out=t2[:, 0:n], in_=t2[:, 0:n], func=AF.Exp, scale=-0.5), reads=[t2], writes=[t2])
                b.op("dve", lambda e: e.tensor_tensor(out=t1[:, 0:n], in0=t1[:, 0:n], in1=t2[:, 0:n], op=ALU.mult), reads=[t1, t2], writes=[t1])
                b.op("dve", lambda e: e.tensor_tensor(out=t2[:, 0:n], in0=t1[:, 0:n], in1=t4[:, 0:n], op=ALU.mult), reads=[t1, t4], writes=[t2])
                b.dma("sp", sBb[cs, t0:t0 + n], t2[:, 0:n], reads=[t2], writes=[sBb.rs[ti]])
                b.op("act", lambda e: e.mul(out=t3[:, 0:n], in_=t1[:, 0:n], mul=-1.0), reads=[t1], writes=[t3])
                b.dma("sp", sA[cs, t0:t0 + n], t3[:, 0:n], reads=[t3], writes=[sA.rs[ti]])
                b.op("dve", lambda e: e.tensor_scalar(out=t4[:, 0:n], in0=t4[:, 0:n], scalar1=-1.0, scalar2=evp[:, 3, cc_:cc_ + 1], op0=ALU.add, op1=ALU.mult), reads=[t4, evp], writes=[t4])
                b.op("dve", lambda e: e.scalar_tensor_tensor(out=t1[:, 0:n], in0=t4[:, 0:n], scalar=1.0, in1=kmix[:, cc_, 0:n], op0=ALU.add, op1=ALU.mult), reads=[t4, kmix], writes=[t1])
                b.dma("sp", sK2[cs, t0:t0 + n], t1[:, 0:n], reads=[t1], writes=[sK2.rs[ti]])
        b.pop()

    TBM = 32
    VB = 8

    def scan(e_, ctx):
        if ctx == 0 and not DO_PROMPT[0]:
            return
        b.push()
        stA = b.sb("stA", [64, 16, 64])
        Ssb = b.sb("Ssb", [128, 8, 64]); stmp = b.sb("stmp", [128, 8, 64]); stm2 = b.sb("stm2", [128, 8, 64])
        blkv = {k: b.sb("blk_" + k, [128, 8, TBM]) for k in ("r", "w", "k", "a", "b")}
        R2 = b.sb("R2", [128, 8, TBM, 2]); vrow = b.sb("vrow", [2, VB, 512]); osb = b.sb("osb", [64, 8, 2, TBM])
        T0, T_, TB = (0, TP, TBM) if ctx == 0 else (TP, TS, TS)
        if ctx == 0:
            b.op("dve", lambda e: e.memset(Ssb[:], 0.0), writes=[Ssb])
        else:
            b.dma("sp", stA[:], st_wkv[e_].rearrange("h v k -> v h k"), writes=[stA])
            for c_ in range(8):
                b.op("pe", lambda e: e.transpose(pst[6][:, 0:64], stA[:, 2 * c_:2 * c_ + 2, :].rearrange("v j k -> v (j k)"), idf[0:64, 0:64]), reads=[stA, idf], writes=[pst[6]])
                b.op("dve", lambda e: e.tensor_copy(out=Ssb[:, c_, :], in_=pst[6][:, 0:64]), reads=[pst[6]], writes=[Ssb])
        srcs = {"r": sR, "w": sW, "k": sK2, "a": sA, "b": sBb}
        for bi in range(T_ // TB):
            t0 = T0 + bi * TB
            ti = t0 // 512
            for k_, s_ in srcs.items():
                b.dma("sp", blkv[k_][:, :, 0:TB], s_[:, t0:t0 + TB].rearrange("(c p) t -> p c t", p=128), reads=[s_.rs[ti]], writes=[blkv[k_]])
            for j in range(2):
                b.op("pool", lambda e: e.tensor_scalar(out=R2[:, :, 0:TB, j], in0=blkv["r"][:, :, 0:TB], scalar1=maskj[:, j:j + 1], scalar2=None, op0=ALU.mult), reads=[blkv["r"], maskj], writes=[R2])
            ops_ = pst[2]
            opv = ops_[0:64, 0:8 * TB * 2].rearrange("p (c t j) -> p c t j", c=8, j=2)
            for t in range(TB):
                if t % VB == 0:
                    nv = min(VB, TB - t)
                    for j in range(2):
                        b.dma("sp", vrow[j:j + 1, 0:nv, :].rearrange("o t (c v) -> o t c v", v=64), sVtm[t0 + t:t0 + t + nv, :].rearrange("t (c j v) -> j t c v", j=2, v=64)[j:j + 1], reads=[sVtm.rs[ti]], writes=[vrow])
                bc = lambda k_: blkv[k_][:, :, t:t + 1].to_broadcast([128, 8, 64])
                b.op("dve", lambda e: e.tensor_tensor(out=stmp[:], in0=Ssb[:], in1=bc("a"), op=ALU.mult), reads=[Ssb, blkv["a"]], writes=[stmp])
                b.op("pe", lambda e: e.matmul(pst[0][:, 0:512], lhsT=blk1[:], rhs=stmp[:].rearrange("p c v -> p (c v)"), start=True, stop=True), reads=[blk1, stmp], writes=[pst[0]])
                pv = pst[4 + t % 2]
                b.op("pe", lambda e: e.matmul(pv[:, 0:512], lhsT=sel[:], rhs=vrow[:, t % VB, :], start=True, stop=True), reads=[sel, vrow], writes=[pv])
                b.op("dve", lambda e: e.tensor_tensor(out=Ssb[:], in0=Ssb[:], in1=bc("w"), op=ALU.mult), reads=[Ssb, blkv["w"]], writes=[Ssb])
                b.op("dve", lambda e: e.tensor_tensor(out=stm2[:], in0=pv[:, 0:512].rearrange("p (c v) -> p c v", v=64), in1=bc("k"), op=ALU.mult), reads=[pv, blkv["k"]], writes=[stm2])
                b.op("dve", lambda e: e.tensor_tensor(out=Ssb[:], in0=Ssb[:], in1=stm2[:], op=ALU.add), reads=[Ssb, stm2], writes=[Ssb])
                b.op("dve", lambda e: e.tensor_tensor(out=stm2[:], in0=pst[0][:, 0:512].rearrange("p (c v) -> p c v", v=64), in1=bc("b"), op=ALU.mult), reads=[pst[0], blkv["b"]], writes=[stm2])
                b.op("dve", lambda e: e.tensor_tensor(out=Ssb[:], in0=Ssb[:], in1=stm2[:], op=ALU.add), reads=[Ssb, stm2], writes=[Ssb])
                for c_ in range(8):
                    b.op("pe", lambda e: e.matmul(opv[:, c_, t, :], lhsT=Ssb[:, c_, :], rhs=R2[:, c_, t, :], start=True, stop=True), reads=[Ssb, R2], writes=[ops_])
            for j in range(2):
                b.op("act", lambda e: e.copy(out=osb[:, :, j, 0:TB], in_=opv[:, :, 0:TB, j]), reads=[ops_], writes=[osb])
            for j in range(2):
                b.dma("sp", sO[:, t0:t0 + TB].rearrange("(c j v) t -> j v c t", j=2, v=64)[j], osb[:, :, j, 0:TB], reads=[osb], writes=[sO.rs[ti]])
        for c_ in range(8):
            b.op("pe", lambda e: e.transpose(pst[6][0:64, 0:128], Ssb[:, c_, :], idf[:]), reads=[Ssb, idf], writes=[pst[6]])
            b.op("dve", lambda e: e.tensor_copy(out=stA[:, 2 * c_:2 * c_ + 2, :].rearrange("v j k -> v (j k)"), in_=pst[6][0:64, 0:128]), reads=[pst[6]], writes=[stA])
        b.dma("sp", o_wkv[ctx][e_].rearrange("h v k -> v h k"), stA[:], reads=[stA], writes=[o_wkv[ctx]])
        b.pop()

    def even_C(l, e_):
        b.push()
        S['yt'] = b.sb("ytC", [128, NKC, 512]); S['big'] = b.sb("bigC", [128, NKC, 512], BF16)
        upad = b.sb("upad", [128, 8, 542]); ucar = b.sb("ucar", [128, 8, 30]); ust = b.sb("ust", [128, 8, 30])
        ubt = b.sb("ubt", [128, 8, 512])
        for (t0, n, ctx) in TILES:
            ti = t0 // 512
            last = (t0 + n == TP) or ctx == 1
            if t0 == 0:
                b.op("dve", lambda e: e.memset(ucar[:], 0.0), writes=[ucar])
            if ctx == 1:
                for c_ in range(8):
                    b.dma("sp", ucar[:, c_, :], st_convb[e_, :, c_ * 128:(c_ + 1) * 128].rearrange("j p -> p j"), writes=[ucar])
            b.op("dve", lambda e: e.tensor_copy(out=upad[:, :, 0:30], in_=ucar[:]), reads=[ucar], writes=[upad])
            b.dma("sp", upad[:, :, 30:30 + n], sU[:, t0:t0 + n].rearrange("(c p) t -> p c t", p=128), reads=[sU.rs[ti]], writes=[upad])
            b.op("dve", lambda e: e.tensor_copy(out=ucar[:], in_=upad[:, :, n:n + 30]), reads=[upad], writes=[ucar])
            if last:
                b.op("dve", lambda e: e.tensor_copy(out=ust[:], in_=upad[:, :, n:n + 30]), reads=[upad], writes=[ust])
                for c_ in range(8):
                    b.dma("sp", o_convb[ctx][e_, :, c_ * 128:(c_ + 1) * 128].rearrange("j p -> p j"), ust[:, c_, :], reads=[ust], writes=[o_convb[ctx]])
            for cc_ in range(8):
                cs = slice(cc_ * 128, (cc_ + 1) * 128)
                b.dma("sp", t1[:, 0:n], sO[cs, t0:t0 + n], reads=[sO.rs[ti]], writes=[t1])
                ps = pst[0]
                b.op("pe", lambda e: e.matmul(ps[:, 0:n], lhsT=blk1[:], rhs=t1[:, 0:n], start=True, stop=True), reads=[blk1, t1], writes=[ps])
                b.op("dve", lambda e: e.scalar_tensor_tensor(out=t1[:, 0:n], in0=ps[:, 0:n], scalar=-1.0 / 64, in1=t1[:, 0:n], op0=ALU.mult, op1=ALU.add), reads=[ps, t1], writes=[t1])
                b.op("dve", lambda e: e.tensor_tensor(out=t2[:, 0:n], in0=t1[:, 0:n], in1=t1[:, 0:n], op=ALU.mult), reads=[t1], writes=[t2])
                ps = pst[1]
                b.op("pe", lambda e: e.matmul(ps[:, 0:n], lhsT=blk1[:], rhs=t2[:, 0:n], start=True, stop=True), reads=[blk1, t2], writes=[ps])
                b.op("act", lambda e: e.activation(out=t2[:, 0:n], in_=ps[:, 0:n], func=AF.Ln, bias=64e-5, scale=1.0 / 64), reads=[ps], writes=[t2])
                b.op("act", lambda e: e.activation(out=t2[:, 0:n], in_=t2[:, 0:n], func=AF.Exp, scale=-0.5), reads=[t2], writes=[t2])
                b.op("dve", lambda e: e.tensor_tensor(out=t1[:, 0:n], in0=t1[:, 0:n], in1=t2[:, 0:n], op=ALU.mult), reads=[t1, t2], writes=[t1])
                b.op("dve", lambda e: e.tensor_scalar(out=t1[:, 0:n], in0=t1[:, 0:n], scalar1=evp[:, 5, cc_:cc_ + 1], scalar2=evp[:, 6, cc_:cc_ + 1], op0=ALU.mult, op1=ALU.add), reads=[t1, evp], writes=[t1])
                b.dma("sp", t2[:, 0:n], sR[cs, t0:t0 + n], reads=[sR.rs[ti]], writes=[t2])
                b.dma("sp", t3[:, 0:n], sK2[cs, t0:t0 + n], reads=[sK2.rs[ti]], writes=[t3])
                b.op("dve", lambda e: e.scalar_tensor_tensor(out=t2[:, 0:n], in0=t2[:, 0:n], scalar=evp[:, 4, cc_:cc_ + 1], in1=t3[:, 0:n], op0=ALU.mult, op1=ALU.mult), reads=[t2, t3, evp], writes=[t2])
                ps = pst[2]
                b.op("pe", lambda e: e.matmul(ps[:, 0:n], lhsT=blk1[:], rhs=t2[:, 0:n], start=True, stop=True), reads=[blk1, t2], writes=[ps])
                b.dma("sp", t3[:, 0:n], sV[cs, t0:t0 + n], reads=[sV.rs[ti]], writes=[t3])
                b.op("dve", lambda e: e.tensor_tensor(out=t3[:, 0:n], in0=t3[:, 0:n], in1=ps[:, 0:n], op=ALU.mult), reads=[t3, ps], writes=[t3])
                b.op("dve", lambda e: e.tensor_tensor(out=t1[:, 0:n], in0=t1[:, 0:n], in1=t3[:, 0:n], op=ALU.add), reads=[t1, t3], writes=[t1])
                b.dma("sp", t4[:, 0:n], sG[cs, t0:t0 + n], reads=[sG.rs[ti]], writes=[t4])
                b.op("dve", lambda e: e.tensor_tensor(out=S['big'][:, cc_, 0:n], in0=t1[:, 0:n], in1=t4[:, 0:n], op=ALU.mult), reads=[t1, t4], writes=[S['big']])
                b.op("pool", lambda e: e.tensor_scalar(out=ubt[:, cc_, 0:n], in0=upad[:, cc_, 0:n], scalar1=cw[:, 0, cc_:cc_ + 1], scalar2=evp[:, 7, cc_:cc_ + 1], op0=ALU.mult, op1=ALU.add), reads=[upad, cw, evp], writes=[ubt])
                for j in range(1, 31):
                    b.op("dve", lambda e: e.scalar_tensor_tensor(out=ubt[:, cc_, 0:n], in0=upad[:, cc_, j:j + n], scalar=cw[:, j, cc_:cc_ + 1], in1=ubt[:, cc_, 0:n], op0=ALU.mult, op1=ALU.add), reads=[upad, cw, ubt], writes=[ubt])
            ps = pst[3]
            for cc_ in range(8):
                b.op("pe", lambda e: e.matmul(ps[:, 0:n], lhsT=onesf[:], rhs=ubt[:, cc_, 0:n], start=(cc_ == 0), stop=(cc_ == 7)), reads=[onesf, ubt], writes=[ps])
            b.op("act", lambda e: e.mul(out=t4[:, 0:n], in_=ps[:, 0:n], mul=-1.0 / 1024), reads=[ps], writes=[t4])
            for cc_ in range(8):
                b.op("dve", lambda e: e.tensor_tensor(out=ubt[:, cc_, 0:n], in0=ubt[:, cc_, 0:n], in1=t4[:, 0:n], op=ALU.add), reads=[ubt, t4], writes=[ubt])
            sumsq_rstd(ubt, n, 8, 1024, 1e-5, ubt)
            for cc_ in range(8):
                b.op("dve", lambda e: e.tensor_tensor(out=t2[:, 0:n], in0=ubt[:, cc_, 0:n], in1=rstd[:, 0:n], op=ALU.mult), reads=[ubt, rstd], writes=[t2])
                b.op("act", lambda e: e.activation(out=S['big'][:, 8 + cc_, 0:n], in_=t2[:, 0:n], func=AF.Silu, bias=evp[:, 9, cc_:cc_ + 1], scale=evp[:, 8, cc_:cc_ + 1]), reads=[t2, evp], writes=[S['big']])
            out_proj(ab_w_out[e_], NKC, lambda kc: S['big'][:, kc, 0:n], n, [S['big']])
            post_norm_residual(l, 0, ti, t0, n, ctx)
        b.pop()

    b.even = (even_A, scan, even_C)

    def odd_E(l, o_):
        b.push()
        qst = b.sb("qst", [128, 4, 512], BF16); ktm = b.sb("ktm", [128, 512]); vtb = b.sb("vtb", [128, 512], BF16)
        for (t0, n, ctx) in TILES:
            ti = t0 // 512
            pre_norm(l, 0, ti, t0, n, ctx)
            for gq in range(12):
                def ev(mc, ps, gq=gq):
                    b.op("act" if mc % 2 else "dve", lambda e: (e.copy if mc % 2 else e.tensor_copy)(out=qst[:, mc, 0:n], in_=ps[:, 0:n]), reads=[ps], writes=[qst])
                mm_group(attn_w_qkv[o_, :, gq * 512:(gq + 1) * 512], NKC, 512, lambda kc: hx[:, kc, 2:n + 2], n, ev, [hx])
                dstT = sQT if gq < 6 else sKT
                r0 = (gq % 6) * 512
                b.dma("sp", dstT[r0:r0 + 512, t0:t0 + n].rearrange("(c p) t -> p c t", p=128), qst[:, :, 0:n], reads=[qst], writes=[dstT.rs[ti]])
            for gi in (range(12) if int(os.environ.get('ODD_E_PART', '9')) >= 2 else ()):
                wt_, wv = load_w(attn_w_qkv[o_, :, 3072 + gi * 512:3072 + (gi + 1) * 512], NKC, 512)
                kv = gi // 6
                g = (gi % 6) // 2
                hh = gi % 2
                for s_ in range((n + 127) // 128):
                    m = min(128, n - s_ * 128)
                    ps = pst[s_ % 4]
                    for kc in range(NKC):
                        b.op("pe", lambda e: e.matmul(ps[0:m, 0:512], lhsT=hx[:, kc, 2 + s_ * 128:2 + s_ * 128 + m], rhs=wv[:, kc, :], start=(kc == 0), stop=(kc == NKC - 1)), reads=[wt_, hx], writes=[ps])
                    tok = t0 + s_ * 128
                    keep0 = TP - WINS[g] if ctx == 0 else TP
                    if tok >= keep0 and int(os.environ.get('ODD_E_PART', '9')) >= 3:
                        b.op("act", lambda e: e.copy(out=ktm[0:m, :], in_=ps[0:m, 0:512]), reads=[ps], writes=[ktm])
                        b.dma("sp", o_kv[ctx][g][o_, kv, tok - keep0:tok - keep0 + m, hh * 512:(hh + 1) * 512], ktm[0:m, :], reads=[ktm], writes=[o_kv[ctx][g]])
                    if kv == 1:
                        b.op("dve", lambda e: e.tensor_copy(out=vtb[0:m, :], in_=ps[0:m, 0:512]), reads=[ps], writes=[vtb])
                        b.dma("sp", sVa[tok:tok + m, (gi - 6) * 512:(gi - 5) * 512], vtb[0:m, :], reads=[vtb], writes=[sVa.rs[ti]])
        b.pop()

    def odd_F(l, o_):
        b.push()
        numacc = b.sb("numacc", [128, TP]); denacc = b.sb("denacc", [128, TP])
        qT = b.sb("qT", [128, TP], BF16); kT = b.sb("kT", [128, TP], BF16)
        qd = b.sb("qd", [128, TP], BF16); kd = b.sb("kd", [128, TP], BF16)
        vt = b.sb("vt", [128, 32, 128], BF16); ot = b.sb("ot", [128, TP], BF16)
        pT = [b.sb(f"pT{i}", [128, 256], BF16) for i in range(2)]
        allres = lambda T_: T_.rs[0:8]
        for h in (range(8) if DO_PROMPT[0] else ()):
            for g in range(3):
                d = DILS[g]
                nb = TP // d // 128
                ch = g * 8 + h
                b.dma("sp", qT[:], sQT[ch * 128:(ch + 1) * 128, 0:TP], reads=allres(sQT), writes=[qT])
                b.dma("sp", kT[:], sKT[ch * 128:(ch + 1) * 128, 0:TP], reads=allres(sKT), writes=[kT])
                if d == 1:
                    qq, kk_ = qT, kT
                else:
                    b.op("pool", lambda e: e.tensor_copy(out=qd[:].rearrange("p (dd i) -> p dd i", dd=d), in_=qT[:].rearrange("p (i dd) -> p dd i", dd=d)), reads=[qT], writes=[qd])
                    b.op("dve", lambda e: e.tensor_copy(out=kd[:].rearrange("p (dd i) -> p dd i", dd=d), in_=kT[:].rearrange("p (i dd) -> p dd i", dd=d)), reads=[kT], writes=[kd])
                    qq, kk_ = qd, kd
                vsrc = sVa[0:TP, ch * 128:(ch + 1) * 128].rearrange("(jt m dd) c -> dd m jt c", m=128, dd=d)
                for rho in range(d):
                    b.dma("sp", vt[:, rho * nb:(rho + 1) * nb, :], vsrc[rho], reads=allres(sVa), writes=[vt])
                for rho in range(d):
                    for blk in range(nb):
                        bi = rho * nb + blk
                        c0 = bi * 128
                        ps_s, ps_n, ps_d, pt = pst[bi % 2], pst[2 + bi % 2], pst[4 + bi % 2], pT[bi % 2]
                        b.op("pe", lambda e: e.matmul(ps_s[:, 0:128], lhsT=kk_[:, c0:c0 + 128], rhs=qq[:, c0:c0 + 128], start=True, stop=True), reads=[kk_, qq], writes=[ps_s])
                        ncol = 128
                        if blk > 0:
                            b.op("pe", lambda e: e.matmul(ps_s[:, 128:256], lhsT=kk_[:, c0 - 128:c0], rhs=qq[:, c0:c0 + 128], start=True, stop=True), reads=[kk_, qq], writes=[ps_s])
                            ncol = 256
                        b.op("act", lambda e: e.activation(out=pt[:, 0:ncol], in_=ps_s[:, 0:ncol], func=AF.Exp, scale=C_SCALE), reads=[ps_s], writes=[pt])
                        b.op("pool", lambda e: e.tensor_tensor(out=pt[:, 0:ncol], in0=pt[:, 0:ncol], in1=mask2[:, 0:ncol], op=ALU.mult), reads=[pt, mask2], writes=[pt])
                        b.op("pe", lambda e: e.matmul(ps_n[:, 0:128], lhsT=vt[:, bi, :], rhs=pt[:, 0:128], start=True, stop=(blk == 0)), reads=[vt, pt], writes=[ps_n])
                        if blk > 0:
                            b.op("pe", lambda e: e.matmul(ps_n[:, 0:128], lhsT=vt[:, bi - 1, :], rhs=pt[:, 128:256], start=False, stop=True), reads=[vt, pt], writes=[ps_n])
                        b.op("pe", lambda e: e.matmul(ps_d[:, 0:128], lhsT=onesb[:], rhs=pt[:, 0:128], start=True, stop=(blk == 0)), reads=[onesb, pt], writes=[ps_d])
                        if blk > 0:
                            b.op("pe", lambda e: e.matmul(ps_d[:, 0:128], lhsT=onesb[:], rhs=pt[:, 128:256], start=False, stop=True), reads=[onesb, pt], writes=[ps_d])
                        st = rho + d * blk * 128
                        sl = slice(st, st + d * 127 + 1, d)
                        if g == 0:
                            b.op("act", lambda e: e.copy(out=numacc[:, sl], in_=ps_n[:, 0:128]), reads=[ps_n], writes=[numacc])
                            b.op("act", lambda e: e.copy(out=denacc[:, sl], in_=ps_d[:, 0:128]), reads=[ps_d], writes=[denacc])
                        else:
                            b.op("dve", lambda e: e.tensor_tensor(out=numacc[:, sl], in0=numacc[:, sl], in1=ps_n[:, 0:128], op=ALU.add), reads=[ps_n, numacc], writes=[numacc])
                            b.op("dve", lambda e: e.tensor_tensor(out=denacc[:, sl], in0=denacc[:, sl], in1=ps_d[:, 0:128], op=ALU.add), reads=[ps_d, denacc], writes=[denacc])
            b.op("dve", lambda e: e.reciprocal(out=denacc[:], in_=denacc[:]), reads=[denacc], writes=[denacc])
            b.op("dve", lambda e: e.tensor_tensor(out=ot[:], in0=numacc[:], in1=denacc[:], op=ALU.mult), reads=[numacc, denacc], writes=[ot])
            b.dma("sp", sOT[h * 128:(h + 1) * 128, 0:TP], ot[:], reads=[ot], writes=allres(sOT))
        b.pop()
        b.push()
        kcf = b.sb("kcf", [128, 1024]); vcf = b.sb("vcf", [128, 1024]); vcb = b.sb("vcb", [128, 1024], BF16)
        kTs = b.sb("kTs", [128, 128], BF16); pS = b.sb("pS", [128, 4], BF16)
        qs4 = b.sb("qs4", [128, 24, 4], BF16); kn4 = b.sb("kn4", [128, 24, 4], BF16); vn4 = b.sb("vn4", [4, 3072], BF16)
        nums = b.sb("nums", [128, 8, 4]); dens = b.sb("dens", [128, 8, 4]); os4 = b.sb("os4", [128, 8, 4], BF16)
        b.dma("sp", qs4[:], sQT[:, TP:NT].rearrange("(c p) t -> p c t", p=128), reads=[sQT.rs[8]], writes=[qs4])
        b.dma("sp", kn4[:], sKT[:, TP:NT].rearrange("(c p) t -> p c t", p=128), reads=[sKT.rs[8]], writes=[kn4])
        b.dma("sp", vn4[:], sVa[TP:NT, :], reads=[sVa.rs[8]], writes=[vn4])
        colsel = b.sb("colsel", [128, 4, 4], BF16)
        b.op("dve", lambda e: e.memset(colsel[:], 0.0), writes=[colsel])
        for tt_ in range(4):
            b.op("dve", lambda e: e.memset(colsel[:, tt_, tt_:tt_ + 1], 1.0), reads=[colsel], writes=[colsel])
        b.op("dve", lambda e: e.memset(nums[:], 0.0), writes=[nums])
        b.op("dve", lambda e: e.memset(dens[:], 0.0), writes=[dens])

        def accum(h, cols, lhs_v, lhs_one, p_ap, krows):
            b.op("pe", lambda e: e.matmul(pst[2][:, 0:len(cols)], lhsT=lhs_v, rhs=p_ap, start=True, stop=True), reads=[vcb, vn4, pS], writes=[pst[2]])
            b.op("pe", lambda e: e.matmul(pst[3][:, 0:len(cols)], lhsT=lhs_one, rhs=p_ap, start=True, stop=True), reads=[onesb, pS], writes=[pst[3]])
            c0, c1 = cols[0], cols[-1] + 1
            b.op("dve", lambda e: e.tensor_tensor(out=nums[:, h, c0:c1], in0=nums[:, h, c0:c1], in1=pst[2][:, 0:len(cols)], op=ALU.add), reads=[nums, pst[2]], writes=[nums])
            b.op("dve", lambda e: e.tensor_tensor(out=dens[:, h, c0:c1], in0=dens[:, h, c0:c1], in1=pst[3][:, 0:len(cols)], op=ALU.add), reads=[dens, pst[3]], writes=[dens])

        for g in range(3):
            d = DILS[g]
            ck = cks[g]
            tiles_ = [(None, [0, 1, 2, 3])] if d == 1 else [(tt, [tt]) for tt in range(4)]
            for tt, cols in tiles_:
                ksrc = ck[o_, 0, :, :] if d == 1 else ck[o_, 0, :, :].rearrange("(i dd) c -> dd i c", dd=d)[tt]
                vsrc = ck[o_, 1, :, :] if d == 1 else ck[o_, 1, :, :].rearrange("(i dd) c -> dd i c", dd=d)[tt]
                b.dma("sp", kcf[:], ksrc, writes=[kcf])
                b.dma("sp", vcf[:], vsrc, writes=[vcf])
                b.op("pool", lambda e: e.tensor_copy(out=vcb[:], in_=vcf[:]), reads=[vcf], writes=[vcb])
                for h in range(8):
                    ch = g * 8 + h
                    b.op("pe", lambda e: e.transpose(pst[0][:, 0:128], kcf[:, h * 128:(h + 1) * 128], idf[:]), reads=[kcf, idf], writes=[pst[0]])
                    b.op("act", lambda e: e.copy(out=kTs[:], in_=pst[0][:, 0:128]), reads=[pst[0]], writes=[kTs])
                    b.op("pe", lambda e: e.matmul(pst[1][:, 0:4], lhsT=kTs[:], rhs=qs4[:, ch, :], start=True, stop=True), reads=[kTs, qs4], writes=[pst[1]])
                    b.op("act", lambda e: e.activation(out=pS[:, 0:4], in_=pst[1][:, 0:4], func=AF.Exp, scale=C_SCALE), reads=[pst[1]], writes=[pS])
                    msk_ = mask2[:, 128:132] if d == 1 else colsel[:, tt, :]
                    b.op("dve", lambda e: e.tensor_tensor(out=pS[:, 0:4], in0=pS[:, 0:4], in1=msk_, op=ALU.mult), reads=[pS, mask2, colsel], writes=[pS])
                    accum(h, [0, 1, 2, 3], vcb[:, h * 128:(h + 1) * 128], onesb[:], pS[:, 0:4], 128)
            for h in range(8):
                ch = g * 8 + h
                b.op("pe", lambda e: e.matmul(pst[1][0:4, 0:4], lhsT=kn4[:, ch, :], rhs=qs4[:, ch, :], start=True, stop=True), reads=[kn4, qs4], writes=[pst[1]])
                b.op("act", lambda e: e.activation(out=pS[0:4, 0:4], in_=pst[1][0:4, 0:4], func=AF.Exp, scale=C_SCALE), reads=[pst[1]], writes=[pS])
                msk = mask2[0:4, 0:4] if d == 1 else idb[0:4, 0:4]
                b.op("dve", lambda e: e.tensor_tensor(out=pS[0:4, 0:4], in0=pS[0:4, 0:4], in1=msk, op=ALU.mult), reads=[pS, mask2, idb], writes=[pS])
                accum(h, [0, 1, 2, 3], vn4[0:4, ch * 128:(ch + 1) * 128], onesb[0:4, :], pS[0:4, 0:4], 4)
        b.op("dve", lambda e: e.reciprocal(out=dens[:], in_=dens[:]), reads=[dens], writes=[dens])
        b.op("dve", lambda e: e.tensor_tensor(out=os4[:], in0=nums[:], in1=dens[:], op=ALU.mult), reads=[nums, dens], writes=[os4])
        b.dma("sp", sOT[:, TP:NT].rearrange("(h p) t -> p h t", p=128), os4[:], reads=[os4], writes=[sOT.rs[8]])
        b.pop()

    def odd_G(l, o_):
        b.push()
        S['yt'] = b.sb("ytG", [128, NKC, 512]); ot8 = b.sb("ot8", [128, 8, 512], BF16)
        for (t0, n, ctx) in TILES:
            ti = t0 // 512
            b.dma("sp", ot8[:, :, 0:n], sOT[:, t0:t0 + n].rearrange("(h p) t -> p h t", p=128), reads=[sOT.rs[ti]], writes=[ot8])
            out_proj(attn_w_out[o_], 8, lambda kc: ot8[:, kc, 0:n], n, [ot8])
            post_norm_residual(l, 0, ti, t0, n, ctx)
        b.pop()

    def final_out():
        b.push()
        xo = b.sb("xo", [128, D])
        for (t0, n, ctx) in TILES:
            ti = t0 // 512
            b.dma("sp", xt[:, :, 0:n], X[:, t0:t0 + n].rearrange("(k p) t -> p k t", p=128), reads=[X.rs[ti]], writes=[xt])
            for s_ in range((n + 127) // 128):
                m = min(128, n - s_ * 128)
                for kc in range(NKC):
                    ps = pst[kc % 4]
                    b.op("pe", lambda e: e.transpose(ps[0:m, 0:128], xt[:, kc, s_ * 128:s_ * 128 + m], idf[:]), reads=[xt, idf], writes=[ps])
                    b.op("act" if kc % 2 else "dve", lambda e: (e.copy if kc % 2 else e.tensor_copy)(out=xo[0:m, kc * 128:(kc + 1) * 128], in_=ps[0:m, 0:128]), reads=[ps], writes=[xo])
                if ctx == 0:
                    b.dma("sp", yp[t0 + s_ * 128:t0 + s_ * 128 + m, :], xo[0:m, :], reads=[xo], writes=[yp.rs[ti]])
                else:
                    b.dma("sp", ys[0:m, :], xo[0:m, :], reads=[xo], writes=[ys])
        b.pop()

    b.odd = (odd_E, odd_F, odd_G)
    b.final_out = final_out
    b.ffn_layer = ffn_layer
    b.ctx_objs = dict(locals())
    return b


def _finish(b):
    o = b.ctx_objs
    b.finish(o["all_outs"])
    o["nc_cm"].__exit__(None, None, None)
    b.close()
    return b.nc


_CACHE = {}


def build_full(stop=None):
    b = build_program()
    ea, sc, ec = b.even
    oe, of, og = b.odd
    stages = []
    for l in range(4):
        if l % 2 == 0:
            stages += [lambda l=l: ea(l, l // 2), lambda l=l: sc(l // 2, 0), lambda l=l: sc(l // 2, 1), lambda l=l: ec(l, l // 2)]
        else:
            stages += [lambda l=l: oe(l, l // 2), lambda l=l: of(l, l // 2), lambda l=l: og(l, l // 2)]
        stages.append(lambda l=l: b.ffn_layer(l))
    stages.append(b.final_out)
    for i, st in enumerate(stages):
        if stop is not None and i >= stop:
            break
        st()
    return _finish(b)


def _get_nc():
    if "nc" not in _CACHE:
        _CACHE["nc"] = build_full()
    return _CACHE["nc"]


def make_in_maps(inp):
    f = lambda a: np.ascontiguousarray(np.asarray(a, dtype=np.float32))
    wnames = ["w_mod", "b_mod", "g_pre_mix", "g_post_mix", "g_pre_ffn", "g_post_ffn", "ab_w_in", "a_mu_rkv", "a_mu_wag",
              "a_w0", "a_w1", "a_w2", "a_a0", "a_a1", "a_a2", "a_g1", "a_g2", "a_k_k", "a_k_a", "a_ln_w", "a_ln_b",
              "b_conv_w", "b_conv_b", "b_ln_w", "b_ln_b", "ab_w_out", "attn_w_qkv", "attn_w_out", "ffn_w_gate", "ffn_w_up",
              "ffn_conv_w", "ffn_conv_b", "ffn_w_down"]
    shared = {k: f(inp[k]) for k in wnames}
    shared["a_r_k"] = f(inp["a_r_k"]).reshape(2, 1024)
    in_maps = []
    for c in range(8):
        bb = c % 2
        m = dict(shared)
        m["xp"] = f(inp["x_prompt"][bb]); m["xs"] = f(inp["x_sample"][c])
        m["cc"] = f(np.stack([inp["c_prompt"][bb], inp["c_sample"][c]]))
        m["st_shift"] = f(inp["state_shift"][:, c]); m["st_wkv"] = f(inp["state_wkv"][:, c])
        m["st_convb"] = f(inp["state_conv_b"][:, c]); m["st_ffn"] = f(inp["state_ffn"][:, c])
        for w in WINS:
            m[f"ck{w}"] = f(np.asarray(inp[f"cache_kv_w{w}"])[:, :, c].reshape(2, 2, w, 1024))
        in_maps.append(m)
    return in_maps


def kernel(**inp):
    nc = _get_nc()
    in_maps = make_in_maps(inp)
    res = run_bass_kernel_spmd(nc, in_maps, core_ids=list(range(8))).results
    P = lambda k: np.stack([res[0][k], res[1][k]])
    Sx = lambda k: np.stack([res[c][k] for c in range(8)])
    y_prompt = P("yp"); y_sample = Sx("ys")
    outs = [y_prompt, y_sample,
            np.moveaxis(P("p_shift"), 0, 1), np.moveaxis(P("p_wkv"), 0, 1), np.moveaxis(P("p_convb"), 0, 1), np.moveaxis(P("p_ffn"), 0, 1)]
    for w in WINS:
        a = P(f"pkv{w}")
        outs.append(np.transpose(a, (1, 2, 0, 3, 4)).reshape(2, 2, 2, w, 8, 128))
    outs += [np.moveaxis(Sx("s_shift"), 0, 1), np.moveaxis(Sx("s_wkv"), 0, 1), np.moveaxis(Sx("s_convb"), 0, 1), np.moveaxis(Sx("s_ffn"), 0, 1)]
    for w in WINS:
        a = Sx(f"skv{w}")
        outs.append(np.transpose(a, (1, 2, 0, 3, 4)).reshape(2, 2, 8, TS, 8, 128))
    return tuple(np.ascontiguousarray(o, dtype=np.float32) for o in outs)
```

```python
import contextlib
import os
import numpy as np
import concourse.bass as bass
import concourse.mybir as mybir
from concourse.bass_utils import run_bass_kernel_spmd

F32 = mybir.dt.float32
BF16 = mybir.dt.bfloat16
ALU = mybir.AluOpType
AF = mybir.ActivationFunctionType
AX = mybir.AxisListType


class Res:
    __slots__ = ("name", "w", "rd", "excl")

    def __init__(self, name):
        self.name = name
        self.excl = False
        self.w = None
        self.rd = {}


class T:
    def __init__(self, t, name, nres=1):
        self.t = t
        self.name = name
        self.rs = [Res(f"{name}.{i}") for i in range(nres)]

    @property
    def r(self):
        return self.rs[0]

    def __getitem__(self, idx):
        return self.t[idx]


class Eng:
    def __init__(self, b, key, h):
        self.b, self.key, self.h = b, key, h
        self.sem = None
        self.semid = None
        self.cnt = 0
        self.seen = {}
        self.nins = 0


class B:
    EPOCH = 8000
    NSLOT = 12

    def __init__(self):
        self.nc = bass.Bass("TRN2", target_bir_lowering=False)
        self.es = contextlib.ExitStack()
        self.semes = self.es
        self._stk = []
        self.old_latest = {}
        nc = self.nc
        self.E = {k: Eng(self, k, h) for k, h in
                  (("pe", nc.tensor), ("dve", nc.vector), ("act", nc.scalar), ("pool", nc.gpsimd), ("sp", nc.sync))}
        self.sems = {}
        self.nsem = 0
        self.slots = {}
        for q in ("sp", "pool"):
            self.slots[q] = [[self._newsem(f"d{q}{i}"), 0] for i in range(self.NSLOT)]
        self.slot_rr = {"sp": 0, "pool": 0}
        self.uid = 0

    def _newsem(self, name):
        h = self.semes.enter_context(self.nc.semaphore(f"{name}_{self.nsem}"))
        key = self.nsem
        self.sems[key] = h
        self.nsem += 1
        return key

    def sb(self, name, shape, dt=F32, nres=1):
        self.uid += 1
        name = f"{name}_u{self.uid}"
        t = self.es.enter_context(self.nc.sbuf_tensor(name, list(shape), dt))
        return T(t, name, nres)

    def ps(self, name, shape, dt=F32):
        t = self.es.enter_context(self.nc.psum_tensor(name, list(shape), dt))
        tt_ = T(t, name)
        tt_.rs[0].excl = True
        return tt_

    def dram(self, name, shape, dt, kind="Internal", nres=1):
        t = self.nc.dram_tensor(name, list(shape), dt, kind=kind).ap()
        return T(t, name, nres)

    def _needs(self, reads, writes):
        need = {}

        def add(kv):
            if kv is None:
                return
            k, v = kv
            if need.get(k, 0) < v:
                need[k] = v
        for r in reads:
            add(r.w)
            if r.excl:
                for kv in r.rd.items():
                    add(kv)
        for r in writes:
            add(r.w)
            for kv in r.rd.items():
                add(kv)
        return need

    def _emit_waits(self, e, need, skip_self=False):
        for k, v in need.items():
            if skip_self and k == e.semid:
                continue
            if e.seen.get(k, 0) >= v:
                continue
            e.h.wait_ge(self.sems[k], v)
            e.seen[k] = v

    @staticmethod
    def _resl(x):
        out = []
        for a in x:
            if isinstance(a, T):
                out.extend(a.rs)
            elif isinstance(a, Res):
                out.append(a)
            elif a is None:
                pass
            else:
                out.extend(B._resl(a))
        return out

    def op(self, ek, fn, reads=(), writes=()):
        e = self.E[ek]
        reads = self._resl(reads)
        writes = self._resl(writes)
        if e.sem is None or e.cnt >= self.EPOCH:
            if e.semid is not None:
                self.old_latest[e.semid] = e.cnt
            e.semid = self._newsem(f"e{ek}")
            e.sem = self.sems[e.semid]
            e.cnt = 0
        need = self._needs(reads, writes)
        self._emit_waits(e, need, skip_self=(ek == "pe"))
        ins = fn(e.h)
        e.cnt += 1
        e.nins += 1
        ins.then_inc(e.sem, 1)
        kv = (e.semid, e.cnt)
        for r in reads:
            r.rd[e.semid] = e.cnt
        for r in writes:
            r.w = kv
            r.rd = {}
        return ins

    def dma(self, q, out, in_, reads=(), writes=(), **kw):
        e = self.E[q]
        reads = self._resl(reads)
        writes = self._resl(writes)
        need = self._needs(reads, writes)
        i = self.slot_rr[q]
        self.slot_rr[q] = (i + 1) % self.NSLOT
        slot = self.slots[q][i]
        if slot[1] > 0:
            need[slot[0]] = max(need.get(slot[0], 0), 16 * slot[1])
        if slot[1] >= 500:
            self.old_latest[slot[0]] = 16 * slot[1]
            slot[0] = self._newsem(f"d{q}{i}")
            slot[1] = 0
        self._emit_waits(e, need)
        ins = e.h.dma_start(out=out, in_=in_, **kw)
        slot[1] += 1
        e.nins += 1
        ins.then_inc(self.sems[slot[0]], 16)
        kv = (slot[0], 16 * slot[1])
        for r in reads:
            r.rd[slot[0]] = 16 * slot[1]
        for r in writes:
            r.w = kv
            r.rd = {}
        return ins

    def push(self):
        self._stk.append(self.es)
        self.es = contextlib.ExitStack()

    def pop(self):
        self.barrier()
        self.es.close()
        self.es = self._stk.pop()

    def barrier(self):
        latest = {}
        for e in self.E.values():
            if e.semid is not None:
                latest[e.semid] = e.cnt
        for q in self.slots:
            for sl in self.slots[q]:
                if sl[1] > 0:
                    latest[sl[0]] = 16 * sl[1]
        for k, v in self.old_latest.items():
            latest.setdefault(k, v)
        for e in self.E.values():
            self._emit_waits(e, latest, skip_self=False)

    def finish(self, outs):
        e = self.E["sp"]
        need = self._needs(self._resl(outs), [])
        self._emit_waits(e, need)

    def close(self):
        self.es.close()


D = 2048
TP = 4096
TS = 4
NT = TP + TS
NKC = 16
DFF = 5632
NFC = 44
TILES = [(i * 512, 512, 0) for i in range(8)] + [(TP, TS, 1)]
EXPC = 0.6065306597126334
C_SCALE = 128 ** -0.5
WINS = (128, 512, 2048)
DO_PROMPT = [True]
DILS = (1, 4, 16)


def build_program():
    b = B()
    nc = b.nc
    nc_cm = nc.allow_non_contiguous_dma(reason="small param / state layout transforms")
    nc_cm.__enter__()
    I = {}

    def inp(name, shape):
        I[name] = b.dram(name, shape, F32, kind="ExternalInput")
        return I[name]

    def outp(name, shape, nres=1):
        I[name] = b.dram(name, shape, F32, kind="ExternalOutput", nres=nres)
        return I[name]

    xp = inp("xp", [TP, D]); xs = inp("xs", [TS, D]); cc = inp("cc", [2, D])
    st_shift = inp("st_shift", [2, D]); st_wkv = inp("st_wkv", [2, 16, 64, 64])
    st_convb = inp("st_convb", [2, 30, 1024]); st_ffn = inp("st_ffn", [4, 2, DFF])
    cks = [inp(f"ck{w}", [2, 2, w, 1024]) for w in WINS]
    w_mod = inp("w_mod", [4, D, 6 * D]); b_mod = inp("b_mod", [4, 6 * D])
    gpar = {k: inp(k, [4, D]) for k in ("g_pre_mix", "g_post_mix", "g_pre_ffn", "g_post_ffn")}
    ab_w_in = inp("ab_w_in", [2, D, 5120]); a_mu_rkv = inp("a_mu_rkv", [2, 3072]); a_mu_wag = inp("a_mu_wag", [2, 3, D])
    a_w0 = inp("a_w0", [2, 1024]); a_w1 = inp("a_w1", [2, D, 96]); a_w2 = inp("a_w2", [2, 96, 1024])
    a_a0 = inp("a_a0", [2, 1024]); a_a1 = inp("a_a1", [2, D, 96]); a_a2 = inp("a_a2", [2, 96, 1024])
    a_g1 = inp("a_g1", [2, D, 256]); a_g2 = inp("a_g2", [2, 256, 1024])
    a_k_k = inp("a_k_k", [2, 1024]); a_k_a = inp("a_k_a", [2, 1024]); a_r_k = inp("a_r_k", [2, 1024])
    a_ln_w = inp("a_ln_w", [2, 1024]); a_ln_b = inp("a_ln_b", [2, 1024])
    b_conv_w = inp("b_conv_w", [2, 31, 1024]); b_conv_b = inp("b_conv_b", [2, 1024])
    b_ln_w = inp("b_ln_w", [2, 1024]); b_ln_b = inp("b_ln_b", [2, 1024])
    ab_w_out = inp("ab_w_out", [2, D, D])
    attn_w_qkv = inp("attn_w_qkv", [2, D, 9216]); attn_w_out = inp("attn_w_out", [2, 1024, D])
    ffn_w_gate = inp("ffn_w_gate", [4, D, DFF]); ffn_w_up = inp("ffn_w_up", [4, D, DFF])
    ffn_conv_w = inp("ffn_conv_w", [4, 3, DFF]); ffn_conv_b = inp("ffn_conv_b", [4, DFF]); ffn_w_down = inp("ffn_w_down", [4, DFF, D])

    yp = outp("yp", [TP, D], nres=8); ys = outp("ys", [TS, D])
    o_shift = [outp("p_shift", [2, D]), outp("s_shift", [2, D])]
    o_wkv = [outp("p_wkv", [2, 16, 64, 64]), outp("s_wkv", [2, 16, 64, 64])]
    o_convb = [outp("p_convb", [2, 30, 1024]), outp("s_convb", [2, 30, 1024])]
    o_ffn = [outp("p_ffn", [4, 2, DFF]), outp("s_ffn", [4, 2, DFF])]
    o_kv = [[outp(f"pkv{w}", [2, 2, w, 1024]) for w in WINS], [outp(f"skv{w}", [2, 2, TS, 1024]) for w in WINS]]
    all_outs = [yp, ys] + o_shift + o_wkv + o_convb + o_ffn + o_kv[0] + o_kv[1]

    X = b.dram("X", [D, NT], F32, nres=9)
    sR, sK2, sV, sW, sA, sBb, sG, sU, sO = [b.dram(n, [1024, NT], F32, nres=9) for n in
                                          ("sR", "sK2", "sV", "sW", "sA", "sBb", "sG", "sU", "sO")]
    sVtm = b.dram("sVtm", [NT, 1024], F32, nres=9)
    sQT = b.dram("sQT", [3072, NT], BF16, nres=9); sKT = b.dram("sKT", [3072, NT], BF16, nres=9)
    sVa = b.dram("sVa", [NT, 3072], BF16, nres=9); sOT = b.dram("sOT", [1024, NT], BF16, nres=9)

    idf = b.sb("idf", [128, 128]); idb = b.sb("idb", [128, 128], BF16)
    onesf = b.sb("onesf", [128, 128]); onesb = b.sb("onesb", [128, 128], BF16)
    blk1 = b.sb("blk1", [128, 128])
    sel = b.sb("sel", [2, 128]); maskj = b.sb("maskj", [128, 2])
    mask2 = b.sb("mask2", [128, 256], BF16)
    b.op("pool", lambda e: e.memset(idf[:], 1.0), writes=[idf])
    b.op("pool", lambda e: e.affine_select(out=idf[:], in_=idf[:], pattern=[[-1, 128]], compare_op=ALU.is_equal, fill=0.0, base=0, channel_multiplier=1), reads=[idf], writes=[idf])
    b.op("dve", lambda e: e.tensor_copy(out=idb[:], in_=idf[:]), reads=[idf], writes=[idb])
    b.op("pool", lambda e: e.memset(onesf[:], 1.0), writes=[onesf])
    b.op("pool", lambda e: e.memset(onesb[:], 1.0), writes=[onesb])
    b.op("pool", lambda e: e.memset(sel[:], 1.0), writes=[sel])
    b.op("pool", lambda e: e.affine_select(out=sel[:], in_=sel[:], pattern=[[1, 128]], compare_op=ALU.is_ge, fill=0.0, base=0, channel_multiplier=-64), reads=[sel], writes=[sel])
    b.op("pool", lambda e: e.affine_select(out=sel[:], in_=sel[:], pattern=[[-1, 128]], compare_op=ALU.is_ge, fill=0.0, base=63, channel_multiplier=64), reads=[sel], writes=[sel])
    pst = [b.ps(f"pst{i}", [128, 512]) for i in range(8)]
    b.op("pe", lambda e: e.matmul(pst[0][:, 0:128], lhsT=sel[:], rhs=sel[:], start=True, stop=True), reads=[sel], writes=[pst[0]])
    b.op("dve", lambda e: e.tensor_copy(out=blk1[:], in_=pst[0][:, 0:128]), reads=[pst[0]], writes=[blk1])
    b.op("pe", lambda e: e.matmul(pst[1][:, 0:2], lhsT=sel[:], rhs=idf[0:2, 0:2], start=True, stop=True), reads=[sel, idf], writes=[pst[1]])
    b.op("dve", lambda e: e.tensor_copy(out=maskj[:], in_=pst[1][:, 0:2]), reads=[pst[1]], writes=[maskj])
    b.op("pool", lambda e: e.memset(mask2[:], 1.0), writes=[mask2])
    b.op("pool", lambda e: e.affine_select(out=mask2[:, 0:128], in_=mask2[:, 0:128], pattern=[[1, 128]], compare_op=ALU.is_ge, fill=0.0, base=0, channel_multiplier=-1), reads=[mask2], writes=[mask2])
    b.op("pool", lambda e: e.affine_select(out=mask2[:, 128:256], in_=mask2[:, 128:256], pattern=[[-1, 128]], compare_op=ALU.is_ge, fill=0.0, base=0, channel_multiplier=1), reads=[mask2], writes=[mask2])

    xt = b.sb("xt", [128, NKC, 512])
    S = {}
    hx = b.sb("hx", [128, NKC, 514], BF16)
    hf = b.sb("hf", [128, 513])
    wb = [b.sb(f"wb{i}", [128, 8192], BF16) for i in range(2)]
    wsel = [0]
    t1 = b.sb("t1", [128, 512]); t2 = b.sb("t2", [128, 512]); t3 = b.sb("t3", [128, 512]); t4 = b.sb("t4", [128, 512])
    rstd = b.sb("rstd", [128, 512])
    carry_h = b.sb("carry_h", [128, NKC, 1], BF16)
    colf = b.sb("colf", [128, NKC, 1])

    def fmvec(dst_ap, dram_ap_1d, q="sp", reads=(), writes=()):
        b.dma(q, dst_ap, dram_ap_1d.rearrange("(k p) -> p k", p=128), reads=reads, writes=writes)

    def load_w(wap, nk, ncols, krows=128):
        t = wb[wsel[0]]
        wsel[0] ^= 1
        v = t[0:krows, 0:nk * ncols].rearrange("p (k c) -> p k c", c=ncols)
        b.dma("pool", v, wap.rearrange("(k p) c -> p k c", p=krows), writes=[t])
        return t, v

    modv = b.sb("modv", [128, 4, 96, 2]); bmod = b.sb("bmod", [128, 4, 96])
    ccT = b.sb("ccT", [128, NKC, 2], BF16)
    b.push()
    ccf = b.sb("ccf", [2, D])
    b.dma("sp", ccf[:], cc[:, :], writes=[ccf])
    for kc in range(NKC):
        b.op("pe", lambda e: e.transpose(pst[kc % 2][:, 0:2], ccf[0:2, kc * 128:(kc + 1) * 128], idf[0:2, 0:2]), reads=[ccf, idf], writes=[pst[kc % 2]])
        b.op("dve", lambda e: e.tensor_copy(out=ccT[:, kc, :], in_=pst[kc % 2][:, 0:2]), reads=[pst[kc % 2]], writes=[ccT])
    b.pop()
    for l in range(4):
        fmvec(bmod[:, l, :], b_mod[l, :], writes=[bmod])
        for gq in range(24):
            wt_, wv = load_w(w_mod[l, :, gq * 512:(gq + 1) * 512], NKC, 512)
            for mc in range(4):
                ps = pst[(gq * 4 + mc) % 4]
                for kc in range(NKC):
                    b.op("pe", lambda e: e.matmul(ps[:, 0:2], lhsT=wv[:, kc, mc * 128:(mc + 1) * 128], rhs=ccT[:, kc, :], start=(kc == 0), stop=(kc == NKC - 1)), reads=[wt_, ccT], writes=[ps])
                ch = gq * 4 + mc
                b.op("dve", lambda e: e.tensor_scalar(out=modv[:, l, ch, :], in0=ps[:, 0:2], scalar1=bmod[:, l, ch:ch + 1], scalar2=None, op0=ALU.add), reads=[ps, bmod], writes=[modv])

    gv = {k: b.sb("gv_" + k, [128, 4, NKC]) for k in gpar}
    for k in gpar:
        for l in range(4):
            fmvec(gv[k][:, l, :], gpar[k][l, :], writes=[gv[k]])
    gs = b.sb("gs", [128, 4, 2, NKC, 2])
    for l in range(4):
        for wi, (gk, si) in enumerate((("g_pre_mix", 1), ("g_pre_ffn", 4))):
            for ctx in range(2):
                b.op("dve", lambda e: e.tensor_scalar(out=gs[:, l, wi, :, ctx], in0=modv[:, l, si * 16:(si + 1) * 16, ctx], scalar1=1.0, scalar2=None, op0=ALU.add), reads=[modv], writes=[gs])
                b.op("dve", lambda e: e.tensor_tensor(out=gs[:, l, wi, :, ctx], in0=gs[:, l, wi, :, ctx], in1=gv[gk][:, l, :], op=ALU.mult), reads=[gs, gv[gk]], writes=[gs])
    gp = b.sb("gp", [128, 4, 2, NKC, 2])
    for l in range(4):
        for wi, (gk, gi) in enumerate((("g_post_mix", 2), ("g_post_ffn", 5))):
            for ctx in range(2):
                b.op("dve", lambda e: e.tensor_tensor(out=gp[:, l, wi, :, ctx], in0=modv[:, l, gi * 16:(gi + 1) * 16, ctx], in1=gv[gk][:, l, :], op=ALU.mult), reads=[modv, gv[gk]], writes=[gp])

    b.push()
    xin = b.sb("xin", [128, D])
    for (t0, n, ctx) in TILES:
        ti = t0 // 512
        for s in range((n + 127) // 128):
            m = min(128, n - s * 128)
            src = xp[t0 + s * 128:t0 + s * 128 + m, :] if ctx == 0 else xs[0:m, :]
            b.dma("sp", xin[0:m, :], src, writes=[xin])
            for kc in range(NKC):
                ps = pst[kc % 4]
                b.op("pe", lambda e: e.transpose(ps[:, 0:m], xin[0:m, kc * 128:(kc + 1) * 128], idf[0:m, 0:m]), reads=[xin, idf], writes=[ps])
                b.op("act" if kc % 2 else "dve", lambda e: (e.copy if kc % 2 else e.tensor_copy)(out=xt[:, kc, s * 128:s * 128 + m], in_=ps[:, 0:m]), reads=[ps], writes=[xt])
        b.dma("sp", X[:, t0:t0 + n].rearrange("(k p) t -> p k t", p=128), xt[:, :, 0:n], reads=[xt], writes=[X.rs[ti]])

    b.pop()
    def sumsq_rstd(src, n, nch, dim, eps, src_res):
        ps = pst[7]
        for kc in range(nch):
            b.op("act", lambda e: e.activation(out=t1[:, 0:n], in_=src[:, kc, 0:n], func=AF.Square), reads=[src_res], writes=[t1])
            b.op("pe", lambda e: e.matmul(ps[:, 0:n], lhsT=onesf[:], rhs=t1[:, 0:n], start=(kc == 0), stop=(kc == nch - 1)), reads=[onesf, t1], writes=[ps])
        b.op("act", lambda e: e.activation(out=rstd[:, 0:n], in_=ps[:, 0:n], func=AF.Ln, bias=float(eps), scale=1.0 / dim), reads=[ps], writes=[rstd])
        b.op("act", lambda e: e.activation(out=rstd[:, 0:n], in_=rstd[:, 0:n], func=AF.Exp, scale=-0.5), reads=[rstd], writes=[rstd])

    def pre_norm(l, wi, ti, t0, n, ctx, shift_out=None):
        si = 0 if wi == 0 else 3
        b.dma("sp", xt[:, :, 0:n], X[:, t0:t0 + n].rearrange("(k p) t -> p k t", p=128), reads=[X.rs[ti]], writes=[xt])
        sumsq_rstd(xt, n, NKC, D, 1e-6, xt)
        for kc in range(NKC):
            b.op("dve", lambda e: e.tensor_tensor(out=hf[:, 1:n + 1], in0=xt[:, kc, 0:n], in1=rstd[:, 0:n], op=ALU.mult), reads=[xt, rstd], writes=[hf])
            if shift_out is not None:
                b.op("dve", lambda e: e.tensor_scalar(out=colf[:, kc, :], in0=hf[:, n:n + 1], scalar1=gs[:, l, wi, kc, ctx:ctx + 1], scalar2=modv[:, l, si * 16 + kc, ctx:ctx + 1], op0=ALU.mult, op1=ALU.add), reads=[hf, gs, modv], writes=[colf])
            b.op("dve", lambda e: e.tensor_scalar(out=hx[:, kc, 2:n + 2], in0=hf[:, 1:n + 1], scalar1=gs[:, l, wi, kc, ctx:ctx + 1], scalar2=modv[:, l, si * 16 + kc, ctx:ctx + 1], op0=ALU.mult, op1=ALU.add), reads=[hf, gs, modv], writes=[hx])
        if shift_out is not None:
            b.dma("sp", shift_out.rearrange("(k p) -> p k", p=128), colf[:, :, 0], reads=[colf], writes=[shift_out_res[0]])

    shift_out_res = [None]

    def post_norm_residual(l, wi, ti, t0, n, ctx):
        sumsq_rstd(S['yt'], n, NKC, D, 1e-6, S['yt'])
        b.dma("sp", xt[:, :, 0:n], X[:, t0:t0 + n].rearrange("(k p) t -> p k t", p=128), reads=[X.rs[ti]], writes=[xt])
        for kc in range(NKC):
            b.op("dve", lambda e: e.tensor_tensor(out=t2[:, 0:n], in0=S['yt'][:, kc, 0:n], in1=rstd[:, 0:n], op=ALU.mult), reads=[S['yt'], rstd], writes=[t2])
            b.op("dve", lambda e: e.scalar_tensor_tensor(out=xt[:, kc, 0:n], in0=t2[:, 0:n], scalar=gp[:, l, wi, kc, ctx:ctx + 1], in1=xt[:, kc, 0:n], op0=ALU.mult, op1=ALU.add), reads=[t2, gp, xt], writes=[xt])
        b.dma("sp", X[:, t0:t0 + n].rearrange("(k p) t -> p k t", p=128), xt[:, :, 0:n], reads=[xt], writes=[X.rs[ti]])

    def mm_group(wap, nk, ncols, rhs_fn, n, evac, rhs_res, krows=128, ps_base=0):
        wt_, wv = load_w(wap, nk, ncols, krows)
        for mc in range(ncols // 128):
            ps = pst[ps_base + (mc % 4)]
            for kc in range(nk):
                b.op("pe", lambda e: e.matmul(ps[:, 0:n], lhsT=wv[:, kc, mc * 128:(mc + 1) * 128], rhs=rhs_fn(kc), start=(kc == 0), stop=(kc == nk - 1)), reads=[wt_] + list(rhs_res), writes=[ps])
            evac(mc, ps)

    def out_proj(wap, nk, rhs_fn, n, rhs_res):
        for gq in range(4):
            def ev(mc, ps, gq=gq):
                ch = gq * 4 + mc
                b.op("act" if ch % 2 else "dve", lambda e: (e.copy if ch % 2 else e.tensor_copy)(out=S['yt'][:, ch, 0:n], in_=ps[:, 0:n]), reads=[ps], writes=[S['yt']])
            mm_group(wap[:, gq * 512:(gq + 1) * 512], nk, 512, rhs_fn, n, ev, rhs_res)

    def ffn_layer(l):
        b.push()
        S['yt'] = b.sb("yt", [128, NKC, 512]); S['big'] = b.sb("big", [128, NFC, 512], BF16)
        gcar = b.sb("gcar", [128, NFC, 2]); gpad = b.sb("gpad", [128, 514]); gst = b.sb("gst", [128, NFC, 2])
        fcw = b.sb("fcw", [128, 3, NFC]); fcb = b.sb("fcb", [128, NFC])
        sg = b.sb("sg", [128, 4, 512])
        for j in range(3):
            fmvec(fcw[:, j, :], ffn_conv_w[l, j, :], writes=[fcw])
        fmvec(fcb[:, :], ffn_conv_b[l, :], writes=[fcb])
        for (t0, n, ctx) in TILES:
            ti = t0 // 512
            if t0 == 0:
                b.op("dve", lambda e: e.memset(gcar[:], 0.0), writes=[gcar])
            if ctx == 1:
                for j in range(2):
                    fmvec(gcar[:, :, j], st_ffn[l, j, :], writes=[gcar])
            pre_norm(l, 1, ti, t0, n, ctx)
            for gq in range(11):
                def ev_gate(mc, ps, gq=gq):
                    ch = gq * 4 + mc
                    b.op("act", lambda e: e.copy(out=gpad[:, 2:n + 2], in_=ps[:, 0:n]), reads=[ps], writes=[gpad])
                    b.op("dve", lambda e: e.tensor_copy(out=gpad[:, 0:2], in_=gcar[:, ch, :]), reads=[gcar], writes=[gpad])
                    b.op("dve", lambda e: e.tensor_copy(out=gcar[:, ch, :], in_=gpad[:, n:n + 2]), reads=[gpad], writes=[gcar])
                    if t0 + n == TP or ctx == 1:
                        b.op("dve", lambda e: e.tensor_copy(out=gst[:, ch, :], in_=gpad[:, n:n + 2]), reads=[gpad], writes=[gst])
                    b.op("dve", lambda e: e.tensor_scalar(out=t3[:, 0:n], in0=gpad[:, 0:n], scalar1=fcw[:, 0, ch:ch + 1], scalar2=fcb[:, ch:ch + 1], op0=ALU.mult, op1=ALU.add), reads=[gpad, fcw, fcb], writes=[t3])
                    b.op("dve", lambda e: e.scalar_tensor_tensor(out=t3[:, 0:n], in0=gpad[:, 1:n + 1], scalar=fcw[:, 1, ch:ch + 1], in1=t3[:, 0:n], op0=ALU.mult, op1=ALU.add), reads=[gpad, fcw, t3], writes=[t3])
                    b.op("dve", lambda e: e.scalar_tensor_tensor(out=t3[:, 0:n], in0=gpad[:, 2:n + 2], scalar=fcw[:, 2, ch:ch + 1], in1=t3[:, 0:n], op0=ALU.mult, op1=ALU.add), reads=[gpad, fcw, t3], writes=[t3])
                    b.op("act", lambda e: e.activation(out=sg[:, mc, 0:n], in_=t3[:, 0:n], func=AF.Silu), reads=[t3], writes=[sg])

                def ev_up(mc, ps, gq=gq):
                    ch = gq * 4 + mc
                    b.op("dve", lambda e: e.tensor_tensor(out=S['big'][:, ch, 0:n], in0=ps[:, 0:n], in1=sg[:, mc, 0:n], op=ALU.mult), reads=[ps, sg], writes=[S['big']])
                mm_group(ffn_w_gate[l, :, gq * 512:(gq + 1) * 512], NKC, 512, lambda kc: hx[:, kc, 2:n + 2], n, ev_gate, [hx])
                mm_group(ffn_w_up[l, :, gq * 512:(gq + 1) * 512], NKC, 512, lambda kc: hx[:, kc, 2:n + 2], n, ev_up, [hx], ps_base=4)
            if t0 + n == TP or ctx == 1:
                for j in range(2):
                    b.dma("sp", o_ffn[ctx][l, j, :].rearrange("(k p) -> p k", p=128), gst[:, :, j], reads=[gst], writes=[o_ffn[ctx]])
            for ch in range(NKC):
                def ev(mc, ps, ch=ch):
                    b.op("act" if ch % 2 else "dve", lambda e: (e.copy if ch % 2 else e.tensor_copy)(out=S['yt'][:, ch, 0:n], in_=ps[:, 0:n]), reads=[ps], writes=[S['yt']])
                mm_group(ffn_w_down[l, :, ch * 128:(ch + 1) * 128], NFC, 128, lambda kc: S['big'][:, kc, 0:n], n, ev, [S['big']], ps_base=ch % 4)
            post_norm_residual(l, 1, ti, t0, n, ctx)
        b.pop()


    evp = b.sb("evp", [128, 16, 8])
    murkv = b.sb("murkv", [128, 24]); muwag = b.sb("muwag", [128, 3, NKC])
    cw = b.sb("cw", [128, 31, 8])
    def even_A(l, e_):
        b.push()
        S['big'] = b.sb("bigA", [128, NKC, 512], BF16)
        lw1 = b.sb("lw1", [128, NKC, 448], BF16)
        lw2 = b.sb("lw2", [128, 4, 1024], BF16)
        pa = b.sb("pa", [128, 513]); pacar = b.sb("pacar", [128, 24, 1])
        kmix = b.sb("kmix", [128, 8, 512]); sigb = b.sb("sigb", [128, 8, 512])
        lmid = b.sb("lmid", [128, 4, 512], BF16)
        vtm = b.sb("vtm", [128, 4, 1024])
        for i, src in enumerate((a_w0, a_a0, a_k_k, a_k_a, a_r_k, a_ln_w, a_ln_b, b_conv_b, b_ln_w, b_ln_b)):
            fmvec(evp[:, i, :], src[e_, :], writes=[evp])
        fmvec(murkv[:, :], a_mu_rkv[e_, :], writes=[murkv])
        for i in range(3):
            fmvec(muwag[:, i, :], a_mu_wag[e_, i, :], writes=[muwag])
        for j in range(31):
            fmvec(cw[:, j, :], b_conv_w[e_, j, :], writes=[cw])
        b.dma("pool", lw1[:, :, 0:96], a_w1[e_].rearrange("(k p) c -> p k c", p=128), writes=[lw1])
        b.dma("pool", lw1[:, :, 96:192], a_a1[e_].rearrange("(k p) c -> p k c", p=128), writes=[lw1])
        b.dma("pool", lw1[:, :, 192:448], a_g1[e_].rearrange("(k p) c -> p k c", p=128), writes=[lw1])
        b.dma("pool", lw2[0:96, 0, :], a_w2[e_], writes=[lw2])
        b.dma("pool", lw2[0:96, 1, :], a_a2[e_], writes=[lw2])
        b.dma("pool", lw2[:, 2:4, :], a_g2[e_].rearrange("(k p) c -> p k c", p=128), writes=[lw2])
        for (t0, n, ctx) in TILES:
            ti = t0 // 512
            last = (t0 + n == TP) or ctx == 1
            shift_out_res[0] = o_shift[ctx]
            pre_norm(l, 0, ti, t0, n, ctx, shift_out=(o_shift[ctx][e_, :] if last else None))
            if t0 == 0:
                b.op("dve", lambda e: e.memset(carry_h[:], 0.0), writes=[carry_h])
                b.op("dve", lambda e: e.memset(pacar[:], 0.0), writes=[pacar])
            if ctx == 1:
                fmvec(colf[:, :, 0], st_shift[e_, :], writes=[colf])
                b.op("dve", lambda e: e.tensor_copy(out=carry_h[:], in_=colf[:]), reads=[colf], writes=[carry_h])
            b.op("dve", lambda e: e.tensor_copy(out=hx[:, :, 1:2], in_=carry_h[:]), reads=[carry_h], writes=[hx])
            b.op("dve", lambda e: e.tensor_copy(out=carry_h[:], in_=hx[:, :, n + 1:n + 2]), reads=[hx], writes=[carry_h])
            c0 = 0 if ctx == 1 else 1
            for gq in (0, 1, 2, 3, 4, 5, 8, 9, 6, 7):
                def ev(mc, ps, gq=gq):
                    ch = gq * 4 + mc
                    if ch < 24:
                        b.op("act", lambda e: e.copy(out=pa[:, c0:n + 1], in_=ps[:, 0:n + 1 - c0]), reads=[ps], writes=[pa])
                        if c0 == 1:
                            b.op("dve", lambda e: e.tensor_copy(out=pa[:, 0:1], in_=pacar[:, ch, :]), reads=[pacar], writes=[pa])
                        b.op("dve", lambda e: e.tensor_copy(out=pacar[:, ch, :], in_=pa[:, n:n + 1]), reads=[pa], writes=[pacar])
                        b.op("dve", lambda e: e.tensor_tensor(out=t2[:, 0:n], in0=pa[:, 0:n], in1=pa[:, 1:n + 1], op=ALU.subtract), reads=[pa], writes=[t2])
                        sec, cc_ = ch // 8, ch % 8
                        dst = kmix[:, cc_, 0:n] if sec == 1 else t3[:, 0:n]
                        b.op("dve", lambda e: e.scalar_tensor_tensor(out=dst, in0=t2[:, 0:n], scalar=murkv[:, ch:ch + 1], in1=pa[:, 1:n + 1], op0=ALU.mult, op1=ALU.add), reads=[t2, murkv, pa], writes=[kmix if sec == 1 else t3])
                        if sec == 0:
                            b.dma("sp", sR[cc_ * 128:(cc_ + 1) * 128, t0:t0 + n], t3[:, 0:n], reads=[t3], writes=[sR.rs[ti]])
                        if sec == 2:
                            b.dma("sp", sV[cc_ * 128:(cc_ + 1) * 128, t0:t0 + n], t3[:, 0:n], reads=[t3], writes=[sV.rs[ti]])
                            for s_ in range((n + 127) // 128):
                                m = min(128, n - s_ * 128)
                                pq = pst[4 + s_ % 2]
                                b.op("pe", lambda e: e.transpose(pq[0:m, 0:128], t3[:, s_ * 128:s_ * 128 + m], idf[:]), reads=[t3, idf], writes=[pq])
                                b.op("act", lambda e: e.copy(out=vtm[0:m, s_, cc_ * 128:(cc_ + 1) * 128], in_=pq[0:m, 0:128]), reads=[pq], writes=[vtm])
                                if cc_ == 7:
                                    b.dma("sp", sVtm[t0 + s_ * 128:t0 + s_ * 128 + m, :], vtm[0:m, s_, :], reads=[vtm], writes=[sVtm.rs[ti]])
                    elif ch >= 32:
                        b.op("act", lambda e: e.activation(out=sigb[:, ch - 32, 0:n], in_=ps[:, c0 ^ 1:n + (c0 ^ 1)], func=AF.Sigmoid), reads=[ps], writes=[sigb])
                    else:
                        cc_ = ch - 24
                        b.op("dve", lambda e: e.tensor_tensor(out=t3[:, 0:n], in0=ps[:, c0 ^ 1:n + (c0 ^ 1)], in1=sigb[:, cc_, 0:n], op=ALU.mult), reads=[ps, sigb], writes=[t3])
                        b.dma("sp", sU[cc_ * 128:(cc_ + 1) * 128, t0:t0 + n], t3[:, 0:n], reads=[t3], writes=[sU.rs[ti]])
                mm_group(ab_w_in[e_, :, gq * 512:(gq + 1) * 512], NKC, 512, lambda kc: hx[:, kc, c0 + 1:n + 2], n + 1 - c0, ev, [hx])
            for i in range(3):
                for kc in range(NKC):
                    b.op("dve", lambda e: e.tensor_tensor(out=t2[:, 0:n], in0=hx[:, kc, 1:n + 1], in1=hx[:, kc, 2:n + 2], op=ALU.subtract), reads=[hx], writes=[t2])
                    b.op("dve", lambda e: e.scalar_tensor_tensor(out=S['big'][:, kc, 0:n], in0=t2[:, 0:n], scalar=muwag[:, i, kc:kc + 1], in1=hx[:, kc, 2:n + 2], op0=ALU.mult, op1=ALU.add), reads=[t2, muwag, hx], writes=[S['big']])
                if i < 2:
                    ps = pst[4 + i]
                    for kc in range(NKC):
                        b.op("pe", lambda e: e.matmul(ps[0:96, 0:n], lhsT=lw1[:, kc, i * 96:(i + 1) * 96], rhs=S['big'][:, kc, 0:n], start=(kc == 0), stop=(kc == NKC - 1)), reads=[lw1, S['big']], writes=[ps])
                    if i == 0:
                        b.op("act", lambda e: e.activation(out=lmid[0:96, 0, 0:n], in_=ps[0:96, 0:n], func=AF.Tanh), reads=[ps], writes=[lmid])
                    else:
                        b.op("act", lambda e: e.copy(out=lmid[0:96, 1, 0:n], in_=ps[0:96, 0:n]), reads=[ps], writes=[lmid])
                else:
                    for hh in range(2):
                        ps = pst[6 + hh]
                        for kc in range(NKC):
                            b.op("pe", lambda e: e.matmul(ps[:, 0:n], lhsT=lw1[:, kc, 192 + hh * 128:192 + (hh + 1) * 128], rhs=S['big'][:, kc, 0:n], start=(kc == 0), stop=(kc == NKC - 1)), reads=[lw1, S['big']], writes=[ps])
                        b.op("act", lambda e: e.activation(out=lmid[:, 2 + hh, 0:n], in_=ps[:, 0:n], func=AF.Sigmoid), reads=[ps], writes=[lmid])
            for cc_ in range(8):
                cs = slice(cc_ * 128, (cc_ + 1) * 128)
                ps = pst[cc_ % 4]
                b.op("pe", lambda e: e.matmul(ps[:, 0:n], lhsT=lw2[0:96, 0, cs], rhs=lmid[0:96, 0, 0:n], start=True, stop=True), reads=[lw2, lmid], writes=[ps])
                b.op("act", lambda e: e.activation(out=t2[:, 0:n], in_=ps[:, 0:n], func=AF.Sigmoid, bias=evp[:, 0, cc_:cc_ + 1], scale=1.0), reads=[ps, evp], writes=[t2])
                b.op("act", lambda e: e.activation(out=t2[:, 0:n], in_=t2[:, 0:n], func=AF.Exp, scale=-EXPC), reads=[t2], writes=[t2])
                b.dma("sp", sW[cs, t0:t0 + n], t2[:, 0:n], reads=[t2], writes=[sW.rs[ti]])
                ps = pst[4 + cc_ % 2]
                b.op("pe", lambda e: e.matmul(ps[:, 0:n], lhsT=lw2[0:96, 1, cs], rhs=lmid[0:96, 1, 0:n], start=True, stop=True), reads=[lw2, lmid], writes=[ps])
                b.op("act", lambda e: e.activation(out=t4[:, 0:n], in_=ps[:, 0:n], func=AF.Sigmoid, bias=evp[:, 1, cc_:cc_ + 1], scale=1.0), reads=[ps, evp], writes=[t4])
                ps = pst[6 + cc_ % 2]
                for hh in range(2):
                    b.op("pe", lambda e: e.matmul(ps[:, 0:n], lhsT=lw2[:, 2 + hh, cs], rhs=lmid[:, 2 + hh, 0:n], start=(hh == 0), stop=(hh == 1)), reads=[lw2, lmid], writes=[ps])
                b.op("act", lambda e: e.copy(out=t3[:, 0:n], in_=ps[:, 0:n]), reads=[ps], writes=[t3])
                b.dma("sp", sG[cs, t0:t0 + n], t3[:, 0:n], reads=[t3], writes=[sG.rs[ti]])
                b.op("dve", lambda e: e.tensor_scalar(out=t1[:, 0:n], in0=kmix[:, cc_, 0:n], scalar1=evp[:, 2, cc_:cc_ + 1], scalar2=None, op0=ALU.mult), reads=[kmix, evp], writes=[t1])
                b.op("dve", lambda e: e.tensor_tensor(out=t2[:, 0:n], in0=t1[:, 0:n], in1=t1[:, 0:n], op=ALU.mult), reads=[t1], writes=[t2])
                ps = pst[cc_ % 4]
                b.op("pe", lambda e: e.matmul(ps[:, 0:n], lhsT=blk1[:], rhs=t2[:, 0:n], start=True, stop=True), reads=[blk1, t2], writes=[ps])
                b.op("dve", lambda e: e.tensor_scalar(out=t2[:, 0:n], in0=ps[:, 0:n], scalar1=1e-24, scalar2=None, op0=ALU.max), reads=[ps], writes=[t2])
                b.op("act", lambda e: e.activation(out=t2[:, 0:n], in_=t2[:, 0:n], func=AF.Ln), reads=[t2], writes=[t2])
                b.op("act", lambda e: e.activation(out=t2[:, 0:n], in_=t2[:, 0:n], func=AF.Exp, scale=-0.5), reads=[t2], writes=[t2])
                b.op("dve", lambda e: e.tensor_tensor(out=t1[:, 0:n], in0=t1[:, 0:n], in1=t2[:, 0:n], op=ALU.mult), reads=[t1, t2], writes=[t1])
                b.op("dve", lambda e: e.tensor_tensor(out=t2[:, 0:n], in0=t1[:, 0:n], in1=t4[:, 0:n], op=ALU.mult), reads=[t1, t4], writes=[t2])
                b.dma("sp", sBb[cs, t0:t0 + n], t2[:, 0:n], reads=[t2], writes=[sBb.rs[ti]])
                b.op("act", lambda e: e.mul(out=t3[:, 0:n], in_=t1[:, 0:n], mul=-1.0), reads=[t1], writes=[t3])
                b.dma("sp", sA[cs, t0:t0 + n], t3[:, 0:n], reads=[t3], writes=[sA.rs[ti]])
                b.op("dve", lambda e: e.tensor_scalar(out=t4[:, 0:n], in0=t4[:, 0:n], scalar1=-1.0, scalar2=evp[:, 3, cc_:cc_ + 1], op0=ALU.add, op1=ALU.mult), reads=[t4, evp], writes=[t4])
                b.op("dve", lambda e: e.scalar_tensor_tensor(out=t1[:, 0:n], in0=t4[:, 0:n], scalar=1.0, in1=kmix[:, cc_, 0:n], op0=ALU.add, op1=ALU.mult), reads=[t4, kmix], writes=[t1])
                b.dma("sp", sK2[cs, t0:t0 + n], t1[:, 0:n], reads=[t1], writes=[sK2.rs[ti]])
        b.pop()

    TBM = 32
    VB = 8

    def scan(e_, ctx):
        if ctx == 0 and not DO_PROMPT[0]:
            return
        b.push()
        stA = b.sb("stA", [64, 16, 64])
        Ssb = b.sb("Ssb", [128, 8, 64]); stmp = b.sb("stmp", [128, 8, 64]); stm2 = b.sb("stm2", [128, 8, 64])
        blkv2 = [{k: b.sb("blk_" + k, [128, 8, TBM]) for k in ("r", "w", "k", "a", "b")} for _ in range(2)]
        R22 = [b.sb("R2", [128, 8, TBM, 2]) for _ in range(2)]
        vrow2 = [b.sb("vrow", [2, VB, 512]) for _ in range(2)]
        osb = b.sb("osb", [64, 8, 2, TBM])
        T0, T_, TB = (0, TP, TBM) if ctx == 0 else (TP, TS, TS)
        if ctx == 0:
            b.op("dve", lambda e: e.memset(Ssb[:], 0.0), writes=[Ssb])
        else:
            b.dma("sp", stA[:], st_wkv[e_].rearrange("h v k -> v h k"), writes=[stA])
            for c_ in range(8):
                b.op("pe", lambda e: e.transpose(pst[6][:, 0:64], stA[:, 2 * c_:2 * c_ + 2, :].rearrange("v j k -> v (j k)"), idf[0:64, 0:64]), reads=[stA, idf], writes=[pst[6]])
                b.op("dve", lambda e: e.tensor_copy(out=Ssb[:, c_, :], in_=pst[6][:, 0:64]), reads=[pst[6]], writes=[Ssb])
        srcs = {"r": sR, "w": sW, "k": sK2, "a": sA, "b": sBb}
        nblk = T_ // TB
        nsub = (TB + VB - 1) // VB

        def issue_block_loads(bi):
            t0 = T0 + bi * TB
            ti = t0 // 512
            blkv = blkv2[bi % 2]
            for k_, s_ in srcs.items():
                b.dma("sp", blkv[k_][:, :, 0:TB], s_[:, t0:t0 + TB].rearrange("(c p) t -> p c t", p=128), reads=[s_.rs[ti]], writes=[blkv[k_]])
            for j in range(2):
                b.op("pool", lambda e: e.tensor_scalar(out=R22[bi % 2][:, :, 0:TB, j], in0=blkv["r"][:, :, 0:TB], scalar1=maskj[:, j:j + 1], scalar2=None, op0=ALU.mult), reads=[blkv["r"], maskj], writes=[R22[bi % 2]])

        def issue_vrow(su):
            bi, sb_ = divmod(su, nsub)
            t = sb_ * VB
            tg = T0 + bi * TB + t
            nv = min(VB, TB - t)
            vr = vrow2[su % 2]
            for j in range(2):
                b.dma("sp", vr[j:j + 1, 0:nv, :].rearrange("o t (c v) -> o t c v", v=64), sVtm[tg:tg + nv, :].rearrange("t (c j v) -> j t c v", j=2, v=64)[j:j + 1], reads=[sVtm.rs[tg // 512]], writes=[vr])

        issue_block_loads(0)
        issue_vrow(0)
        for bi in range(nblk):
            t0 = T0 + bi * TB
            ti = t0 // 512
            blkv = blkv2[bi % 2]
            R2 = R22[bi % 2]
            if bi + 1 < nblk:
                issue_block_loads(bi + 1)
            ops_ = pst[2]
            opv = ops_[0:64, 0:8 * TB * 2].rearrange("p (c t j) -> p c t j", c=8, j=2)
            for t in range(TB):
                su = bi * nsub + t // VB
                vrow = vrow2[su % 2]
                if t % VB == 0 and su + 1 < nblk * nsub:
                    issue_vrow(su + 1)
                bc = lambda k_: blkv[k_][:, :, t:t + 1].to_broadcast([128, 8, 64])
                b.op("dve", lambda e: e.tensor_tensor(out=stmp[:], in0=Ssb[:], in1=bc("a"), op=ALU.mult), reads=[Ssb, blkv["a"]], writes=[stmp])
                b.op("pe", lambda e: e.matmul(pst[0][:, 0:512], lhsT=blk1[:], rhs=stmp[:].rearrange("p c v -> p (c v)"), start=True, stop=True), reads=[blk1, stmp], writes=[pst[0]])
                pv = pst[4 + t % 2]
                b.op("pe", lambda e: e.matmul(pv[:, 0:512], lhsT=sel[:], rhs=vrow[:, t % VB, :], start=True, stop=True), reads=[sel, vrow], writes=[pv])
                b.op("dve", lambda e: e.tensor_tensor(out=Ssb[:], in0=Ssb[:], in1=bc("w"), op=ALU.mult), reads=[Ssb, blkv["w"]], writes=[Ssb])
                b.op("dve", lambda e: e.tensor_tensor(out=stm2[:], in0=pv[:, 0:512].rearrange("p (c v) -> p c v", v=64), in1=bc("k"), op=ALU.mult), reads=[pv, blkv["k"]], writes=[stm2])
                b.op("dve", lambda e: e.tensor_tensor(out=Ssb[:], in0=Ssb[:], in1=stm2[:], op=ALU.add), reads=[Ssb, stm2], writes=[Ssb])
                b.op("dve", lambda e: e.tensor_tensor(out=stm2[:], in0=pst[0][:, 0:512].rearrange("p (c v) -> p c v", v=64), in1=bc("b"), op=ALU.mult), reads=[pst[0], blkv["b"]], writes=[stm2])
                b.op("dve", lambda e: e.tensor_tensor(out=Ssb[:], in0=Ssb[:], in1=stm2[:], op=ALU.add), reads=[Ssb, stm2], writes=[Ssb])
                for c_ in range(8):
                    b.op("pe", lambda e: e.matmul(opv[:, c_, t, :], lhsT=Ssb[:, c_, :], rhs=R2[:, c_, t, :], start=True, stop=True), reads=[Ssb, R2], writes=[ops_])
            for j in range(2):
                b.op("act", lambda e: e.copy(out=osb[:, :, j, 0:TB], in_=opv[:, :, 0:TB, j]), reads=[ops_], writes=[osb])
            for j in range(2):
                b.dma("sp", sO[:, t0:t0 + TB].rearrange("(c j v) t -> j v c t", j=2, v=64)[j], osb[:, :, j, 0:TB], reads=[osb], writes=[sO.rs[ti]])
        for c_ in range(8):
            b.op("pe", lambda e: e.transpose(pst[6][0:64, 0:128], Ssb[:, c_, :], idf[:]), reads=[Ssb, idf], writes=[pst[6]])
            b.op("dve", lambda e: e.tensor_copy(out=stA[:, 2 * c_:2 * c_ + 2, :].rearrange("v j k -> v (j k)"), in_=pst[6][0:64, 0:128]), reads=[pst[6]], writes=[stA])
        b.dma("sp", o_wkv[ctx][e_].rearrange("h v k -> v h k"), stA[:], reads=[stA], writes=[o_wkv[ctx]])
        b.pop()

    def even_C(l, e_):
        b.push()
        S['yt'] = b.sb("ytC", [128, NKC, 512]); S['big'] = b.sb("bigC", [128, NKC, 512], BF16)
        upad = b.sb("upad", [128, 8, 542]); ucar = b.sb("ucar", [128, 8, 30]); ust = b.sb("ust", [128, 8, 30])
        ubt = b.sb("ubt", [128, 8, 512])
        for (t0, n, ctx) in TILES:
            ti = t0 // 512
            last = (t0 + n == TP) or ctx == 1
            if t0 == 0:
                b.op("dve", lambda e: e.memset(ucar[:], 0.0), writes=[ucar])
            if ctx == 1:
                for c_ in range(8):
                    b.dma("sp", ucar[:, c_, :], st_convb[e_, :, c_ * 128:(c_ + 1) * 128].rearrange("j p -> p j"), writes=[ucar])
            b.op("dve", lambda e: e.tensor_copy(out=upad[:, :, 0:30], in_=ucar[:]), reads=[ucar], writes=[upad])
            b.dma("sp", upad[:, :, 30:30 + n], sU[:, t0:t0 + n].rearrange("(c p) t -> p c t", p=128), reads=[sU.rs[ti]], writes=[upad])
            b.op("dve", lambda e: e.tensor_copy(out=ucar[:], in_=upad[:, :, n:n + 30]), reads=[upad], writes=[ucar])
            if last:
                b.op("dve", lambda e: e.tensor_copy(out=ust[:], in_=upad[:, :, n:n + 30]), reads=[upad], writes=[ust])
                for c_ in range(8):
                    b.dma("sp", o_convb[ctx][e_, :, c_ * 128:(c_ + 1) * 128].rearrange("j p -> p j"), ust[:, c_, :], reads=[ust], writes=[o_convb[ctx]])
            for cc_ in range(8):
                cs = slice(cc_ * 128, (cc_ + 1) * 128)
                b.dma("sp", t1[:, 0:n], sO[cs, t0:t0 + n], reads=[sO.rs[ti]], writes=[t1])
                ps = pst[0]
                b.op("pe", lambda e: e.matmul(ps[:, 0:n], lhsT=blk1[:], rhs=t1[:, 0:n], start=True, stop=True), reads=[blk1, t1], writes=[ps])
                b.op("dve", lambda e: e.scalar_tensor_tensor(out=t1[:, 0:n], in0=ps[:, 0:n], scalar=-1.0 / 64, in1=t1[:, 0:n], op0=ALU.mult, op1=ALU.add), reads=[ps, t1], writes=[t1])
                b.op("dve", lambda e: e.tensor_tensor(out=t2[:, 0:n], in0=t1[:, 0:n], in1=t1[:, 0:n], op=ALU.mult), reads=[t1], writes=[t2])
                ps = pst[1]
                b.op("pe", lambda e: e.matmul(ps[:, 0:n], lhsT=blk1[:], rhs=t2[:, 0:n], start=True, stop=True), reads=[blk1, t2], writes=[ps])
                b.op("act", lambda e: e.activation(out=t2[:, 0:n], in_=ps[:, 0:n], func=AF.Ln, bias=64e-5, scale=1.0 / 64), reads=[ps], writes=[t2])
                b.op("act", lambda e: e.activation(out=t2[:, 0:n], in_=t2[:, 0:n], func=AF.Exp, scale=-0.5), reads=[t2], writes=[t2])
                b.op("dve", lambda e: e.tensor_tensor(out=t1[:, 0:n], in0=t1[:, 0:n], in1=t2[:, 0:n], op=ALU.mult), reads=[t1, t2], writes=[t1])
                b.op("dve", lambda e: e.tensor_scalar(out=t1[:, 0:n], in0=t1[:, 0:n], scalar1=evp[:, 5, cc_:cc_ + 1], scalar2=evp[:, 6, cc_:cc_ + 1], op0=ALU.mult, op1=ALU.add), reads=[t1, evp], writes=[t1])
                b.dma("sp", t2[:, 0:n], sR[cs, t0:t0 + n], reads=[sR.rs[ti]], writes=[t2])
                b.dma("sp", t3[:, 0:n], sK2[cs, t0:t0 + n], reads=[sK2.rs[ti]], writes=[t3])
                b.op("dve", lambda e: e.scalar_tensor_tensor(out=t2[:, 0:n], in0=t2[:, 0:n], scalar=evp[:, 4, cc_:cc_ + 1], in1=t3[:, 0:n], op0=ALU.mult, op1=ALU.mult), reads=[t2, t3, evp], writes=[t2])
                ps = pst[2]
                b.op("pe", lambda e: e.matmul(ps[:, 0:n], lhsT=blk1[:], rhs=t2[:, 0:n], start=True, stop=True), reads=[blk1, t2], writes=[ps])
                b.dma("sp", t3[:, 0:n], sV[cs, t0:t0 + n], reads=[sV.rs[ti]], writes=[t3])
                b.op("dve", lambda e: e.tensor_tensor(out=t3[:, 0:n], in0=t3[:, 0:n], in1=ps[:, 0:n], op=ALU.mult), reads=[t3, ps], writes=[t3])
                b.op("dve", lambda e: e.tensor_tensor(out=t1[:, 0:n], in0=t1[:, 0:n], in1=t3[:, 0:n], op=ALU.add), reads=[t1, t3], writes=[t1])
                b.dma("sp", t4[:, 0:n], sG[cs, t0:t0 + n], reads=[sG.rs[ti]], writes=[t4])
                b.op("dve", lambda e: e.tensor_tensor(out=S['big'][:, cc_, 0:n], in0=t1[:, 0:n], in1=t4[:, 0:n], op=ALU.mult), reads=[t1, t4], writes=[S['big']])
                b.op("pool", lambda e: e.tensor_scalar(out=ubt[:, cc_, 0:n], in0=upad[:, cc_, 0:n], scalar1=cw[:, 0, cc_:cc_ + 1], scalar2=evp[:, 7, cc_:cc_ + 1], op0=ALU.mult, op1=ALU.add), reads=[upad, cw, evp], writes=[ubt])
                for j in range(1, 31):
                    b.op("dve", lambda e: e.scalar_tensor_tensor(out=ubt[:, cc_, 0:n], in0=upad[:, cc_, j:j + n], scalar=cw[:, j, cc_:cc_ + 1], in1=ubt[:, cc_, 0:n], op0=ALU.mult, op1=ALU.add), reads=[upad, cw, ubt], writes=[ubt])
            ps = pst[3]
            for cc_ in range(8):
                b.op("pe", lambda e: e.matmul(ps[:, 0:n], lhsT=onesf[:], rhs=ubt[:, cc_, 0:n], start=(cc_ == 0), stop=(cc_ == 7)), reads=[onesf, ubt], writes=[ps])
            b.op("act", lambda e: e.mul(out=t4[:, 0:n], in_=ps[:, 0:n], mul=-1.0 / 1024), reads=[ps], writes=[t4])
            for cc_ in range(8):
                b.op("dve", lambda e: e.tensor_tensor(out=ubt[:, cc_, 0:n], in0=ubt[:, cc_, 0:n], in1=t4[:, 0:n], op=ALU.add), reads=[ubt, t4], writes=[ubt])
            sumsq_rstd(ubt, n, 8, 1024, 1e-5, ubt)
            for cc_ in range(8):
                b.op("dve", lambda e: e.tensor_tensor(out=t2[:, 0:n], in0=ubt[:, cc_, 0:n], in1=rstd[:, 0:n], op=ALU.mult), reads=[ubt, rstd], writes=[t2])
                b.op("act", lambda e: e.activation(out=S['big'][:, 8 + cc_, 0:n], in_=t2[:, 0:n], func=AF.Silu, bias=evp[:, 9, cc_:cc_ + 1], scale=evp[:, 8, cc_:cc_ + 1]), reads=[t2, evp], writes=[S['big']])
            out_proj(ab_w_out[e_], NKC, lambda kc: S['big'][:, kc, 0:n], n, [S['big']])
            post_norm_residual(l, 0, ti, t0, n, ctx)
        b.pop()

    b.even = (even_A, scan, even_C)

    def odd_E(l, o_):
        b.push()
        qst = b.sb("qst", [128, 4, 512], BF16); ktm = b.sb("ktm", [128, 512]); vtb = b.sb("vtb", [128, 512], BF16)
        for (t0, n, ctx) in TILES:
            ti = t0 // 512
            pre_norm(l, 0, ti, t0, n, ctx)
            for gq in range(12):
                def ev(mc, ps, gq=gq):
                    b.op("act" if mc % 2 else "dve", lambda e: (e.copy if mc % 2 else e.tensor_copy)(out=qst[:, mc, 0:n], in_=ps[:, 0:n]), reads=[ps], writes=[qst])
                mm_group(attn_w_qkv[o_, :, gq * 512:(gq + 1) * 512], NKC, 512, lambda kc: hx[:, kc, 2:n + 2], n, ev, [hx])
                dstT = sQT if gq < 6 else sKT
                r0 = (gq % 6) * 512
                b.dma("sp", dstT[r0:r0 + 512, t0:t0 + n].rearrange("(c p) t -> p c t", p=128), qst[:, :, 0:n], reads=[qst], writes=[dstT.rs[ti]])
            for gi in range(12):
                kv = gi // 6
                g = (gi % 6) // 2
                hh = gi % 2
                keep0 = TP - WINS[g] if ctx == 0 else TP
                if kv == 0 and t0 + n <= keep0:
                    continue
                wt_, wv = load_w(attn_w_qkv[o_, :, 3072 + gi * 512:3072 + (gi + 1) * 512], NKC, 512)
                for s_ in range((n + 127) // 128):
                    m = min(128, n - s_ * 128)
                    if kv == 0 and t0 + s_ * 128 < keep0:
                        continue
                    ps = pst[s_ % 4]
                    for kc in range(NKC):
                        b.op("pe", lambda e: e.matmul(ps[0:m, 0:512], lhsT=hx[:, kc, 2 + s_ * 128:2 + s_ * 128 + m], rhs=wv[:, kc, :], start=(kc == 0), stop=(kc == NKC - 1)), reads=[wt_, hx], writes=[ps])
                    tok = t0 + s_ * 128
                    if tok >= keep0:
                        b.op("act", lambda e: e.copy(out=ktm[0:m, :], in_=ps[0:m, 0:512]), reads=[ps], writes=[ktm])
                        b.dma("sp", o_kv[ctx][g][o_, kv, tok - keep0:tok - keep0 + m, hh * 512:(hh + 1) * 512], ktm[0:m, :], reads=[ktm], writes=[o_kv[ctx][g]])
                    if kv == 1:
                        b.op("dve", lambda e: e.tensor_copy(out=vtb[0:m, :], in_=ps[0:m, 0:512]), reads=[ps], writes=[vtb])
                        b.dma("sp", sVa[tok:tok + m, (gi - 6) * 512:(gi - 5) * 512], vtb[0:m, :], reads=[vtb], writes=[sVa.rs[ti]])
        b.pop()

    def odd_F(l, o_):
        b.push()
        numacc = b.sb("numacc", [128, TP]); denacc = b.sb("denacc", [128, TP])
        qT = b.sb("qT", [128, TP], BF16); kT = b.sb("kT", [128, TP], BF16)
        qd = b.sb("qd", [128, TP], BF16); kd = b.sb("kd", [128, TP], BF16)
        vt = b.sb("vt", [128, 32, 128], BF16); ot = b.sb("ot", [128, TP], BF16)
        pT = [b.sb(f"pT{i}", [128, 256], BF16) for i in range(2)]
        allres = lambda T_: T_.rs[0:8]
        for h in (range(8) if DO_PROMPT[0] else ()):
            for g in range(3):
                d = DILS[g]
                nb = TP // d // 128
                ch = g * 8 + h
                b.dma("sp", qT[:], sQT[ch * 128:(ch + 1) * 128, 0:TP], reads=allres(sQT), writes=[qT])
                b.dma("sp", kT[:], sKT[ch * 128:(ch + 1) * 128, 0:TP], reads=allres(sKT), writes=[kT])
                if d == 1:
                    qq, kk_ = qT, kT
                else:
                    b.op("pool", lambda e: e.tensor_copy(out=qd[:].rearrange("p (dd i) -> p dd i", dd=d), in_=qT[:].rearrange("p (i dd) -> p dd i", dd=d)), reads=[qT], writes=[qd])
                    b.op("dve", lambda e: e.tensor_copy(out=kd[:].rearrange("p (dd i) -> p dd i", dd=d), in_=kT[:].rearrange("p (i dd) -> p dd i", dd=d)), reads=[kT], writes=[kd])
                    qq, kk_ = qd, kd
                vsrc = sVa[0:TP, ch * 128:(ch + 1) * 128].rearrange("(jt m dd) c -> dd m jt c", m=128, dd=d)
                for rho in range(d):
                    b.dma("sp", vt[:, rho * nb:(rho + 1) * nb, :], vsrc[rho], reads=allres(sVa), writes=[vt])
                for rho in range(d):
                    for blk in range(nb):
                        bi = rho * nb + blk
                        c0 = bi * 128
                        ps_s, ps_n, ps_d, pt = pst[bi % 2], pst[2 + bi % 2], pst[4 + bi % 2], pT[bi % 2]
                        b.op("pe", lambda e: e.matmul(ps_s[:, 0:128], lhsT=kk_[:, c0:c0 + 128], rhs=qq[:, c0:c0 + 128], start=True, stop=True), reads=[kk_, qq], writes=[ps_s])
                        ncol = 128
                        if blk > 0:
                            b.op("pe", lambda e: e.matmul(ps_s[:, 128:256], lhsT=kk_[:, c0 - 128:c0], rhs=qq[:, c0:c0 + 128], start=True, stop=True), reads=[kk_, qq], writes=[ps_s])
                            ncol = 256
                        b.op("act", lambda e: e.activation(out=pt[:, 0:ncol], in_=ps_s[:, 0:ncol], func=AF.Exp, scale=C_SCALE), reads=[ps_s], writes=[pt])
                        b.op("pool", lambda e: e.tensor_tensor(out=pt[:, 0:ncol], in0=pt[:, 0:ncol], in1=mask2[:, 0:ncol], op=ALU.mult), reads=[pt, mask2], writes=[pt])
                        b.op("pe", lambda e: e.matmul(ps_n[:, 0:128], lhsT=vt[:, bi, :], rhs=pt[:, 0:128], start=True, stop=(blk == 0)), reads=[vt, pt], writes=[ps_n])
                        if blk > 0:
                            b.op("pe", lambda e: e.matmul(ps_n[:, 0:128], lhsT=vt[:, bi - 1, :], rhs=pt[:, 128:256], start=False, stop=True), reads=[vt, pt], writes=[ps_n])
                        b.op("pe", lambda e: e.matmul(ps_d[:, 0:128], lhsT=onesb[:], rhs=pt[:, 0:128], start=True, stop=(blk == 0)), reads=[onesb, pt], writes=[ps_d])
                        if blk > 0:
                            b.op("pe", lambda e: e.matmul(ps_d[:, 0:128], lhsT=onesb[:], rhs=pt[:, 128:256], start=False, stop=True), reads=[onesb, pt], writes=[ps_d])
                        st = rho + d * blk * 128
                        sl = slice(st, st + d * 127 + 1, d)
                        if g == 0:
                            b.op("act", lambda e: e.copy(out=numacc[:, sl], in_=ps_n[:, 0:128]), reads=[ps_n], writes=[numacc])
                            b.op("act", lambda e: e.copy(out=denacc[:, sl], in_=ps_d[:, 0:128]), reads=[ps_d], writes=[denacc])
                        else:
                            b.op("dve", lambda e: e.tensor_tensor(out=numacc[:, sl], in0=numacc[:, sl], in1=ps_n[:, 0:128], op=ALU.add), reads=[ps_n, numacc], writes=[numacc])
                            b.op("dve", lambda e: e.tensor_tensor(out=denacc[:, sl], in0=denacc[:, sl], in1=ps_d[:, 0:128], op=ALU.add), reads=[ps_d, denacc], writes=[denacc])
            b.op("dve", lambda e: e.reciprocal(out=denacc[:], in_=denacc[:]), reads=[denacc], writes=[denacc])
            b.op("dve", lambda e: e.tensor_tensor(out=ot[:], in0=numacc[:], in1=denacc[:], op=ALU.mult), reads=[numacc, denacc], writes=[ot])
            b.dma("sp", sOT[h * 128:(h + 1) * 128, 0:TP], ot[:], reads=[ot], writes=allres(sOT))
        b.pop()
        b.push()
        kcf = b.sb("kcf", [128, 1024]); vcf = b.sb("vcf", [128, 1024]); vcb = b.sb("vcb", [128, 1024], BF16)
        kTs = b.sb("kTs", [128, 128], BF16); pS = b.sb("pS", [128, 4], BF16)
        qs4 = b.sb("qs4", [128, 24, 4], BF16); kn4 = b.sb("kn4", [128, 24, 4], BF16); vn4 = b.sb("vn4", [4, 3072], BF16)
        nums = b.sb("nums", [128, 8, 4]); dens = b.sb("dens", [128, 8, 4]); os4 = b.sb("os4", [128, 8, 4], BF16)
        b.dma("sp", qs4[:], sQT[:, TP:NT].rearrange("(c p) t -> p c t", p=128), reads=[sQT.rs[8]], writes=[qs4])
        b.dma("sp", kn4[:], sKT[:, TP:NT].rearrange("(c p) t -> p c t", p=128), reads=[sKT.rs[8]], writes=[kn4])
        b.dma("sp", vn4[:], sVa[TP:NT, :], reads=[sVa.rs[8]], writes=[vn4])
        colsel = b.sb("colsel", [128, 4, 4], BF16)
        b.op("dve", lambda e: e.memset(colsel[:], 0.0), writes=[colsel])
        for tt_ in range(4):
            b.op("dve", lambda e: e.memset(colsel[:, tt_, tt_:tt_ + 1], 1.0), reads=[colsel], writes=[colsel])
        b.op("dve", lambda e: e.memset(nums[:], 0.0), writes=[nums])
        b.op("dve", lambda e: e.memset(dens[:], 0.0), writes=[dens])

        def accum(h, cols, lhs_v, lhs_one, p_ap, krows):
            b.op("pe", lambda e: e.matmul(pst[2][:, 0:len(cols)], lhsT=lhs_v, rhs=p_ap, start=True, stop=True), reads=[vcb, vn4, pS], writes=[pst[2]])
            b.op("pe", lambda e: e.matmul(pst[3][:, 0:len(cols)], lhsT=lhs_one, rhs=p_ap, start=True, stop=True), reads=[onesb, pS], writes=[pst[3]])
            c0, c1 = cols[0], cols[-1] + 1
            b.op("dve", lambda e: e.tensor_tensor(out=nums[:, h, c0:c1], in0=nums[:, h, c0:c1], in1=pst[2][:, 0:len(cols)], op=ALU.add), reads=[nums, pst[2]], writes=[nums])
            b.op("dve", lambda e: e.tensor_tensor(out=dens[:, h, c0:c1], in0=dens[:, h, c0:c1], in1=pst[3][:, 0:len(cols)], op=ALU.add), reads=[dens, pst[3]], writes=[dens])

        for g in range(3):
            d = DILS[g]
            ck = cks[g]
            tiles_ = [(None, [0, 1, 2, 3])] if d == 1 else [(tt, [tt]) for tt in range(4)]
            for tt, cols in tiles_:
                ksrc = ck[o_, 0, :, :] if d == 1 else ck[o_, 0, :, :].rearrange("(i dd) c -> dd i c", dd=d)[tt]
                vsrc = ck[o_, 1, :, :] if d == 1 else ck[o_, 1, :, :].rearrange("(i dd) c -> dd i c", dd=d)[tt]
                b.dma("sp", kcf[:], ksrc, writes=[kcf])
                b.dma("sp", vcf[:], vsrc, writes=[vcf])
                b.op("pool", lambda e: e.tensor_copy(out=vcb[:], in_=vcf[:]), reads=[vcf], writes=[vcb])
                for h in range(8):
                    ch = g * 8 + h
                    b.op("pe", lambda e: e.transpose(pst[0][:, 0:128], kcf[:, h * 128:(h + 1) * 128], idf[:]), reads=[kcf, idf], writes=[pst[0]])
                    b.op("act", lambda e: e.copy(out=kTs[:], in_=pst[0][:, 0:128]), reads=[pst[0]], writes=[kTs])
                    b.op("pe", lambda e: e.matmul(pst[1][:, 0:4], lhsT=kTs[:], rhs=qs4[:, ch, :], start=True, stop=True), reads=[kTs, qs4], writes=[pst[1]])
                    b.op("act", lambda e: e.activation(out=pS[:, 0:4], in_=pst[1][:, 0:4], func=AF.Exp, scale=C_SCALE), reads=[pst[1]], writes=[pS])
                    msk_ = mask2[:, 128:132] if d == 1 else colsel[:, tt, :]
                    b.op("dve", lambda e: e.tensor_tensor(out=pS[:, 0:4], in0=pS[:, 0:4], in1=msk_, op=ALU.mult), reads=[pS, mask2, colsel], writes=[pS])
                    accum(h, [0, 1, 2, 3], vcb[:, h * 128:(h + 1) * 128], onesb[:], pS[:, 0:4], 128)
            for h in range(8):
                ch = g * 8 + h
                b.op("pe", lambda e: e.matmul(pst[1][0:4, 0:4], lhsT=kn4[:, ch, :], rhs=qs4[:, ch, :], start=True, stop=True), reads=[kn4, qs4], writes=[pst[1]])
                b.op("act", lambda e: e.activation(out=pS[0:4, 0:4], in_=pst[1][0:4, 0:4], func=AF.Exp, scale=C_SCALE), reads=[pst[1]], writes=[pS])
                msk = mask2[0:4, 0:4] if d == 1 else idb[0:4, 0:4]
                b.op("dve", lambda e: e.tensor_tensor(out=pS[0:4, 0:4], in0=pS[0:4, 0:4], in1=msk, op=ALU.mult), reads=[pS, mask2, idb], writes=[pS])
                accum(h, [0, 1, 2, 3], vn4[0:4, ch * 128:(ch + 1) * 128], onesb[0:4, :], pS[0:4, 0:4], 4)
        b.op("dve", lambda e: e.reciprocal(out=dens[:], in_=dens[:]), reads=[dens], writes=[dens])
        b.op("dve", lambda e: e.tensor_tensor(out=os4[:], in0=nums[:], in1=dens[:], op=ALU.mult), reads=[nums, dens], writes=[os4])
        b.dma("sp", sOT[:, TP:NT].rearrange("(h p) t -> p h t", p=128), os4[:], reads=[os4], writes=[sOT.rs[8]])
        b.pop()

    def odd_G(l, o_):
        b.push()
        S['yt'] = b.sb("ytG", [128, NKC, 512]); ot8 = b.sb("ot8", [128, 8, 512], BF16)
        for (t0, n, ctx) in TILES:
            ti = t0 // 512
            b.dma("sp", ot8[:, :, 0:n], sOT[:, t0:t0 + n].rearrange("(h p) t -> p h t", p=128), reads=[sOT.rs[ti]], writes=[ot8])
            out_proj(attn_w_out[o_], 8, lambda kc: ot8[:, kc, 0:n], n, [ot8])
            post_norm_residual(l, 0, ti, t0, n, ctx)
        b.pop()

    def final_out():
        b.push()
        xo = b.sb("xo", [128, D])
        for (t0, n, ctx) in TILES:
            ti = t0 // 512
            b.dma("sp", xt[:, :, 0:n], X[:, t0:t0 + n].rearrange("(k p) t -> p k t", p=128), reads=[X.rs[ti]], writes=[xt])
            for s_ in range((n + 127) // 128):
                m = min(128, n - s_ * 128)
                for kc in range(NKC):
                    ps = pst[kc % 4]
                    b.op("pe", lambda e: e.transpose(ps[0:m, 0:128], xt[:, kc, s_ * 128:s_ * 128 + m], idf[:]), reads=[xt, idf], writes=[ps])
                    b.op("act" if kc % 2 else "dve", lambda e: (e.copy if kc % 2 else e.tensor_copy)(out=xo[0:m, kc * 128:(kc + 1) * 128], in_=ps[0:m, 0:128]), reads=[ps], writes=[xo])
                if ctx == 0:
                    b.dma("sp", yp[t0 + s_ * 128:t0 + s_ * 128 + m, :], xo[0:m, :], reads=[xo], writes=[yp.rs[ti]])
                else:
                    b.dma("sp", ys[0:m, :], xo[0:m, :], reads=[xo], writes=[ys])
        b.pop()

    b.odd = (odd_E, odd_F, odd_G)
    b.final_out = final_out
    b.ffn_layer = ffn_layer
    b.ctx_objs = dict(locals())
    return b


def _finish(b):
    o = b.ctx_objs
    b.finish(o["all_outs"])
    o["nc_cm"].__exit__(None, None, None)
    b.close()
    return b.nc


_CACHE = {}


def build_full(stop=None):
    b = build_program()
    ea, sc, ec = b.even
    oe, of, og = b.odd
    stages = []
    for l in range(4):
        if l % 2 == 0:
            stages += [lambda l=l: ea(l, l // 2), lambda l=l: sc(l // 2, 0), lambda l=l: sc(l // 2, 1), lambda l=l: ec(l, l // 2)]
        else:
            stages += [lambda l=l: oe(l, l // 2), lambda l=l: of(l, l // 2), lambda l=l: og(l, l // 2)]
        stages.append(lambda l=l: b.ffn_layer(l))
    stages.append(b.final_out)
    for i, st in enumerate(stages):
        if stop is not None and i >= stop:
            break
        st()
    return _finish(b)


def _get_nc():
    if "nc" not in _CACHE:
        _CACHE["nc"] = build_full()
    return _CACHE["nc"]


def make_in_maps(inp):
    f = lambda a: np.ascontiguousarray(np.asarray(a, dtype=np.float32))
    wnames = ["w_mod", "b_mod", "g_pre_mix", "g_post_mix", "g_pre_ffn", "g_post_ffn", "ab_w_in", "a_mu_rkv", "a_mu_wag",
              "a_w0", "a_w1", "a_w2", "a_a0", "a_a1", "a_a2", "a_g1", "a_g2", "a_k_k", "a_k_a", "a_ln_w", "a_ln_b",
              "b_conv_w", "b_conv_b", "b_ln_w", "b_ln_b", "ab_w_out", "attn_w_qkv", "attn_w_out", "ffn_w_gate", "ffn_w_up",
              "ffn_conv_w", "ffn_conv_b", "ffn_w_down"]
    shared = {k: f(inp[k]) for k in wnames}
    shared["a_r_k"] = f(inp["a_r_k"]).reshape(2, 1024)
    in_maps = []
    for c in range(8):
        bb = c % 2
        m = dict(shared)
        m["xp"] = f(inp["x_prompt"][bb]); m["xs"] = f(inp["x_sample"][c])
        m["cc"] = f(np.stack([inp["c_prompt"][bb], inp["c_sample"][c]]))
        m["st_shift"] = f(inp["state_shift"][:, c]); m["st_wkv"] = f(inp["state_wkv"][:, c])
        m["st_convb"] = f(inp["state_conv_b"][:, c]); m["st_ffn"] = f(inp["state_ffn"][:, c])
        for w in WINS:
            m[f"ck{w}"] = f(np.asarray(inp[f"cache_kv_w{w}"])[:, :, c].reshape(2, 2, w, 1024))
        in_maps.append(m)
    return in_maps


def kernel(**inp):
    nc = _get_nc()
    in_maps = make_in_maps(inp)
    res = run_bass_kernel_spmd(nc, in_maps, core_ids=list(range(8))).results
    P = lambda k: np.stack([res[0][k], res[1][k]])
    Sx = lambda k: np.stack([res[c][k] for c in range(8)])
    y_prompt = P("yp"); y_sample = Sx("ys")
    outs = [y_prompt, y_sample,
            np.moveaxis(P("p_shift"), 0, 1), np.moveaxis(P("p_wkv"), 0, 1), np.moveaxis(P("p_convb"), 0, 1), np.moveaxis(P("p_ffn"), 0, 1)]
    for w in WINS:
        a = P(f"pkv{w}")
        outs.append(np.transpose(a, (1, 2, 0, 3, 4)).reshape(2, 2, 2, w, 8, 128))
    outs += [np.moveaxis(Sx("s_shift"), 0, 1), np.moveaxis(Sx("s_wkv"), 0, 1), np.moveaxis(Sx("s_convb"), 0, 1), np.moveaxis(Sx("s_ffn"), 0, 1)]
    for w in WINS:
        a = Sx(f"skv{w}")
        outs.append(np.transpose(a, (1, 2, 0, 3, 4)).reshape(2, 2, 8, TS, 8, 128))
    return tuple(np.ascontiguousarray(o, dtype=np.float32) for o in outs)
```

```python
import contextlib
import os
import numpy as np
import concourse.bass as bass
import concourse.mybir as mybir
from concourse.bass_utils import run_bass_kernel_spmd

F32 = mybir.dt.float32
BF16 = mybir.dt.bfloat16
ALU = mybir.AluOpType
AF = mybir.ActivationFunctionType
AX = mybir.AxisListType


class Res:
    __slots__ = ("name", "w", "rd", "excl")

    def __init__(self, name):
        self.name = name
        self.excl = False
        self.w = None
        self.rd = {}


class T:
    def __init__(self, t, name, nres=1):
        self.t = t
        self.name = name
        self.rs = [Res(f"{name}.{i}") for i in range(nres)]

    @property
    def r(self):
        return self.rs[0]

    def __getitem__(self, idx):
        return self.t[idx]


class Eng:
    def __init__(self, b, key, h):
        self.b, self.key, self.h = b, key, h
        self.sem = None
        self.semid = None
        self.cnt = 0
        self.seen = {}
        self.nins = 0


class B:
    EPOCH = 8000
    NSLOT = 12

    def __init__(self):
        self.nc = bass.Bass("TRN2", target_bir_lowering=False)
        self.es = contextlib.ExitStack()
        self.semes = self.es
        self._stk = []
        self.old_latest = {}
        nc = self.nc
        self.E = {k: Eng(self, k, h) for k, h in
                  (("pe", nc.tensor), ("dve", nc.vector), ("act", nc.scalar), ("pool", nc.gpsimd), ("sp", nc.sync))}
        self.sems = {}
        self.nsem = 0
        self.slots = {}
        for q in ("sp", "pool"):
            self.slots[q] = [[self._newsem(f"d{q}{i}"), 0] for i in range(self.NSLOT)]
        self.slot_rr = {"sp": 0, "pool": 0}
        self.uid = 0

    def _newsem(self, name):
        h = self.semes.enter_context(self.nc.semaphore(f"{name}_{self.nsem}"))
        key = self.nsem
        self.sems[key] = h
        self.nsem += 1
        return key

    def sb(self, name, shape, dt=F32, nres=1):
        self.uid += 1
        name = f"{name}_u{self.uid}"
        t = self.es.enter_context(self.nc.sbuf_tensor(name, list(shape), dt))
        return T(t, name, nres)

    def ps(self, name, shape, dt=F32):
        t = self.es.enter_context(self.nc.psum_tensor(name, list(shape), dt))
        tt_ = T(t, name)
        tt_.rs[0].excl = True
        return tt_

    def dram(self, name, shape, dt, kind="Internal", nres=1):
        t = self.nc.dram_tensor(name, list(shape), dt, kind=kind).ap()
        return T(t, name, nres)

    def _needs(self, reads, writes):
        need = {}

        def add(kv):
            if kv is None:
                return
            k, v = kv
            if need.get(k, 0) < v:
                need[k] = v
        for r in reads:
            add(r.w)
            if r.excl:
                for kv in r.rd.items():
                    add(kv)
        for r in writes:
            add(r.w)
            for kv in r.rd.items():
                add(kv)
        return need

    def _emit_waits(self, e, need, skip_self=False):
        for k, v in need.items():
            if skip_self and k == e.semid:
                continue
            if e.seen.get(k, 0) >= v:
                continue
            e.h.wait_ge(self.sems[k], v)
            e.seen[k] = v

    @staticmethod
    def _resl(x):
        out = []
        for a in x:
            if isinstance(a, T):
                out.extend(a.rs)
            elif isinstance(a, Res):
                out.append(a)
            elif a is None:
                pass
            else:
                out.extend(B._resl(a))
        return out

    def op(self, ek, fn, reads=(), writes=()):
        e = self.E[ek]
        reads = self._resl(reads)
        writes = self._resl(writes)
        if e.sem is None or e.cnt >= self.EPOCH:
            if e.semid is not None:
                self.old_latest[e.semid] = e.cnt
            e.semid = self._newsem(f"e{ek}")
            e.sem = self.sems[e.semid]
            e.cnt = 0
        need = self._needs(reads, writes)
        self._emit_waits(e, need, skip_self=(ek == "pe"))
        ins = fn(e.h)
        e.cnt += 1
        e.nins += 1
        ins.then_inc(e.sem, 1)
        kv = (e.semid, e.cnt)
        for r in reads:
            r.rd[e.semid] = e.cnt
        for r in writes:
            r.w = kv
            r.rd = {}
        return ins

    def dma(self, q, out, in_, reads=(), writes=(), **kw):
        e = self.E[q]
        reads = self._resl(reads)
        writes = self._resl(writes)
        need = self._needs(reads, writes)
        i = self.slot_rr[q]
        self.slot_rr[q] = (i + 1) % self.NSLOT
        slot = self.slots[q][i]
        if slot[1] > 0:
            need[slot[0]] = max(need.get(slot[0], 0), 16 * slot[1])
        if slot[1] >= 500:
            self.old_latest[slot[0]] = 16 * slot[1]
            slot[0] = self._newsem(f"d{q}{i}")
            slot[1] = 0
        self._emit_waits(e, need)
        ins = e.h.dma_start(out=out, in_=in_, **kw)
        slot[1] += 1
        e.nins += 1
        ins.then_inc(self.sems[slot[0]], 16)
        kv = (slot[0], 16 * slot[1])
        for r in reads:
            r.rd[slot[0]] = 16 * slot[1]
        for r in writes:
            r.w = kv
            r.rd = {}
        return ins

    def push(self):
        self._stk.append(self.es)
        self.es = contextlib.ExitStack()

    def pop(self):
        self.barrier()
        self.es.close()
        self.es = self._stk.pop()

    def barrier(self):
        latest = {}
        for e in self.E.values():
            if e.semid is not None:
                latest[e.semid] = e.cnt
        for q in self.slots:
            for sl in self.slots[q]:
                if sl[1] > 0:
                    latest[sl[0]] = 16 * sl[1]
        for k, v in self.old_latest.items():
            latest.setdefault(k, v)
        for e in self.E.values():
            self._emit_waits(e, latest, skip_self=False)

    def finish(self, outs):
        e = self.E["sp"]
        need = self._needs(self._resl(outs), [])
        self._emit_waits(e, need)

    def close(self):
        self.es.close()


D = 2048
TP = 4096
TS = 4
NT = TP + TS
NKC = 16
DFF = 5632
NFC = 44
TILES = [(i * 512, 512, 0) for i in range(8)] + [(TP, TS, 1)]
EXPC = 0.6065306597126334
C_SCALE = 128 ** -0.5
WINS = (128, 512, 2048)
DO_PROMPT = [True]
DILS = (1, 4, 16)


def build_program():
    b = B()
    nc = b.nc
    nc_cm = nc.allow_non_contiguous_dma(reason="small param / state layout transforms")
    nc_cm.__enter__()
    I = {}

    def inp(name, shape):
        I[name] = b.dram(name, shape, F32, kind="ExternalInput")
        return I[name]

    def outp(name, shape, nres=1):
        I[name] = b.dram(name, shape, F32, kind="ExternalOutput", nres=nres)
        return I[name]

    xp = inp("xp", [TP, D]); xs = inp("xs", [TS, D]); cc = inp("cc", [2, D])
    st_shift = inp("st_shift", [2, D]); st_wkv = inp("st_wkv", [2, 16, 64, 64])
    st_convb = inp("st_convb", [2, 30, 1024]); st_ffn = inp("st_ffn", [4, 2, DFF])
    cks = [inp(f"ck{w}", [2, 2, w, 1024]) for w in WINS]
    w_mod = inp("w_mod", [4, D, 6 * D]); b_mod = inp("b_mod", [4, 6 * D])
    gpar = {k: inp(k, [4, D]) for k in ("g_pre_mix", "g_post_mix", "g_pre_ffn", "g_post_ffn")}
    ab_w_in = inp("ab_w_in", [2, D, 5120]); a_mu_rkv = inp("a_mu_rkv", [2, 3072]); a_mu_wag = inp("a_mu_wag", [2, 3, D])
    a_w0 = inp("a_w0", [2, 1024]); a_w1 = inp("a_w1", [2, D, 96]); a_w2 = inp("a_w2", [2, 96, 1024])
    a_a0 = inp("a_a0", [2, 1024]); a_a1 = inp("a_a1", [2, D, 96]); a_a2 = inp("a_a2", [2, 96, 1024])
    a_g1 = inp("a_g1", [2, D, 256]); a_g2 = inp("a_g2", [2, 256, 1024])
    a_k_k = inp("a_k_k", [2, 1024]); a_k_a = inp("a_k_a", [2, 1024]); a_r_k = inp("a_r_k", [2, 1024])
    a_ln_w = inp("a_ln_w", [2, 1024]); a_ln_b = inp("a_ln_b", [2, 1024])
    b_conv_w = inp("b_conv_w", [2, 31, 1024]); b_conv_b = inp("b_conv_b", [2, 1024])
    b_ln_w = inp("b_ln_w", [2, 1024]); b_ln_b = inp("b_ln_b", [2, 1024])
    ab_w_out = inp("ab_w_out", [2, D, D])
    attn_w_qkv = inp("attn_w_qkv", [2, D, 9216]); attn_w_out = inp("attn_w_out", [2, 1024, D])
    ffn_w_gate = inp("ffn_w_gate", [4, D, DFF]); ffn_w_up = inp("ffn_w_up", [4, D, DFF])
    ffn_conv_w = inp("ffn_conv_w", [4, 3, DFF]); ffn_conv_b = inp("ffn_conv_b", [4, DFF]); ffn_w_down = inp("ffn_w_down", [4, DFF, D])

    yp = outp("yp", [TP, D], nres=8); ys = outp("ys", [TS, D])
    o_shift = [outp("p_shift", [2, D]), outp("s_shift", [2, D])]
    o_wkv = [outp("p_wkv", [2, 16, 64, 64]), outp("s_wkv", [2, 16, 64, 64])]
    o_convb = [outp("p_convb", [2, 30, 1024]), outp("s_convb", [2, 30, 1024])]
    o_ffn = [outp("p_ffn", [4, 2, DFF]), outp("s_ffn", [4, 2, DFF])]
    o_kv = [[outp(f"pkv{w}", [2, 2, w, 1024]) for w in WINS], [outp(f"skv{w}", [2, 2, TS, 1024]) for w in WINS]]
    all_outs = [yp, ys] + o_shift + o_wkv + o_convb + o_ffn + o_kv[0] + o_kv[1]

    X = b.dram("X", [D, NT], F32, nres=9)
    sR, sK2, sV, sW, sA, sBb, sG, sU, sO = [b.dram(n, [1024, NT], F32, nres=9) for n in
                                          ("sR", "sK2", "sV", "sW", "sA", "sBb", "sG", "sU", "sO")]
    sVtm = b.dram("sVtm", [NT, 1024], F32, nres=9)
    sQT = b.dram("sQT", [3072, NT], BF16, nres=9); sKT = b.dram("sKT", [3072, NT], BF16, nres=9)
    sVa = b.dram("sVa", [NT, 3072], BF16, nres=9); sOT = b.dram("sOT", [1024, NT], BF16, nres=9)

    idf = b.sb("idf", [128, 128]); idb = b.sb("idb", [128, 128], BF16)
    onesf = b.sb("onesf", [128, 128]); onesb = b.sb("onesb", [128, 128], BF16)
    blk1 = b.sb("blk1", [128, 128])
    sel = b.sb("sel", [2, 128]); maskj = b.sb("maskj", [128, 2])
    mask2 = b.sb("mask2", [128, 256], BF16)
    b.op("pool", lambda e: e.memset(idf[:], 1.0), writes=[idf])
    b.op("pool", lambda e: e.affine_select(out=idf[:], in_=idf[:], pattern=[[-1, 128]], compare_op=ALU.is_equal, fill=0.0, base=0, channel_multiplier=1), reads=[idf], writes=[idf])
    b.op("dve", lambda e: e.tensor_copy(out=idb[:], in_=idf[:]), reads=[idf], writes=[idb])
    b.op("pool", lambda e: e.memset(onesf[:], 1.0), writes=[onesf])
    b.op("pool", lambda e: e.memset(onesb[:], 1.0), writes=[onesb])
    b.op("pool", lambda e: e.memset(sel[:], 1.0), writes=[sel])
    b.op("pool", lambda e: e.affine_select(out=sel[:], in_=sel[:], pattern=[[1, 128]], compare_op=ALU.is_ge, fill=0.0, base=0, channel_multiplier=-64), reads=[sel], writes=[sel])
    b.op("pool", lambda e: e.affine_select(out=sel[:], in_=sel[:], pattern=[[-1, 128]], compare_op=ALU.is_ge, fill=0.0, base=63, channel_multiplier=64), reads=[sel], writes=[sel])
    pst = [b.ps(f"pst{i}", [128, 512]) for i in range(8)]
    b.op("pe", lambda e: e.matmul(pst[0][:, 0:128], lhsT=sel[:], rhs=sel[:], start=True, stop=True), reads=[sel], writes=[pst[0]])
    b.op("dve", lambda e: e.tensor_copy(out=blk1[:], in_=pst[0][:, 0:128]), reads=[pst[0]], writes=[blk1])
    b.op("pe", lambda e: e.matmul(pst[1][:, 0:2], lhsT=sel[:], rhs=idf[0:2, 0:2], start=True, stop=True), reads=[sel, idf], writes=[pst[1]])
    b.op("dve", lambda e: e.tensor_copy(out=maskj[:], in_=pst[1][:, 0:2]), reads=[pst[1]], writes=[maskj])
    b.op("pool", lambda e: e.memset(mask2[:], 1.0), writes=[mask2])
    b.op("pool", lambda e: e.affine_select(out=mask2[:, 0:128], in_=mask2[:, 0:128], pattern=[[1, 128]], compare_op=ALU.is_ge, fill=0.0, base=0, channel_multiplier=-1), reads=[mask2], writes=[mask2])
    b.op("pool", lambda e: e.affine_select(out=mask2[:, 128:256], in_=mask2[:, 128:256], pattern=[[-1, 128]], compare_op=ALU.is_ge, fill=0.0, base=0, channel_multiplier=1), reads=[mask2], writes=[mask2])

    xt = b.sb("xt", [128, NKC, 512])
    S = {}
    hx = b.sb("hx", [128, NKC, 514], BF16)
    hf = b.sb("hf", [128, 513])
    wb = [b.sb(f"wb{i}", [128, 8192], BF16) for i in range(2)]
    wsel = [0]
    t1 = b.sb("t1", [128, 512]); t2 = b.sb("t2", [128, 512]); t3 = b.sb("t3", [128, 512]); t4 = b.sb("t4", [128, 512])
    rstd = b.sb("rstd", [128, 512])
    carry_h = b.sb("carry_h", [128, NKC, 1], BF16)
    colf = b.sb("colf", [128, NKC, 1])

    def fmvec(dst_ap, dram_ap_1d, q="sp", reads=(), writes=()):
        b.dma(q, dst_ap, dram_ap_1d.rearrange("(k p) -> p k", p=128), reads=reads, writes=writes)

    def load_w(wap, nk, ncols, krows=128):
        t = wb[wsel[0]]
        wsel[0] ^= 1
        v = t[0:krows, 0:nk * ncols].rearrange("p (k c) -> p k c", c=ncols)
        b.dma("pool", v, wap.rearrange("(k p) c -> p k c", p=krows), writes=[t])
        return t, v

    modv = b.sb("modv", [128, 4, 96, 2]); bmod = b.sb("bmod", [128, 4, 96])
    ccT = b.sb("ccT", [128, NKC, 2], BF16)
    b.push()
    ccf = b.sb("ccf", [2, D])
    b.dma("sp", ccf[:], cc[:, :], writes=[ccf])
    for kc in range(NKC):
        b.op("pe", lambda e: e.transpose(pst[kc % 2][:, 0:2], ccf[0:2, kc * 128:(kc + 1) * 128], idf[0:2, 0:2]), reads=[ccf, idf], writes=[pst[kc % 2]])
        b.op("dve", lambda e: e.tensor_copy(out=ccT[:, kc, :], in_=pst[kc % 2][:, 0:2]), reads=[pst[kc % 2]], writes=[ccT])
    b.pop()
    for l in range(4):
        fmvec(bmod[:, l, :], b_mod[l, :], writes=[bmod])
        for gq in range(24):
            wt_, wv = load_w(w_mod[l, :, gq * 512:(gq + 1) * 512], NKC, 512)
            for mc in range(4):
                ps = pst[(gq * 4 + mc) % 4]
                for kc in range(NKC):
                    b.op("pe", lambda e: e.matmul(ps[:, 0:2], lhsT=wv[:, kc, mc * 128:(mc + 1) * 128], rhs=ccT[:, kc, :], start=(kc == 0), stop=(kc == NKC - 1)), reads=[wt_, ccT], writes=[ps])
                ch = gq * 4 + mc
                b.op("dve", lambda e: e.tensor_scalar(out=modv[:, l, ch, :], in0=ps[:, 0:2], scalar1=bmod[:, l, ch:ch + 1], scalar2=None, op0=ALU.add), reads=[ps, bmod], writes=[modv])

    gv = {k: b.sb("gv_" + k, [128, 4, NKC]) for k in gpar}
    for k in gpar:
        for l in range(4):
            fmvec(gv[k][:, l, :], gpar[k][l, :], writes=[gv[k]])
    gs = b.sb("gs", [128, 4, 2, NKC, 2])
    for l in range(4):
        for wi, (gk, si) in enumerate((("g_pre_mix", 1), ("g_pre_ffn", 4))):
            for ctx in range(2):
                b.op("dve", lambda e: e.tensor_scalar(out=gs[:, l, wi, :, ctx], in0=modv[:, l, si * 16:(si + 1) * 16, ctx], scalar1=1.0, scalar2=None, op0=ALU.add), reads=[modv], writes=[gs])
                b.op("dve", lambda e: e.tensor_tensor(out=gs[:, l, wi, :, ctx], in0=gs[:, l, wi, :, ctx], in1=gv[gk][:, l, :], op=ALU.mult), reads=[gs, gv[gk]], writes=[gs])
    gp = b.sb("gp", [128, 4, 2, NKC, 2])
    for l in range(4):
        for wi, (gk, gi) in enumerate((("g_post_mix", 2), ("g_post_ffn", 5))):
            for ctx in range(2):
                b.op("dve", lambda e: e.tensor_tensor(out=gp[:, l, wi, :, ctx], in0=modv[:, l, gi * 16:(gi + 1) * 16, ctx], in1=gv[gk][:, l, :], op=ALU.mult), reads=[modv, gv[gk]], writes=[gp])

    b.push()
    xin = b.sb("xin", [128, D])
    for (t0, n, ctx) in TILES:
        ti = t0 // 512
        for s in range((n + 127) // 128):
            m = min(128, n - s * 128)
            src = xp[t0 + s * 128:t0 + s * 128 + m, :] if ctx == 0 else xs[0:m, :]
            b.dma("sp", xin[0:m, :], src, writes=[xin])
            for kc in range(NKC):
                ps = pst[kc % 4]
                b.op("pe", lambda e: e.transpose(ps[:, 0:m], xin[0:m, kc * 128:(kc + 1) * 128], idf[0:m, 0:m]), reads=[xin, idf], writes=[ps])
                b.op("act" if kc % 2 else "dve", lambda e: (e.copy if kc % 2 else e.tensor_copy)(out=xt[:, kc, s * 128:s * 128 + m], in_=ps[:, 0:m]), reads=[ps], writes=[xt])
        b.dma("sp", X[:, t0:t0 + n].rearrange("(k p) t -> p k t", p=128), xt[:, :, 0:n], reads=[xt], writes=[X.rs[ti]])

    b.pop()
    def sumsq_rstd(src, n, nch, dim, eps, src_res):
        ps = pst[7]
        for kc in range(nch):
            b.op("act", lambda e: e.activation(out=t1[:, 0:n], in_=src[:, kc, 0:n], func=AF.Square), reads=[src_res], writes=[t1])
            b.op("pe", lambda e: e.matmul(ps[:, 0:n], lhsT=onesf[:], rhs=t1[:, 0:n], start=(kc == 0), stop=(kc == nch - 1)), reads=[onesf, t1], writes=[ps])
        b.op("act", lambda e: e.activation(out=rstd[:, 0:n], in_=ps[:, 0:n], func=AF.Ln, bias=float(eps), scale=1.0 / dim), reads=[ps], writes=[rstd])
        b.op("act", lambda e: e.activation(out=rstd[:, 0:n], in_=rstd[:, 0:n], func=AF.Exp, scale=-0.5), reads=[rstd], writes=[rstd])

    def pre_norm(l, wi, ti, t0, n, ctx, shift_out=None):
        si = 0 if wi == 0 else 3
        b.dma("sp", xt[:, :, 0:n], X[:, t0:t0 + n].rearrange("(k p) t -> p k t", p=128), reads=[X.rs[ti]], writes=[xt])
        sumsq_rstd(xt, n, NKC, D, 1e-6, xt)
        for kc in range(NKC):
            b.op("dve", lambda e: e.tensor_tensor(out=hf[:, 1:n + 1], in0=xt[:, kc, 0:n], in1=rstd[:, 0:n], op=ALU.mult), reads=[xt, rstd], writes=[hf])
            if shift_out is not None:
                b.op("dve", lambda e: e.tensor_scalar(out=colf[:, kc, :], in0=hf[:, n:n + 1], scalar1=gs[:, l, wi, kc, ctx:ctx + 1], scalar2=modv[:, l, si * 16 + kc, ctx:ctx + 1], op0=ALU.mult, op1=ALU.add), reads=[hf, gs, modv], writes=[colf])
            b.op("dve", lambda e: e.tensor_scalar(out=hx[:, kc, 2:n + 2], in0=hf[:, 1:n + 1], scalar1=gs[:, l, wi, kc, ctx:ctx + 1], scalar2=modv[:, l, si * 16 + kc, ctx:ctx + 1], op0=ALU.mult, op1=ALU.add), reads=[hf, gs, modv], writes=[hx])
        if shift_out is not None:
            b.dma("sp", shift_out.rearrange("(k p) -> p k", p=128), colf[:, :, 0], reads=[colf], writes=[shift_out_res[0]])

    shift_out_res = [None]

    def post_norm_residual(l, wi, ti, t0, n, ctx):
        sumsq_rstd(S['yt'], n, NKC, D, 1e-6, S['yt'])
        b.dma("sp", xt[:, :, 0:n], X[:, t0:t0 + n].rearrange("(k p) t -> p k t", p=128), reads=[X.rs[ti]], writes=[xt])
        for kc in range(NKC):
            b.op("dve", lambda e: e.tensor_tensor(out=t2[:, 0:n], in0=S['yt'][:, kc, 0:n], in1=rstd[:, 0:n], op=ALU.mult), reads=[S['yt'], rstd], writes=[t2])
            b.op("dve", lambda e: e.scalar_tensor_tensor(out=xt[:, kc, 0:n], in0=t2[:, 0:n], scalar=gp[:, l, wi, kc, ctx:ctx + 1], in1=xt[:, kc, 0:n], op0=ALU.mult, op1=ALU.add), reads=[t2, gp, xt], writes=[xt])
        b.dma("sp", X[:, t0:t0 + n].rearrange("(k p) t -> p k t", p=128), xt[:, :, 0:n], reads=[xt], writes=[X.rs[ti]])

    def mm_group(wap, nk, ncols, rhs_fn, n, evac, rhs_res, krows=128, ps_base=0):
        wt_, wv = load_w(wap, nk, ncols, krows)
        for mc in range(ncols // 128):
            ps = pst[ps_base + (mc % 4)]
            for kc in range(nk):
                b.op("pe", lambda e: e.matmul(ps[:, 0:n], lhsT=wv[:, kc, mc * 128:(mc + 1) * 128], rhs=rhs_fn(kc), start=(kc == 0), stop=(kc == nk - 1)), reads=[wt_] + list(rhs_res), writes=[ps])
            evac(mc, ps)

    def out_proj(wap, nk, rhs_fn, n, rhs_res):
        for gq in range(4):
            def ev(mc, ps, gq=gq):
                ch = gq * 4 + mc
                b.op("act" if ch % 2 else "dve", lambda e: (e.copy if ch % 2 else e.tensor_copy)(out=S['yt'][:, ch, 0:n], in_=ps[:, 0:n]), reads=[ps], writes=[S['yt']])
            mm_group(wap[:, gq * 512:(gq + 1) * 512], nk, 512, rhs_fn, n, ev, rhs_res)

    def ffn_layer(l):
        b.push()
        S['yt'] = b.sb("yt", [128, NKC, 512]); S['big'] = b.sb("big", [128, NFC, 512], BF16)
        gcar = b.sb("gcar", [128, NFC, 2]); gpad = b.sb("gpad", [128, 514]); gst = b.sb("gst", [128, NFC, 2])
        fcw = b.sb("fcw", [128, 3, NFC]); fcb = b.sb("fcb", [128, NFC])
        sg = b.sb("sg", [128, 4, 512])
        for j in range(3):
            fmvec(fcw[:, j, :], ffn_conv_w[l, j, :], writes=[fcw])
        fmvec(fcb[:, :], ffn_conv_b[l, :], writes=[fcb])
        for (t0, n, ctx) in TILES:
            ti = t0 // 512
            if t0 == 0:
                b.op("dve", lambda e: e.memset(gcar[:], 0.0), writes=[gcar])
            if ctx == 1:
                for j in range(2):
                    fmvec(gcar[:, :, j], st_ffn[l, j, :], writes=[gcar])
            pre_norm(l, 1, ti, t0, n, ctx)
            for gq in range(11):
                def ev_gate(mc, ps, gq=gq):
                    ch = gq * 4 + mc
                    b.op("act", lambda e: e.copy(out=gpad[:, 2:n + 2], in_=ps[:, 0:n]), reads=[ps], writes=[gpad])
                    b.op("dve", lambda e: e.tensor_copy(out=gpad[:, 0:2], in_=gcar[:, ch, :]), reads=[gcar], writes=[gpad])
                    b.op("dve", lambda e: e.tensor_copy(out=gcar[:, ch, :], in_=gpad[:, n:n + 2]), reads=[gpad], writes=[gcar])
                    if t0 + n == TP or ctx == 1:
                        b.op("dve", lambda e: e.tensor_copy(out=gst[:, ch, :], in_=gpad[:, n:n + 2]), reads=[gpad], writes=[gst])
                    b.op("dve", lambda e: e.tensor_scalar(out=t3[:, 0:n], in0=gpad[:, 0:n], scalar1=fcw[:, 0, ch:ch + 1], scalar2=fcb[:, ch:ch + 1], op0=ALU.mult, op1=ALU.add), reads=[gpad, fcw, fcb], writes=[t3])
                    b.op("dve", lambda e: e.scalar_tensor_tensor(out=t3[:, 0:n], in0=gpad[:, 1:n + 1], scalar=fcw[:, 1, ch:ch + 1], in1=t3[:, 0:n], op0=ALU.mult, op1=ALU.add), reads=[gpad, fcw, t3], writes=[t3])
                    b.op("dve", lambda e: e.scalar_tensor_tensor(out=t3[:, 0:n], in0=gpad[:, 2:n + 2], scalar=fcw[:, 2, ch:ch + 1], in1=t3[:, 0:n], op0=ALU.mult, op1=ALU.add), reads=[gpad, fcw, t3], writes=[t3])
                    b.op("act", lambda e: e.activation(out=sg[:, mc, 0:n], in_=t3[:, 0:n], func=AF.Silu), reads=[t3], writes=[sg])

                def ev_up(mc, ps, gq=gq):
                    ch = gq * 4 + mc
                    b.op("dve", lambda e: e.tensor_tensor(out=S['big'][:, ch, 0:n], in0=ps[:, 0:n], in1=sg[:, mc, 0:n], op=ALU.mult), reads=[ps, sg], writes=[S['big']])
                mm_group(ffn_w_gate[l, :, gq * 512:(gq + 1) * 512], NKC, 512, lambda kc: hx[:, kc, 2:n + 2], n, ev_gate, [hx])
                mm_group(ffn_w_up[l, :, gq * 512:(gq + 1) * 512], NKC, 512, lambda kc: hx[:, kc, 2:n + 2], n, ev_up, [hx], ps_base=4)
            if t0 + n == TP or ctx == 1:
                for j in range(2):
                    b.dma("sp", o_ffn[ctx][l, j, :].rearrange("(k p) -> p k", p=128), gst[:, :, j], reads=[gst], writes=[o_ffn[ctx]])
            for ch in range(NKC):
                def ev(mc, ps, ch=ch):
                    b.op("act" if ch % 2 else "dve", lambda e: (e.copy if ch % 2 else e.tensor_copy)(out=S['yt'][:, ch, 0:n], in_=ps[:, 0:n]), reads=[ps], writes=[S['yt']])
                mm_group(ffn_w_down[l, :, ch * 128:(ch + 1) * 128], NFC, 128, lambda kc: S['big'][:, kc, 0:n], n, ev, [S['big']], ps_base=ch % 4)
            post_norm_residual(l, 1, ti, t0, n, ctx)
        b.pop()


    evp = b.sb("evp", [128, 16, 8])
    murkv = b.sb("murkv", [128, 24]); muwag = b.sb("muwag", [128, 3, NKC])
    cw = b.sb("cw", [128, 31, 8])
    def even_A(l, e_):
        b.push()
        S['big'] = b.sb("bigA", [128, NKC, 512], BF16)
        lw1 = b.sb("lw1", [128, NKC, 448], BF16)
        lw2 = b.sb("lw2", [128, 4, 1024], BF16)
        pa = b.sb("pa", [128, 513]); pacar = b.sb("pacar", [128, 24, 1])
        kmix = b.sb("kmix", [128, 8, 512]); sigb = b.sb("sigb", [128, 8, 512])
        lmid = b.sb("lmid", [128, 4, 512], BF16)
        vtm = b.sb("vtm", [128, 4, 1024])
        for i, src in enumerate((a_w0, a_a0, a_k_k, a_k_a, a_r_k, a_ln_w, a_ln_b, b_conv_b, b_ln_w, b_ln_b)):
            fmvec(evp[:, i, :], src[e_, :], writes=[evp])
        fmvec(murkv[:, :], a_mu_rkv[e_, :], writes=[murkv])
        for i in range(3):
            fmvec(muwag[:, i, :], a_mu_wag[e_, i, :], writes=[muwag])
        for j in range(31):
            fmvec(cw[:, j, :], b_conv_w[e_, j, :], writes=[cw])
        b.dma("pool", lw1[:, :, 0:96], a_w1[e_].rearrange("(k p) c -> p k c", p=128), writes=[lw1])
        b.dma("pool", lw1[:, :, 96:192], a_a1[e_].rearrange("(k p) c -> p k c", p=128), writes=[lw1])
        b.dma("pool", lw1[:, :, 192:448], a_g1[e_].rearrange("(k p) c -> p k c", p=128), writes=[lw1])
        b.dma("pool", lw2[0:96, 0, :], a_w2[e_], writes=[lw2])
        b.dma("pool", lw2[0:96, 1, :], a_a2[e_], writes=[lw2])
        b.dma("pool", lw2[:, 2:4, :], a_g2[e_].rearrange("(k p) c -> p k c", p=128), writes=[lw2])
        for (t0, n, ctx) in TILES:
            ti = t0 // 512
            last = (t0 + n == TP) or ctx == 1
            shift_out_res[0] = o_shift[ctx]
            pre_norm(l, 0, ti, t0, n, ctx, shift_out=(o_shift[ctx][e_, :] if last else None))
            if t0 == 0:
                b.op("dve", lambda e: e.memset(carry_h[:], 0.0), writes=[carry_h])
                b.op("dve", lambda e: e.memset(pacar[:], 0.0), writes=[pacar])
            if ctx == 1:
                fmvec(colf[:, :, 0], st_shift[e_, :], writes=[colf])
                b.op("dve", lambda e: e.tensor_copy(out=carry_h[:], in_=colf[:]), reads=[colf], writes=[carry_h])
            b.op("dve", lambda e: e.tensor_copy(out=hx[:, :, 1:2], in_=carry_h[:]), reads=[carry_h], writes=[hx])
            b.op("dve", lambda e: e.tensor_copy(out=carry_h[:], in_=hx[:, :, n + 1:n + 2]), reads=[hx], writes=[carry_h])
            c0 = 0 if ctx == 1 else 1
            for gq in (0, 1, 2, 3, 4, 5, 8, 9, 6, 7):
                def ev(mc, ps, gq=gq):
                    ch = gq * 4 + mc
                    if ch < 24:
                        b.op("act", lambda e: e.copy(out=pa[:, c0:n + 1], in_=ps[:, 0:n + 1 - c0]), reads=[ps], writes=[pa])
                        if c0 == 1:
                            b.op("dve", lambda e: e.tensor_copy(out=pa[:, 0:1], in_=pacar[:, ch, :]), reads=[pacar], writes=[pa])
                        b.op("dve", lambda e: e.tensor_copy(out=pacar[:, ch, :], in_=pa[:, n:n + 1]), reads=[pa], writes=[pacar])
                        b.op("dve", lambda e: e.tensor_tensor(out=t2[:, 0:n], in0=pa[:, 0:n], in1=pa[:, 1:n + 1], op=ALU.subtract), reads=[pa], writes=[t2])
                        sec, cc_ = ch // 8, ch % 8
                        dst = kmix[:, cc_, 0:n] if sec == 1 else t3[:, 0:n]
                        b.op("dve", lambda e: e.scalar_tensor_tensor(out=dst, in0=t2[:, 0:n], scalar=murkv[:, ch:ch + 1], in1=pa[:, 1:n + 1], op0=ALU.mult, op1=ALU.add), reads=[t2, murkv, pa], writes=[kmix if sec == 1 else t3])
                        if sec == 0:
                            b.dma("sp", sR[cc_ * 128:(cc_ + 1) * 128, t0:t0 + n], t3[:, 0:n], reads=[t3], writes=[sR.rs[ti]])
                        if sec == 2:
                            b.dma("sp", sV[cc_ * 128:(cc_ + 1) * 128, t0:t0 + n], t3[:, 0:n], reads=[t3], writes=[sV.rs[ti]])
                            for s_ in range((n + 127) // 128):
                                m = min(128, n - s_ * 128)
                                pq = pst[4 + s_ % 2]
                                b.op("pe", lambda e: e.transpose(pq[0:m, 0:128], t3[:, s_ * 128:s_ * 128 + m], idf[:]), reads=[t3, idf], writes=[pq])
                                b.op("act", lambda e: e.copy(out=vtm[0:m, s_, cc_ * 128:(cc_ + 1) * 128], in_=pq[0:m, 0:128]), reads=[pq], writes=[vtm])
                                if cc_ == 7:
                                    b.dma("sp", sVtm[t0 + s_ * 128:t0 + s_ * 128 + m, :], vtm[0:m, s_, :], reads=[vtm], writes=[sVtm.rs[ti]])
                    elif ch >= 32:
                        b.op("act", lambda e: e.activation(out=sigb[:, ch - 32, 0:n], in_=ps[:, c0 ^ 1:n + (c0 ^ 1)], func=AF.Sigmoid), reads=[ps], writes=[sigb])
                    else:
                        cc_ = ch - 24
                        b.op("dve", lambda e: e.tensor_tensor(out=t3[:, 0:n], in0=ps[:, c0 ^ 1:n + (c0 ^ 1)], in1=sigb[:, cc_, 0:n], op=ALU.mult), reads=[ps, sigb], writes=[t3])
                        b.dma("sp", sU[cc_ * 128:(cc_ + 1) * 128, t0:t0 + n], t3[:, 0:n], reads=[t3], writes=[sU.rs[ti]])
                mm_group(ab_w_in[e_, :, gq * 512:(gq + 1) * 512], NKC, 512, lambda kc: hx[:, kc, c0 + 1:n + 2], n + 1 - c0, ev, [hx])
            for i in range(3):
                for kc in range(NKC):
                    b.op("dve", lambda e: e.tensor_tensor(out=t2[:, 0:n], in0=hx[:, kc, 1:n + 1], in1=hx[:, kc, 2:n + 2], op=ALU.subtract), reads=[hx], writes=[t2])
                    b.op("dve", lambda e: e.scalar_tensor_tensor(out=S['big'][:, kc, 0:n], in0=t2[:, 0:n], scalar=muwag[:, i, kc:kc + 1], in1=hx[:, kc, 2:n + 2], op0=ALU.mult, op1=ALU.add), reads=[t2, muwag, hx], writes=[S['big']])
                if i < 2:
                    ps = pst[4 + i]
                    for kc in range(NKC):
                        b.op("pe", lambda e: e.matmul(ps[0:96, 0:n], lhsT=lw1[:, kc, i * 96:(i + 1) * 96], rhs=S['big'][:, kc, 0:n], start=(kc == 0), stop=(kc == NKC - 1)), reads=[lw1, S['big']], writes=[ps])
                    if i == 0:
                        b.op("act", lambda e: e.activation(out=lmid[0:96, 0, 0:n], in_=ps[0:96, 0:n], func=AF.Tanh), reads=[ps], writes=[lmid])
                    else:
                        b.op("act", lambda e: e.copy(out=lmid[0:96, 1, 0:n], in_=ps[0:96, 0:n]), reads=[ps], writes=[lmid])
                else:
                    for hh in range(2):
                        ps = pst[6 + hh]
                        for kc in range(NKC):
                            b.op("pe", lambda e: e.matmul(ps[:, 0:n], lhsT=lw1[:, kc, 192 + hh * 128:192 + (hh + 1) * 128], rhs=S['big'][:, kc, 0:n], start=(kc == 0), stop=(kc == NKC - 1)), reads=[lw1, S['big']], writes=[ps])
                        b.op("act", lambda e: e.activation(out=lmid[:, 2 + hh, 0:n], in_=ps[:, 0:n], func=AF.Sigmoid), reads=[ps], writes=[lmid])
            for cc_ in range(8):
                cs = slice(cc_ * 128, (cc_ + 1) * 128)
                ps = pst[cc_ % 4]
                b.op("pe", lambda e: e.matmul(ps[:, 0:n], lhsT=lw2[0:96, 0, cs], rhs=lmid[0:96, 0, 0:n], start=True, stop=True), reads=[lw2, lmid], writes=[ps])
                b.op("act", lambda e: e.activation(out=t2[:, 0:n], in_=ps[:, 0:n], func=AF.Sigmoid, bias=evp[:, 0, cc_:cc_ + 1], scale=1.0), reads=[ps, evp], writes=[t2])
                b.op("act", lambda e: e.activation(out=t2[:, 0:n], in_=t2[:, 0:n], func=AF.Exp, scale=-EXPC), reads=[t2], writes=[t2])
                b.dma("sp", sW[cs, t0:t0 + n], t2[:, 0:n], reads=[t2], writes=[sW.rs[ti]])
                ps = pst[4 + cc_ % 2]
                b.op("pe", lambda e: e.matmul(ps[:, 0:n], lhsT=lw2[0:96, 1, cs], rhs=lmid[0:96, 1, 0:n], start=True, stop=True), reads=[lw2, lmid], writes=[ps])
                b.op("act", lambda e: e.activation(out=t4[:, 0:n], in_=ps[:, 0:n], func=AF.Sigmoid, bias=evp[:, 1, cc_:cc_ + 1], scale=1.0), reads=[ps, evp], writes=[t4])
                ps = pst[6 + cc_ % 2]
                for hh in range(2):
                    b.op("pe", lambda e: e.matmul(ps[:, 0:n], lhsT=lw2[:, 2 + hh, cs], rhs=lmid[:, 2 + hh, 0:n], start=(hh == 0), stop=(hh == 1)), reads=[lw2, lmid], writes=[ps])
                b.op("act", lambda e: e.copy(out=t3[:, 0:n], in_=ps[:, 0:n]), reads=[ps], writes=[t3])
                b.dma("sp", sG[cs, t0:t0 + n], t3[:, 0:n], reads=[t3], writes=[sG.rs[ti]])
                b.op("dve", lambda e: e.tensor_scalar(out=t1[:, 0:n], in0=kmix[:, cc_, 0:n], scalar1=evp[:, 2, cc_:cc_ + 1], scalar2=None, op0=ALU.mult), reads=[kmix, evp], writes=[t1])
                b.op("dve", lambda e: e.tensor_tensor(out=t2[:, 0:n], in0=t1[:, 0:n], in1=t1[:, 0:n], op=ALU.mult), reads=[t1], writes=[t2])
                ps = pst[cc_ % 4]
                b.op("pe", lambda e: e.matmul(ps[:, 0:n], lhsT=blk1[:], rhs=t2[:, 0:n], start=True, stop=True), reads=[blk1, t2], writes=[ps])
                b.op("dve", lambda e: e.tensor_scalar(out=t2[:, 0:n], in0=ps[:, 0:n], scalar1=1e-24, scalar2=None, op0=ALU.max), reads=[ps], writes=[t2])
                b.op("act", lambda e: e.activation(out=t2[:, 0:n], in_=t2[:, 0:n], func=AF.Ln), reads=[t2], writes=[t2])
                b.op("act", lambda e: e.activation(out=t2[:, 0:n], in_=t2[:, 0:n], func=AF.Exp, scale=-0.5), reads=[t2], writes=[t2])
                b.op("dve", lambda e: e.tensor_tensor(out=t1[:, 0:n], in0=t1[:, 0:n], in1=t2[:, 0:n], op=ALU.mult), reads=[t1, t2], writes=[t1])
                b.op("dve", lambda e: e.tensor_tensor(out=t2[:, 0:n], in0=t1[:, 0:n], in1=t4[:, 0:n], op=ALU.mult), reads=[t1, t4], writes=[t2])
                b.dma("sp", sBb[cs, t0:t0 + n], t2[:, 0:n], reads=[t2], writes=[sBb.rs[ti]])
                b.op("act", lambda e: e.mul(out=t3[:, 0:n], in_=t1[:, 0:n], mul=-1.0), reads=[t1], writes=[t3])
                b.dma("sp", sA[cs, t0:t0 + n], t3[:, 0:n], reads=[t3], writes=[sA.rs[ti]])
                b.op("dve", lambda e: e.tensor_scalar(out=t4[:, 0:n], in0=t4[:, 0:n], scalar1=-1.0, scalar2=evp[:, 3, cc_:cc_ + 1], op0=ALU.add, op1=ALU.mult), reads=[t4, evp], writes=[t4])
                b.op("dve", lambda e: e.scalar_tensor_tensor(out=t1[:, 0:n], in0=t4[:, 0:n], scalar=1.0, in1=kmix[:, cc_, 0:n], op0=ALU.add, op1=ALU.mult), reads=[t4, kmix], writes=[t1])
                b.dma("sp", sK2[cs, t0:t0 + n], t1[:, 0:n], reads=[t1], writes=[sK2.rs[ti]])
        b.pop()

    TBM = 32
    VB = 8

    def scan(e_, ctx):
        if ctx == 0 and not DO_PROMPT[0]:
            return
        b.push()
        stA = b.sb("stA", [64, 16, 64])
        SS = [b.sb("Ssb", [128, 8, 64]) for _ in range(2)]
        Ssb = SS[0]
        stmp = b.sb("stmp", [128, 8, 64]); stm2 = b.sb("stm2", [128, 8, 64])
        blkv2 = [{k: b.sb("blk_" + k, [128, 8, TBM]) for k in ("r", "w", "k", "a", "b")} for _ in range(2)]
        R22 = [b.sb("R2", [128, 8, TBM, 2]) for _ in range(2)]
        vrow2 = [b.sb("vrow", [2, VB, 512]) for _ in range(2)]
        osb = b.sb("osb", [64, 8, 2, TBM])
        T0, T_, TB = (0, TP, TBM) if ctx == 0 else (TP, TS, TS)
        if ctx == 0:
            b.op("dve", lambda e: e.memset(Ssb[:], 0.0), writes=[Ssb])
        else:
            b.dma("sp", stA[:], st_wkv[e_].rearrange("h v k -> v h k"), writes=[stA])
            for c_ in range(8):
                b.op("pe", lambda e: e.transpose(pst[6][:, 0:64], stA[:, 2 * c_:2 * c_ + 2, :].rearrange("v j k -> v (j k)"), idf[0:64, 0:64]), reads=[stA, idf], writes=[pst[6]])
                b.op("dve", lambda e: e.tensor_copy(out=Ssb[:, c_, :], in_=pst[6][:, 0:64]), reads=[pst[6]], writes=[Ssb])
        srcs = {"r": sR, "w": sW, "k": sK2, "a": sA, "b": sBb}
        nblk = T_ // TB
        nsub = (TB + VB - 1) // VB

        def issue_block_loads(bi):
            t0 = T0 + bi * TB
            ti = t0 // 512
            blkv = blkv2[bi % 2]
            for k_, s_ in srcs.items():
                b.dma("sp", blkv[k_][:, :, 0:TB], s_[:, t0:t0 + TB].rearrange("(c p) t -> p c t", p=128), reads=[s_.rs[ti]], writes=[blkv[k_]])
            for j in range(2):
                b.op("pool", lambda e: e.tensor_scalar(out=R22[bi % 2][:, :, 0:TB, j], in0=blkv["r"][:, :, 0:TB], scalar1=maskj[:, j:j + 1], scalar2=None, op0=ALU.mult), reads=[blkv["r"], maskj], writes=[R22[bi % 2]])

        def issue_vrow(su):
            bi, sb_ = divmod(su, nsub)
            t = sb_ * VB
            tg = T0 + bi * TB + t
            nv = min(VB, TB - t)
            vr = vrow2[su % 2]
            for j in range(2):
                b.dma("sp", vr[j:j + 1, 0:nv, :].rearrange("o t (c v) -> o t c v", v=64), sVtm[tg:tg + nv, :].rearrange("t (c j v) -> j t c v", j=2, v=64)[j:j + 1], reads=[sVtm.rs[tg // 512]], writes=[vr])

        issue_block_loads(0)
        issue_vrow(0)
        for bi in range(nblk):
            t0 = T0 + bi * TB
            ti = t0 // 512
            blkv = blkv2[bi % 2]
            R2 = R22[bi % 2]
            if bi + 1 < nblk:
                issue_block_loads(bi + 1)
            ops_ = pst[2]
            opv = ops_[0:64, 0:8 * TB * 2].rearrange("p (c t j) -> p c t j", c=8, j=2)
            pend = [None]

            def flush_o():
                if pend[0] is not None:
                    Sn_, t_ = pend[0]
                    for c_ in range(8):
                        b.op("pe", lambda e: e.matmul(opv[:, c_, t_, :], lhsT=Sn_[:, c_, :], rhs=R2[:, c_, t_, :], start=True, stop=True), reads=[Sn_, R2], writes=[ops_])
                    pend[0] = None

            for t in range(TB):
                su = bi * nsub + t // VB
                vrow = vrow2[su % 2]
                if t % VB == 0 and su + 1 < nblk * nsub:
                    issue_vrow(su + 1)
                gstep = bi * TB + t
                Sc, Sn = SS[gstep % 2], SS[(gstep + 1) % 2]
                bc = lambda k_: blkv[k_][:, :, t:t + 1].to_broadcast([128, 8, 64])
                b.op("dve", lambda e: e.tensor_tensor(out=stmp[:], in0=Sc[:], in1=bc("a"), op=ALU.mult), reads=[Sc, blkv["a"]], writes=[stmp])
                b.op("pe", lambda e: e.matmul(pst[0][:, 0:512], lhsT=blk1[:], rhs=stmp[:].rearrange("p c v -> p (c v)"), start=True, stop=True), reads=[blk1, stmp], writes=[pst[0]])
                pv = pst[4 + t % 2]
                b.op("pe", lambda e: e.matmul(pv[:, 0:512], lhsT=sel[:], rhs=vrow[:, t % VB, :], start=True, stop=True), reads=[sel, vrow], writes=[pv])
                flush_o()
                b.op("dve", lambda e: e.tensor_tensor(out=Sn[:], in0=Sc[:], in1=bc("w"), op=ALU.mult), reads=[Sc, blkv["w"]], writes=[Sn])
                b.op("dve", lambda e: e.tensor_tensor(out=stm2[:], in0=pv[:, 0:512].rearrange("p (c v) -> p c v", v=64), in1=bc("k"), op=ALU.mult), reads=[pv, blkv["k"]], writes=[stm2])
                b.op("dve", lambda e: e.tensor_tensor(out=Sn[:], in0=Sn[:], in1=stm2[:], op=ALU.add), reads=[Sn, stm2], writes=[Sn])
                b.op("dve", lambda e: e.tensor_tensor(out=stm2[:], in0=pst[0][:, 0:512].rearrange("p (c v) -> p c v", v=64), in1=bc("b"), op=ALU.mult), reads=[pst[0], blkv["b"]], writes=[stm2])
                b.op("dve", lambda e: e.tensor_tensor(out=Sn[:], in0=Sn[:], in1=stm2[:], op=ALU.add), reads=[Sn, stm2], writes=[Sn])
                pend[0] = (Sn, t)
            flush_o()
            for j in range(2):
                b.op("act", lambda e: e.copy(out=osb[:, :, j, 0:TB], in_=opv[:, :, 0:TB, j]), reads=[ops_], writes=[osb])
            for j in range(2):
                b.dma("sp", sO[:, t0:t0 + TB].rearrange("(c j v) t -> j v c t", j=2, v=64)[j], osb[:, :, j, 0:TB], reads=[osb], writes=[sO.rs[ti]])
        Sfin = SS[T_ % 2]
        for c_ in range(8):
            b.op("pe", lambda e: e.transpose(pst[6][0:64, 0:128], Sfin[:, c_, :], idf[:]), reads=[Sfin, idf], writes=[pst[6]])
            b.op("dve", lambda e: e.tensor_copy(out=stA[:, 2 * c_:2 * c_ + 2, :].rearrange("v j k -> v (j k)"), in_=pst[6][0:64, 0:128]), reads=[pst[6]], writes=[stA])
        b.dma("sp", o_wkv[ctx][e_].rearrange("h v k -> v h k"), stA[:], reads=[stA], writes=[o_wkv[ctx]])
        b.pop()

    def even_C(l, e_):
        b.push()
        S['yt'] = b.sb("ytC", [128, NKC, 512]); S['big'] = b.sb("bigC", [128, NKC, 512], BF16)
        upad = b.sb("upad", [128, 8, 542]); ucar = b.sb("ucar", [128, 8, 30]); ust = b.sb("ust", [128, 8, 30])
        ubt = b.sb("ubt", [128, 8, 512])
        for (t0, n, ctx) in TILES:
            ti = t0 // 512
            last = (t0 + n == TP) or ctx == 1
            if t0 == 0:
                b.op("dve", lambda e: e.memset(ucar[:], 0.0), writes=[ucar])
            if ctx == 1:
                for c_ in range(8):
                    b.dma("sp", ucar[:, c_, :], st_convb[e_, :, c_ * 128:(c_ + 1) * 128].rearrange("j p -> p j"), writes=[ucar])
            b.op("dve", lambda e: e.tensor_copy(out=upad[:, :, 0:30], in_=ucar[:]), reads=[ucar], writes=[upad])
            b.dma("sp", upad[:, :, 30:30 + n], sU[:, t0:t0 + n].rearrange("(c p) t -> p c t", p=128), reads=[sU.rs[ti]], writes=[upad])
            b.op("dve", lambda e: e.tensor_copy(out=ucar[:], in_=upad[:, :, n:n + 30]), reads=[upad], writes=[ucar])
            if last:
                b.op("dve", lambda e: e.tensor_copy(out=ust[:], in_=upad[:, :, n:n + 30]), reads=[upad], writes=[ust])
                for c_ in range(8):
                    b.dma("sp", o_convb[ctx][e_, :, c_ * 128:(c_ + 1) * 128].rearrange("j p -> p j"), ust[:, c_, :], reads=[ust], writes=[o_convb[ctx]])
            for cc_ in range(8):
                cs = slice(cc_ * 128, (cc_ + 1) * 128)
                b.dma("sp", t1[:, 0:n], sO[cs, t0:t0 + n], reads=[sO.rs[ti]], writes=[t1])
                ps = pst[0]
                b.op("pe", lambda e: e.matmul(ps[:, 0:n], lhsT=blk1[:], rhs=t1[:, 0:n], start=True, stop=True), reads=[blk1, t1], writes=[ps])
                b.op("dve", lambda e: e.scalar_tensor_tensor(out=t1[:, 0:n], in0=ps[:, 0:n], scalar=-1.0 / 64, in1=t1[:, 0:n], op0=ALU.mult, op1=ALU.add), reads=[ps, t1], writes=[t1])
                b.op("dve", lambda e: e.tensor_tensor(out=t2[:, 0:n], in0=t1[:, 0:n], in1=t1[:, 0:n], op=ALU.mult), reads=[t1], writes=[t2])
                ps = pst[1]
                b.op("pe", lambda e: e.matmul(ps[:, 0:n], lhsT=blk1[:], rhs=t2[:, 0:n], start=True, stop=True), reads=[blk1, t2], writes=[ps])
                b.op("act", lambda e: e.activation(out=t2[:, 0:n], in_=ps[:, 0:n], func=AF.Ln, bias=64e-5, scale=1.0 / 64), reads=[ps], writes=[t2])
                b.op("act", lambda e: e.activation(out=t2[:, 0:n], in_=t2[:, 0:n], func=AF.Exp, scale=-0.5), reads=[t2], writes=[t2])
                b.op("dve", lambda e: e.tensor_tensor(out=t1[:, 0:n], in0=t1[:, 0:n], in1=t2[:, 0:n], op=ALU.mult), reads=[t1, t2], writes=[t1])
                b.op("dve", lambda e: e.tensor_scalar(out=t1[:, 0:n], in0=t1[:, 0:n], scalar1=evp[:, 5, cc_:cc_ + 1], scalar2=evp[:, 6, cc_:cc_ + 1], op0=ALU.mult, op1=ALU.add), reads=[t1, evp], writes=[t1])
                b.dma("sp", t2[:, 0:n], sR[cs, t0:t0 + n], reads=[sR.rs[ti]], writes=[t2])
                b.dma("sp", t3[:, 0:n], sK2[cs, t0:t0 + n], reads=[sK2.rs[ti]], writes=[t3])
                b.op("dve", lambda e: e.scalar_tensor_tensor(out=t2[:, 0:n], in0=t2[:, 0:n], scalar=evp[:, 4, cc_:cc_ + 1], in1=t3[:, 0:n], op0=ALU.mult, op1=ALU.mult), reads=[t2, t3, evp], writes=[t2])
                ps = pst[2]
                b.op("pe", lambda e: e.matmul(ps[:, 0:n], lhsT=blk1[:], rhs=t2[:, 0:n], start=True, stop=True), reads=[blk1, t2], writes=[ps])
                b.dma("sp", t3[:, 0:n], sV[cs, t0:t0 + n], reads=[sV.rs[ti]], writes=[t3])
                b.op("dve", lambda e: e.tensor_tensor(out=t3[:, 0:n], in0=t3[:, 0:n], in1=ps[:, 0:n], op=ALU.mult), reads=[t3, ps], writes=[t3])
                b.op("dve", lambda e: e.tensor_tensor(out=t1[:, 0:n], in0=t1[:, 0:n], in1=t3[:, 0:n], op=ALU.add), reads=[t1, t3], writes=[t1])
                b.dma("sp", t4[:, 0:n], sG[cs, t0:t0 + n], reads=[sG.rs[ti]], writes=[t4])
                b.op("dve", lambda e: e.tensor_tensor(out=S['big'][:, cc_, 0:n], in0=t1[:, 0:n], in1=t4[:, 0:n], op=ALU.mult), reads=[t1, t4], writes=[S['big']])
                b.op("pool", lambda e: e.tensor_scalar(out=ubt[:, cc_, 0:n], in0=upad[:, cc_, 0:n], scalar1=cw[:, 0, cc_:cc_ + 1], scalar2=evp[:, 7, cc_:cc_ + 1], op0=ALU.mult, op1=ALU.add), reads=[upad, cw, evp], writes=[ubt])
                for j in range(1, 31):
                    b.op("dve", lambda e: e.scalar_tensor_tensor(out=ubt[:, cc_, 0:n], in0=upad[:, cc_, j:j + n], scalar=cw[:, j, cc_:cc_ + 1], in1=ubt[:, cc_, 0:n], op0=ALU.mult, op1=ALU.add), reads=[upad, cw, ubt], writes=[ubt])
            ps = pst[3]
            for cc_ in range(8):
                b.op("pe", lambda e: e.matmul(ps[:, 0:n], lhsT=onesf[:], rhs=ubt[:, cc_, 0:n], start=(cc_ == 0), stop=(cc_ == 7)), reads=[onesf, ubt], writes=[ps])
            b.op("act", lambda e: e.mul(out=t4[:, 0:n], in_=ps[:, 0:n], mul=-1.0 / 1024), reads=[ps], writes=[t4])
            for cc_ in range(8):
                b.op("dve", lambda e: e.tensor_tensor(out=ubt[:, cc_, 0:n], in0=ubt[:, cc_, 0:n], in1=t4[:, 0:n], op=ALU.add), reads=[ubt, t4], writes=[ubt])
            sumsq_rstd(ubt, n, 8, 1024, 1e-5, ubt)
            for cc_ in range(8):
                b.op("dve", lambda e: e.tensor_tensor(out=t2[:, 0:n], in0=ubt[:, cc_, 0:n], in1=rstd[:, 0:n], op=ALU.mult), reads=[ubt, rstd], writes=[t2])
                b.op("act", lambda e: e.activation(out=S['big'][:, 8 + cc_, 0:n], in_=t2[:, 0:n], func=AF.Silu, bias=evp[:, 9, cc_:cc_ + 1], scale=evp[:, 8, cc_:cc_ + 1]), reads=[t2, evp], writes=[S['big']])
            out_proj(ab_w_out[e_], NKC, lambda kc: S['big'][:, kc, 0:n], n, [S['big']])
            post_norm_residual(l, 0, ti, t0, n, ctx)
        b.pop()

    b.even = (even_A, scan, even_C)

    def odd_E(l, o_):
        b.push()
        qst = b.sb("qst", [128, 4, 512], BF16); ktm = b.sb("ktm", [128, 512]); vtb = b.sb("vtb", [128, 512], BF16)
        for (t0, n, ctx) in TILES:
            ti = t0 // 512
            pre_norm(l, 0, ti, t0, n, ctx)
            for gq in range(12):
                def ev(mc, ps, gq=gq):
                    b.op("act" if mc % 2 else "dve", lambda e: (e.copy if mc % 2 else e.tensor_copy)(out=qst[:, mc, 0:n], in_=ps[:, 0:n]), reads=[ps], writes=[qst])
                mm_group(attn_w_qkv[o_, :, gq * 512:(gq + 1) * 512], NKC, 512, lambda kc: hx[:, kc, 2:n + 2], n, ev, [hx])
                dstT = sQT if gq < 6 else sKT
                r0 = (gq % 6) * 512
                b.dma("sp", dstT[r0:r0 + 512, t0:t0 + n].rearrange("(c p) t -> p c t", p=128), qst[:, :, 0:n], reads=[qst], writes=[dstT.rs[ti]])
            for gi in range(12):
                kv = gi // 6
                g = (gi % 6) // 2
                hh = gi % 2
                keep0 = TP - WINS[g] if ctx == 0 else TP
                if kv == 0 and t0 + n <= keep0:
                    continue
                wt_, wv = load_w(attn_w_qkv[o_, :, 3072 + gi * 512:3072 + (gi + 1) * 512], NKC, 512)
                for s_ in range((n + 127) // 128):
                    m = min(128, n - s_ * 128)
                    if kv == 0 and t0 + s_ * 128 < keep0:
                        continue
                    ps = pst[s_ % 4]
                    for kc in range(NKC):
                        b.op("pe", lambda e: e.matmul(ps[0:m, 0:512], lhsT=hx[:, kc, 2 + s_ * 128:2 + s_ * 128 + m], rhs=wv[:, kc, :], start=(kc == 0), stop=(kc == NKC - 1)), reads=[wt_, hx], writes=[ps])
                    tok = t0 + s_ * 128
                    if tok >= keep0:
                        b.op("act", lambda e: e.copy(out=ktm[0:m, :], in_=ps[0:m, 0:512]), reads=[ps], writes=[ktm])
                        b.dma("sp", o_kv[ctx][g][o_, kv, tok - keep0:tok - keep0 + m, hh * 512:(hh + 1) * 512], ktm[0:m, :], reads=[ktm], writes=[o_kv[ctx][g]])
                    if kv == 1:
                        b.op("dve", lambda e: e.tensor_copy(out=vtb[0:m, :], in_=ps[0:m, 0:512]), reads=[ps], writes=[vtb])
                        b.dma("sp", sVa[tok:tok + m, (gi - 6) * 512:(gi - 5) * 512], vtb[0:m, :], reads=[vtb], writes=[sVa.rs[ti]])
        b.pop()

    def odd_F(l, o_):
        b.push()
        numacc = b.sb("numacc", [128, TP]); denacc = b.sb("denacc", [128, TP])
        qT = b.sb("qT", [128, TP], BF16); kT = b.sb("kT", [128, TP], BF16)
        qd = b.sb("qd", [128, TP], BF16); kd = b.sb("kd", [128, TP], BF16)
        vt = b.sb("vt", [128, 32, 128], BF16); ot = b.sb("ot", [128, TP], BF16)
        pT = [b.sb(f"pT{i}", [128, 256], BF16) for i in range(2)]
        allres = lambda T_: T_.rs[0:8]
        for h in (range(8) if DO_PROMPT[0] else ()):
            for g in range(3):
                d = DILS[g]
                nb = TP // d // 128
                ch = g * 8 + h
                b.dma("sp", qT[:], sQT[ch * 128:(ch + 1) * 128, 0:TP], reads=allres(sQT), writes=[qT])
                b.dma("sp", kT[:], sKT[ch * 128:(ch + 1) * 128, 0:TP], reads=allres(sKT), writes=[kT])
                if d == 1:
                    qq, kk_ = qT, kT
                else:
                    b.op("pool", lambda e: e.tensor_copy(out=qd[:].rearrange("p (dd i) -> p dd i", dd=d), in_=qT[:].rearrange("p (i dd) -> p dd i", dd=d)), reads=[qT], writes=[qd])
                    b.op("dve", lambda e: e.tensor_copy(out=kd[:].rearrange("p (dd i) -> p dd i", dd=d), in_=kT[:].rearrange("p (i dd) -> p dd i", dd=d)), reads=[kT], writes=[kd])
                    qq, kk_ = qd, kd
                vsrc = sVa[0:TP, ch * 128:(ch + 1) * 128].rearrange("(jt m dd) c -> dd m jt c", m=128, dd=d)
                for rho in range(d):
                    b.dma("sp", vt[:, rho * nb:(rho + 1) * nb, :], vsrc[rho], reads=allres(sVa), writes=[vt])
                for rho in range(d):
                    for blk in range(nb):
                        bi = rho * nb + blk
                        c0 = bi * 128
                        ps_s, ps_n, ps_d, pt = pst[bi % 2], pst[2 + bi % 2], pst[4 + bi % 2], pT[bi % 2]
                        b.op("pe", lambda e: e.matmul(ps_s[:, 0:128], lhsT=kk_[:, c0:c0 + 128], rhs=qq[:, c0:c0 + 128], start=True, stop=True), reads=[kk_, qq], writes=[ps_s])
                        ncol = 128
                        if blk > 0:
                            b.op("pe", lambda e: e.matmul(ps_s[:, 128:256], lhsT=kk_[:, c0 - 128:c0], rhs=qq[:, c0:c0 + 128], start=True, stop=True), reads=[kk_, qq], writes=[ps_s])
                            ncol = 256
                        b.op("act", lambda e: e.activation(out=pt[:, 0:ncol], in_=ps_s[:, 0:ncol], func=AF.Exp, scale=C_SCALE), reads=[ps_s], writes=[pt])
                        b.op("pool", lambda e: e.tensor_tensor(out=pt[:, 0:ncol], in0=pt[:, 0:ncol], in1=mask2[:, 0:ncol], op=ALU.mult), reads=[pt, mask2], writes=[pt])
                        b.op("pe", lambda e: e.matmul(ps_n[:, 0:128], lhsT=vt[:, bi, :], rhs=pt[:, 0:128], start=True, stop=(blk == 0)), reads=[vt, pt], writes=[ps_n])
                        if blk > 0:
                            b.op("pe", lambda e: e.matmul(ps_n[:, 0:128], lhsT=vt[:, bi - 1, :], rhs=pt[:, 128:256], start=False, stop=True), reads=[vt, pt], writes=[ps_n])
                        b.op("pe", lambda e: e.matmul(ps_d[:, 0:128], lhsT=onesb[:], rhs=pt[:, 0:128], start=True, stop=(blk == 0)), reads=[onesb, pt], writes=[ps_d])
                        if blk > 0:
                            b.op("pe", lambda e: e.matmul(ps_d[:, 0:128], lhsT=onesb[:], rhs=pt[:, 128:256], start=False, stop=True), reads=[onesb, pt], writes=[ps_d])
                        st = rho + d * blk * 128
                        sl = slice(st, st + d * 127 + 1, d)
                        if g == 0:
                            b.op("act", lambda e: e.copy(out=numacc[:, sl], in_=ps_n[:, 0:128]), reads=[ps_n], writes=[numacc])
                            b.op("act", lambda e: e.copy(out=denacc[:, sl], in_=ps_d[:, 0:128]), reads=[ps_d], writes=[denacc])
                        else:
                            b.op("dve", lambda e: e.tensor_tensor(out=numacc[:, sl], in0=numacc[:, sl], in1=ps_n[:, 0:128], op=ALU.add), reads=[ps_n, numacc], writes=[numacc])
                            b.op("dve", lambda e: e.tensor_tensor(out=denacc[:, sl], in0=denacc[:, sl], in1=ps_d[:, 0:128], op=ALU.add), reads=[ps_d, denacc], writes=[denacc])
            b.op("dve", lambda e: e.reciprocal(out=denacc[:], in_=denacc[:]), reads=[denacc], writes=[denacc])
            b.op("dve", lambda e: e.tensor_tensor(out=ot[:], in0=numacc[:], in1=denacc[:], op=ALU.mult), reads=[numacc, denacc], writes=[ot])
            b.dma("sp", sOT[h * 128:(h + 1) * 128, 0:TP], ot[:], reads=[ot], writes=allres(sOT))
        b.pop()
        b.push()
        kcf = b.sb("kcf", [128, 1024]); vcf = b.sb("vcf", [128, 1024]); vcb = b.sb("vcb", [128, 1024], BF16)
        kTs = b.sb("kTs", [128, 128], BF16); pS = b.sb("pS", [128, 4], BF16)
        qs4 = b.sb("qs4", [128, 24, 4], BF16); kn4 = b.sb("kn4", [128, 24, 4], BF16); vn4 = b.sb("vn4", [4, 3072], BF16)
        nums = b.sb("nums", [128, 8, 4]); dens = b.sb("dens", [128, 8, 4]); os4 = b.sb("os4", [128, 8, 4], BF16)
        b.dma("sp", qs4[:], sQT[:, TP:NT].rearrange("(c p) t -> p c t", p=128), reads=[sQT.rs[8]], writes=[qs4])
        b.dma("sp", kn4[:], sKT[:, TP:NT].rearrange("(c p) t -> p c t", p=128), reads=[sKT.rs[8]], writes=[kn4])
        b.dma("sp", vn4[:], sVa[TP:NT, :], reads=[sVa.rs[8]], writes=[vn4])
        colsel = b.sb("colsel", [128, 4, 4], BF16)
        b.op("dve", lambda e: e.memset(colsel[:], 0.0), writes=[colsel])
        for tt_ in range(4):
            b.op("dve", lambda e: e.memset(colsel[:, tt_, tt_:tt_ + 1], 1.0), reads=[colsel], writes=[colsel])
        b.op("dve", lambda e: e.memset(nums[:], 0.0), writes=[nums])
        b.op("dve", lambda e: e.memset(dens[:], 0.0), writes=[dens])

        def accum(h, cols, lhs_v, lhs_one, p_ap, krows):
            b.op("pe", lambda e: e.matmul(pst[2][:, 0:len(cols)], lhsT=lhs_v, rhs=p_ap, start=True, stop=True), reads=[vcb, vn4, pS], writes=[pst[2]])
            b.op("pe", lambda e: e.matmul(pst[3][:, 0:len(cols)], lhsT=lhs_one, rhs=p_ap, start=True, stop=True), reads=[onesb, pS], writes=[pst[3]])
            c0, c1 = cols[0], cols[-1] + 1
            b.op("dve", lambda e: e.tensor_tensor(out=nums[:, h, c0:c1], in0=nums[:, h, c0:c1], in1=pst[2][:, 0:len(cols)], op=ALU.add), reads=[nums, pst[2]], writes=[nums])
            b.op("dve", lambda e: e.tensor_tensor(out=dens[:, h, c0:c1], in0=dens[:, h, c0:c1], in1=pst[3][:, 0:len(cols)], op=ALU.add), reads=[dens, pst[3]], writes=[dens])

        for g in range(3):
            d = DILS[g]
            ck = cks[g]
            tiles_ = [(None, [0, 1, 2, 3])] if d == 1 else [(tt, [tt]) for tt in range(4)]
            for tt, cols in tiles_:
                ksrc = ck[o_, 0, :, :] if d == 1 else ck[o_, 0, :, :].rearrange("(i dd) c -> dd i c", dd=d)[tt]
                vsrc = ck[o_, 1, :, :] if d == 1 else ck[o_, 1, :, :].rearrange("(i dd) c -> dd i c", dd=d)[tt]
                b.dma("sp", kcf[:], ksrc, writes=[kcf])
                b.dma("sp", vcf[:], vsrc, writes=[vcf])
                b.op("pool", lambda e: e.tensor_copy(out=vcb[:], in_=vcf[:]), reads=[vcf], writes=[vcb])
                for h in range(8):
                    ch = g * 8 + h
                    b.op("pe", lambda e: e.transpose(pst[0][:, 0:128], kcf[:, h * 128:(h + 1) * 128], idf[:]), reads=[kcf, idf], writes=[pst[0]])
                    b.op("act", lambda e: e.copy(out=kTs[:], in_=pst[0][:, 0:128]), reads=[pst[0]], writes=[kTs])
                    b.op("pe", lambda e: e.matmul(pst[1][:, 0:4], lhsT=kTs[:], rhs=qs4[:, ch, :], start=True, stop=True), reads=[kTs, qs4], writes=[pst[1]])
                    b.op("act", lambda e: e.activation(out=pS[:, 0:4], in_=pst[1][:, 0:4], func=AF.Exp, scale=C_SCALE), reads=[pst[1]], writes=[pS])
                    msk_ = mask2[:, 128:132] if d == 1 else colsel[:, tt, :]
                    b.op("dve", lambda e: e.tensor_tensor(out=pS[:, 0:4], in0=pS[:, 0:4], in1=msk_, op=ALU.mult), reads=[pS, mask2, colsel], writes=[pS])
                    accum(h, [0, 1, 2, 3], vcb[:, h * 128:(h + 1) * 128], onesb[:], pS[:, 0:4], 128)
            for h in range(8):
                ch = g * 8 + h
                b.op("pe", lambda e: e.matmul(pst[1][0:4, 0:4], lhsT=kn4[:, ch, :], rhs=qs4[:, ch, :], start=True, stop=True), reads=[kn4, qs4], writes=[pst[1]])
                b.op("act", lambda e: e.activation(out=pS[0:4, 0:4], in_=pst[1][0:4, 0:4], func=AF.Exp, scale=C_SCALE), reads=[pst[1]], writes=[pS])
                msk = mask2[0:4, 0:4] if d == 1 else idb[0:4, 0:4]
                b.op("dve", lambda e: e.tensor_tensor(out=pS[0:4, 0:4], in0=pS[0:4, 0:4], in1=msk, op=ALU.mult), reads=[pS, mask2, idb], writes=[pS])
                accum(h, [0, 1, 2, 3], vn4[0:4, ch * 128:(ch + 1) * 128], onesb[0:4, :], pS[0:4, 0:4], 4)
        b.op("dve", lambda e: e.reciprocal(out=dens[:], in_=dens[:]), reads=[dens], writes=[dens])
        b.op("dve", lambda e: e.tensor_tensor(out=os4[:], in0=nums[:], in1=dens[:], op=ALU.mult), reads=[nums, dens], writes=[os4])
        b.dma("sp", sOT[:, TP:NT].rearrange("(h p) t -> p h t", p=128), os4[:], reads=[os4], writes=[sOT.rs[8]])
        b.pop()

    def odd_G(l, o_):
        b.push()
        S['yt'] = b.sb("ytG", [128, NKC, 512]); ot8 = b.sb("ot8", [128, 8, 512], BF16)
        for (t0, n, ctx) in TILES:
            ti = t0 // 512
            b.dma("sp", ot8[:, :, 0:n], sOT[:, t0:t0 + n].rearrange("(h p) t -> p h t", p=128), reads=[sOT.rs[ti]], writes=[ot8])
            out_proj(attn_w_out[o_], 8, lambda kc: ot8[:, kc, 0:n], n, [ot8])
            post_norm_residual(l, 0, ti, t0, n, ctx)
        b.pop()

    def final_out():
        b.push()
        xo = b.sb("xo", [128, D])
        for (t0, n, ctx) in TILES:
            ti = t0 // 512
            b.dma("sp", xt[:, :, 0:n], X[:, t0:t0 + n].rearrange("(k p) t -> p k t", p=128), reads=[X.rs[ti]], writes=[xt])
            for s_ in range((n + 127) // 128):
                m = min(128, n - s_ * 128)
                for kc in range(NKC):
                    ps = pst[kc % 4]
                    b.op("pe", lambda e: e.transpose(ps[0:m, 0:128], xt[:, kc, s_ * 128:s_ * 128 + m], idf[:]), reads=[xt, idf], writes=[ps])
                    b.op("act" if kc % 2 else "dve", lambda e: (e.copy if kc % 2 else e.tensor_copy)(out=xo[0:m, kc * 128:(kc + 1) * 128], in_=ps[0:m, 0:128]), reads=[ps], writes=[xo])
                if ctx == 0:
                    b.dma("sp", yp[t0 + s_ * 128:t0 + s_ * 128 + m, :], xo[0:m, :], reads=[xo], writes=[yp.rs[ti]])
                else:
                    b.dma("sp", ys[0:m, :], xo[0:m, :], reads=[xo], writes=[ys])
        b.pop()

    b.odd = (odd_E, odd_F, odd_G)
    b.final_out = final_out
    b.ffn_layer = ffn_layer
    b.ctx_objs = dict(locals())
    return b


def _finish(b):
    o = b.ctx_objs
    b.finish(o["all_outs"])
    o["nc_cm"].__exit__(None, None, None)
    b.close()
    return b.nc


_CACHE = {}


def build_full(stop=None):
    b = build_program()
    ea, sc, ec = b.even
    oe, of, og = b.odd
    stages = []
    for l in range(4):
        if l % 2 == 0:
            stages += [lambda l=l: ea(l, l // 2), lambda l=l: sc(l // 2, 0), lambda l=l: sc(l // 2, 1), lambda l=l: ec(l, l // 2)]
        else:
            stages += [lambda l=l: oe(l, l // 2), lambda l=l: of(l, l // 2), lambda l=l: og(l, l // 2)]
        stages.append(lambda l=l: b.ffn_layer(l))
    stages.append(b.final_out)
    for i, st in enumerate(stages):
        if stop is not None and i >= stop:
            break
        st()
    return _finish(b)


def _get_nc():
    if "nc" not in _CACHE:
        _CACHE["nc"] = build_full()
    return _CACHE["nc"]


def make_in_maps(inp):
    f = lambda a: np.ascontiguousarray(np.asarray(a, dtype=np.float32))
    wnames = ["w_mod", "b_mod", "g_pre_mix", "g_post_mix", "g_pre_ffn", "g_post_ffn", "ab_w_in", "a_mu_rkv", "a_mu_wag",
              "a_w0", "a_w1", "a_w2", "a_a0", "a_a1", "a_a2", "a_g1", "a_g2", "a_k_k", "a_k_a", "a_ln_w", "a_ln_b",
              "b_conv_w", "b_conv_b", "b_ln_w", "b_ln_b", "ab_w_out", "attn_w_qkv", "attn_w_out", "ffn_w_gate", "ffn_w_up",
              "ffn_conv_w", "ffn_conv_b", "ffn_w_down"]
    shared = {k: f(inp[k]) for k in wnames}
    shared["a_r_k"] = f(inp["a_r_k"]).reshape(2, 1024)
    in_maps = []
    for c in range(8):
        bb = c % 2
        m = dict(shared)
        m["xp"] = f(inp["x_prompt"][bb]); m["xs"] = f(inp["x_sample"][c])
        m["cc"] = f(np.stack([inp["c_prompt"][bb], inp["c_sample"][c]]))
        m["st_shift"] = f(inp["state_shift"][:, c]); m["st_wkv"] = f(inp["state_wkv"][:, c])
        m["st_convb"] = f(inp["state_conv_b"][:, c]); m["st_ffn"] = f(inp["state_ffn"][:, c])
        for w in WINS:
            m[f"ck{w}"] = f(np.asarray(inp[f"cache_kv_w{w}"])[:, :, c].reshape(2, 2, w, 1024))
        in_maps.append(m)
    return in_maps


def kernel(**inp):
    nc = _get_nc()
    in_maps = make_in_maps(inp)
    res = run_bass_kernel_spmd(nc, in_maps, core_ids=list(range(8))).results
    P = lambda k: np.stack([res[0][k], res[1][k]])
    Sx = lambda k: np.stack([res[c][k] for c in range(8)])
    y_prompt = P("yp"); y_sample = Sx("ys")
    outs = [y_prompt, y_sample,
            np.moveaxis(P("p_shift"), 0, 1), np.moveaxis(P("p_wkv"), 0, 1), np.moveaxis(P("p_convb"), 0, 1), np.moveaxis(P("p_ffn"), 0, 1)]
    for w in WINS:
        a = P(f"pkv{w}")
        outs.append(np.transpose(a, (1, 2, 0, 3, 4)).reshape(2, 2, 2, w, 8, 128))
    outs += [np.moveaxis(Sx("s_shift"), 0, 1), np.moveaxis(Sx("s_wkv"), 0, 1), np.moveaxis(Sx("s_convb"), 0, 1), np.moveaxis(Sx("s_ffn"), 0, 1)]
    for w in WINS:
        a = Sx(f"skv{w}")
        outs.append(np.transpose(a, (1, 2, 0, 3, 4)).reshape(2, 2, 8, TS, 8, 128))
    return tuple(np.ascontiguousarray(o, dtype=np.float32) for o in outs)
```
